# Optimizing a Trainium2 kernel written in Bass

```python
import math
import jax
import jax.numpy as jnp
from jax import lax
import numpy as np

D_MODEL = 1024
BATCH = 4
SEQ = 8192
DEPTH = 1

SSM_WIDTH = 512
SSM_GROUP = 16
SSM_GROUPS = SSM_WIDTH // SSM_GROUP
SSM_STATE = 64
ATT_HEADS = 8
ATT_HEAD_DIM = 64
ATT_WIDTH = ATT_HEADS * ATT_HEAD_DIM
KV_LATENT = 128
IDX_HEADS = 4
IDX_HEAD_DIM = 64
TOPK_MAX = 256
Q_BLOCK = 128
FFN_HIDDEN = ((-(-8 * D_MODEL // 3) + 255) // 256) * 256
IN_SIZES = (SSM_WIDTH, ATT_WIDTH, KV_LATENT, IDX_HEADS * IDX_HEAD_DIM, IDX_HEAD_DIM, IDX_HEADS, D_MODEL, D_MODEL)
D_IN = sum(IN_SIZES)
EPS = 1e-6

kernel_name = "hybrid_s5_dsa_swiglu_adaln"


def rms_norm(x, g):
    xf = x.astype(jnp.float32)
    y = xf * lax.rsqrt(jnp.mean(xf * xf, axis=-1, keepdims=True) + EPS)
    return (y * g.astype(jnp.float32)).astype(x.dtype)


def split_cols(z, sizes):
    offs = []
    o = 0
    for s in sizes[:-1]:
        o += s
        offs.append(o)
    return jnp.split(z, offs, axis=-1)


def s5_branch(xs, a_re, a_im, log_dt, b_re, b_im, c_re, c_im, d_skip, w_glu, b_glu):
    bsz, seq, _ = xs.shape
    xg = xs.reshape(bsz, seq, SSM_GROUPS, SSM_GROUP)
    dt = jnp.exp(log_dt)[:, None]
    mag = jnp.exp(a_re * dt)
    ang = a_im * dt
    lb_re = mag * jnp.cos(ang)
    lb_im = mag * jnp.sin(ang)
    den = a_re * a_re + a_im * a_im
    coef_re = ((lb_re - 1.0) * a_re + lb_im * a_im) / den
    coef_im = (lb_im * a_re - (lb_re - 1.0) * a_im) / den
    bb_re = coef_re[..., None] * b_re - coef_im[..., None] * b_im
    bb_im = coef_re[..., None] * b_im + coef_im[..., None] * b_re
    bu_re = jnp.einsum('blgc,gpc->blgp', xg, bb_re)
    bu_im = jnp.einsum('blgc,gpc->blgp', xg, bb_im)
    ar = jnp.broadcast_to(lb_re, bu_re.shape)
    ai = jnp.broadcast_to(lb_im, bu_im.shape)

    def combine(e1, e2):
        a1r, a1i, b1r, b1i = e1
        a2r, a2i, b2r, b2i = e2
        nar = a1r * a2r - a1i * a2i
        nai = a1r * a2i + a1i * a2r
        nbr = a2r * b1r - a2i * b1i + b2r
        nbi = a2r * b1i + a2i * b1r + b2i
        return (nar, nai, nbr, nbi)

    _, _, s_re, s_im = lax.associative_scan(combine, (ar, ai, bu_re, bu_im), axis=1)
    y = jnp.einsum('blgp,gcp->blgc', s_re, c_re) - jnp.einsum('blgp,gcp->blgc', s_im, c_im)
    y = y.reshape(bsz, seq, SSM_WIDTH) + d_skip * xs
    y = jax.nn.gelu(y)
    z = y @ w_glu + b_glu
    val, gate = jnp.split(z, 2, axis=-1)
    return val * jax.nn.sigmoid(gate)


def dsa_branch(q, c_kv, q_idx, k_idx, w_idx, w_uk, w_uv):
    bsz, seq = q.shape[0], q.shape[1]
    topk = min(TOPK_MAX, seq // 4)
    nblk = seq // Q_BLOCK
    q_lat = jnp.einsum('blhd,chd->blhc', q, w_uk) * (ATT_HEAD_DIM ** -0.5)
    w_idx = w_idx * (IDX_HEADS ** -0.5)
    q_idx = q_idx * (IDX_HEAD_DIM ** -0.5)

    def to_blocks(t):
        return jnp.moveaxis(t.reshape((bsz, nblk, Q_BLOCK) + t.shape[2:]), 1, 0)

    key_pos = jnp.arange(seq)

    def one_block(args):
        blk, ql, qi, wi = args
        q_pos = blk * Q_BLOCK + jnp.arange(Q_BLOCK)
        rel = jax.nn.relu(jnp.einsum('bqhd,bkd->bqhk', qi, k_idx).astype(jnp.float32))
        iscore = jnp.einsum('bqhk,bqh->bqk', rel, wi.astype(jnp.float32))
        causal = key_pos[None, :] <= q_pos[:, None]
        iscore = jnp.where(causal[None], iscore, -jnp.inf)
        _, sel = lax.top_k(iscore, topk)
        valid = sel <= q_pos[None, :, None]
        c_sel = jax.vmap(lambda cb, ib: cb[ib])(c_kv, sel)
        logits = jnp.einsum('bqhc,bqkc->bhqk', ql, c_sel).astype(jnp.float32)
        logits = jnp.where(valid[:, None], logits, -jnp.inf)
        p = jax.nn.softmax(logits, axis=-1).astype(c_sel.dtype)
        return jnp.einsum('bhqk,bqkc->bqhc', p, c_sel)

    o = lax.map(one_block, (jnp.arange(nblk), to_blocks(q_lat), to_blocks(q_idx), to_blocks(w_idx)))
    o = jnp.moveaxis(o, 0, 1).reshape(bsz, seq, ATT_HEADS, KV_LATENT)
    out = jnp.einsum('blhc,chd->blhd', o, w_uv)
    return out.reshape(bsz, seq, ATT_WIDTH)


def setup_inputs(seed: int = 0) -> dict:
    key = jax.random.key(seed)
    ks = jax.random.split(key, 27)
    f32 = jnp.float32

    def nrm(k, shape, s):
        return jax.random.normal(k, shape, f32) * s

    def gain(k, shape):
        return 1.0 + 0.05 * jax.random.normal(k, shape, f32)

    G, P, Cg = SSM_GROUPS, SSM_STATE, SSM_GROUP
    a_im0 = jnp.broadcast_to(jnp.pi * jnp.arange(P, dtype=f32), (DEPTH, G, P))
    return {
        'x': nrm(ks[0], (BATCH, SEQ, D_MODEL), 1.0),
        'c': nrm(ks[1], (BATCH, D_MODEL), 1.0),
        'w_mod': nrm(ks[2], (DEPTH, D_MODEL, 6 * D_MODEL), 0.5 * D_MODEL ** -0.5),
        'b_mod': nrm(ks[3], (DEPTH, 6 * D_MODEL), 0.01),
        'norm1_g': gain(ks[4], (DEPTH, D_MODEL)),
        'w_in': nrm(ks[5], (DEPTH, D_MODEL, D_IN), D_MODEL ** -0.5),
        'ssm_a_re': -0.5 + nrm(ks[6], (DEPTH, G, P), 0.01),
        'ssm_a_im': a_im0 + nrm(ks[7], (DEPTH, G, P), 0.01),
        'ssm_log_dt': jax.random.uniform(ks[8], (DEPTH, G), f32, math.log(1e-3), math.log(1e-1)),
        'ssm_b_re': nrm(ks[9], (DEPTH, G, P, Cg), (2 * Cg) ** -0.5),
        'ssm_b_im': nrm(ks[10], (DEPTH, G, P, Cg), (2 * Cg) ** -0.5),
        'ssm_c_re': nrm(ks[11], (DEPTH, G, Cg, P), (2 * P) ** -0.5),
        'ssm_c_im': nrm(ks[12], (DEPTH, G, Cg, P), (2 * P) ** -0.5),
        'ssm_d': nrm(ks[13], (DEPTH, SSM_WIDTH), 1.0),
        'w_ssm_glu': nrm(ks[14], (DEPTH, SSM_WIDTH, 2 * D_MODEL), SSM_WIDTH ** -0.5),
        'b_ssm_glu': nrm(ks[15], (DEPTH, 2 * D_MODEL), 0.01),
        'kv_norm_g': gain(ks[16], (DEPTH, KV_LATENT)),
        'idx_k_norm_g': gain(ks[17], (DEPTH, IDX_HEAD_DIM)),
        'w_uk': nrm(ks[18], (DEPTH, KV_LATENT, ATT_HEADS, ATT_HEAD_DIM), KV_LATENT ** -0.5),
        'w_uv': nrm(ks[19], (DEPTH, KV_LATENT, ATT_HEADS, ATT_HEAD_DIM), KV_LATENT ** -0.5),
        'w_attn_proj': nrm(ks[20], (DEPTH, ATT_WIDTH, D_MODEL), ATT_WIDTH ** -0.5),
        'w_out': nrm(ks[21], (DEPTH, D_MODEL, D_MODEL), D_MODEL ** -0.5),
        'norm2_g': gain(ks[22], (DEPTH, D_MODEL)),
        'w_ffn_gate': nrm(ks[23], (DEPTH, D_MODEL, FFN_HIDDEN), D_MODEL ** -0.5),
        'w_ffn_up': nrm(ks[24], (DEPTH, D_MODEL, FFN_HIDDEN), D_MODEL ** -0.5),
        'w_ffn_down': nrm(ks[25], (DEPTH, FFN_HIDDEN, D_MODEL), FFN_HIDDEN ** -0.5),
        'final_g': gain(ks[26], (D_MODEL,)),
    }


def reference(x, c, w_mod, b_mod, norm1_g, w_in, ssm_a_re, ssm_a_im, ssm_log_dt, ssm_b_re, ssm_b_im, ssm_c_re, ssm_c_im, ssm_d, w_ssm_glu, b_ssm_glu, kv_norm_g, idx_k_norm_g, w_uk, w_uv, w_attn_proj, w_out, norm2_g, w_ffn_gate, w_ffn_up, w_ffn_down, final_g):
    bsz, seq, _ = x.shape
    h = x
    cond = jax.nn.silu(c)
    for layer in range(DEPTH):
        mod = cond @ w_mod[layer] + b_mod[layer]
        shift1, scale1, gate1, shift2, scale2, gate2 = jnp.split(mod[:, None, :], 6, axis=-1)
        u = rms_norm(h, norm1_g[layer]) * (1.0 + scale1) + shift1
        xs, q, ckv, qi, ki, wi, ga, gb = split_cols(u @ w_in[layer], IN_SIZES)
        y_ssm = s5_branch(xs, ssm_a_re[layer], ssm_a_im[layer], ssm_log_dt[layer], ssm_b_re[layer], ssm_b_im[layer], ssm_c_re[layer], ssm_c_im[layer], ssm_d[layer], w_ssm_glu[layer], b_ssm_glu[layer])
        y_att = dsa_branch(q.reshape(bsz, seq, ATT_HEADS, ATT_HEAD_DIM), rms_norm(ckv, kv_norm_g[layer]), qi.reshape(bsz, seq, IDX_HEADS, IDX_HEAD_DIM), rms_norm(ki, idx_k_norm_g[layer]), wi, w_uk[layer], w_uv[layer]) @ w_attn_proj[layer]
        merged = jax.nn.sigmoid(ga) * y_ssm + jax.nn.sigmoid(gb) * y_att
        h = h + gate1 * (merged @ w_out[layer])
        u2 = rms_norm(h, norm2_g[layer]) * (1.0 + scale2) + shift2
        ffn = (jax.nn.silu(u2 @ w_ffn_gate[layer]) * (u2 @ w_ffn_up[layer])) @ w_ffn_down[layer]
        h = h + gate2 * ffn
    return rms_norm(h, final_g)
```

```python
import contextlib
import math
import numpy as np
import ml_dtypes
import concourse.bass as bass
import concourse.mybir as mybir
from concourse.bass_utils import run_bass_kernel_spmd

F32 = mybir.dt.float32
BF16 = mybir.dt.bfloat16
I32 = mybir.dt.int32
AF = mybir.ActivationFunctionType
ALU = mybir.AluOpType
AX = mybir.AxisListType

D = 1024
L = 8192
NB = 64
NOWN = 32
TC = 8
OFF = dict(xs=0, q=512, ckv=1024, qi=1152, ki=1408, wi=1472, ga=1476, gb=2500)
FF = 2816
EPS = 1e-6
NEG = -1.0e30
BIGM = 30000.0
NBIS = 16
TOPK = 256
DEV_NBLK = None
DEV_STAGE = 0
DEV_SKIP_A = False


class _Op:
    __slots__ = ("eng", "fn", "deps", "inc", "tok", "kind", "uid")


class Sched:
    NDMA = 8

    def __init__(self, nc, es, self_sync=True):
        self.nc = nc
        self.E = {"pe": nc.tensor, "act": nc.scalar, "dve": nc.vector, "pool": nc.gpsimd, "sp": nc.sync}
        self.sem = {k: es.enter_context(nc.semaphore("sem_" + k)) for k in self.E}
        self.dsem = {q: [es.enter_context(nc.semaphore(f"dsem_{q}_{i}")) for i in range(self.NDMA)]
                     for q in ("sp", "pool", "act")}
        self.cnt = {k: 0 for k in self.E}
        self.dcnt = {q: 0 for q in self.dsem}
        self.seen = {k: {} for k in self.E}
        self.lw = {}
        self.lr = {}
        self.ops = []
        self.self_sync = self_sync
        self.uid = 0
        self.final = []
        self.last = {}
        self.pend = []

    def _add(self, o, reads, writes):
        deps = {}
        for b in reads:
            w = self.lw.get(b)
            if w is not None:
                deps[w.uid] = w
        for b in writes:
            w = self.lw.get(b)
            if w is not None:
                deps[w.uid] = w
            for r in self.lr.get(b, {}).values():
                deps[r.uid] = r
        out = []
        for d in deps.values():
            if d is o:
                continue
            if d.kind == "c" and o.kind == "c" and d.eng == o.eng:
                if o.eng == "pe" or not self.self_sync:
                    continue
            out.append(d)
            d.inc = True
        o.deps = out
        for b in reads:
            key = o.eng if o.kind == "c" else ("d", o.uid)
            self.lr.setdefault(b, {})[key] = o
        for b in writes:
            self.lw[b] = o
            self.lr[b] = {}
        self.ops.append(o)
        if o.kind == "c":
            self.last[o.eng] = o
        else:
            self.pend.append(o)
        return o

    def op(self, eng, fn, reads=(), writes=()):
        o = _Op()
        o.eng, o.fn, o.kind, o.inc, o.tok = eng, fn, "c", False, None
        self.uid += 1
        o.uid = self.uid
        return self._add(o, reads, writes)

    def dma(self, q, fn, reads=(), writes=(), final=False):
        o = _Op()
        o.eng, o.fn, o.kind, o.inc, o.tok = q, fn, "d", True, None
        self.uid += 1
        o.uid = self.uid
        if final:
            self.final.append(o)
        return self._add(o, reads, writes)

    def _wait(self, eng, tok):
        sem, val = tok
        if self.seen[eng].get(id(sem), 0) < val:
            self.E[eng].wait_ge(sem, val)
            self.seen[eng][id(sem)] = val

    def _wait_all(self, eng, toks):
        best = {}
        for sem, val in toks:
            if best.get(id(sem), (None, 0))[1] < val:
                best[id(sem)] = (sem, val)
        for tok in best.values():
            self._wait(eng, tok)

    def barrier(self):
        lasts = [o for o in self.last.values()] + list(self.pend)
        for d in lasts:
            d.inc = True
        for eng in self.E:
            o = _Op()
            o.eng, o.fn, o.kind, o.inc, o.tok = eng, None, "w", False, None
            self.uid += 1
            o.uid = self.uid
            o.deps = [d for d in lasts if not (d.kind == "c" and d.eng == eng)]
            self.ops.append(o)
        self.pend = []
        self.lw = {}
        self.lr = {}

    def emit(self):
        for o in self.ops:
            e = self.E[o.eng]
            self._wait_all(o.eng, [d.tok for d in o.deps])
            if o.kind == "w":
                continue
            if o.kind == "d":
                k = self.dcnt[o.eng]
                self.dcnt[o.eng] += 1
                slot, gen = k % self.NDMA, k // self.NDMA
                sem = self.dsem[o.eng][slot]
                if gen > 0:
                    self._wait(o.eng, (sem, 16 * gen))
                o.fn(e).then_inc(sem, 16)
                o.tok = (sem, 16 * (gen + 1))
            else:
                inst = o.fn(e)
                if o.inc:
                    self.cnt[o.eng] += 1
                    inst.then_inc(self.sem[o.eng], 1)
                    o.tok = (self.sem[o.eng], self.cnt[o.eng])
        for o in self.final:
            self._wait("sp", o.tok)
        self.ops = []


class Vw:
    __slots__ = ("ap", "keys")

    def __init__(self, ap, keys):
        self.ap = ap
        self.keys = keys if isinstance(keys, list) else [keys]

    def k(self, *keys):
        return Vw(self.ap, list(keys))

    def __getitem__(self, idx):
        return Vw(self.ap[idx], self.keys)

    def rearrange(self, *a, **kw):
        return Vw(self.ap.rearrange(*a, **kw), self.keys)

    def bitcast(self, dt):
        return Vw(self.ap.bitcast(dt), self.keys)

    def broadcast_to(self, shape):
        return Vw(self.ap.broadcast_to(list(shape)), self.keys)

    def unsqueeze(self, ax):
        return Vw(self.ap.unsqueeze(ax), self.keys)


class Tl:
    def __init__(self, t, key):
        self.t = t
        self.key = key

    def __getitem__(self, idx):
        return Vw(self.t[idx], self.key)

    @property
    def v(self):
        return Vw(self.t[:], self.key)


def _keys(*vs):
    out = []
    for v in vs:
        if isinstance(v, Vw):
            out.extend(v.keys)
    return out


def _ap(v):
    return v.ap if isinstance(v, Vw) else v


class Ops:
    def __init__(self, S):
        self.S = S

    def mm(self, out, lhsT, rhs, start, stop):
        self.S.op("pe", lambda e: e.matmul(out.ap, lhsT.ap, rhs.ap, start=start, stop=stop, skip_group_check=True),
                  reads=_keys(lhsT, rhs), writes=_keys(out))

    def tr(self, out, in_, ident):
        self.S.op("pe", lambda e: e.transpose(out.ap, in_.ap, ident.ap), reads=_keys(in_, ident), writes=_keys(out))

    def act(self, out, in_, func, bias=None, scale=None, accum=None, eng="act"):
        kw = {}
        if bias is not None:
            kw["bias"] = _ap(bias)
        if scale is not None:
            kw["scale"] = _ap(scale)
        if accum is not None:
            kw["accum_out"] = accum.ap
        self.S.op("act", lambda e: e.activation(out=out.ap, in_=in_.ap, func=func, **kw),
                  reads=_keys(in_, bias, scale), writes=_keys(out, accum))

    def ts(self, eng, out, in0, s1, op0, s2=None, op1=None, accum=None):
        kw = dict(scalar1=_ap(s1), scalar2=_ap(s2) if s2 is not None else None, op0=op0)
        if op1 is not None:
            kw["op1"] = op1
        if accum is not None:
            kw["accum_out"] = accum.ap
        self.S.op(eng, lambda e: e.tensor_scalar(out=out.ap, in0=in0.ap, **kw),
                  reads=_keys(in0, s1, s2), writes=_keys(out, accum))

    def tt(self, eng, out, in0, in1, op):
        self.S.op(eng, lambda e: e.tensor_tensor(out=out.ap, in0=in0.ap, in1=in1.ap, op=op),
                  reads=_keys(in0, in1), writes=_keys(out))

    def stt(self, out, in0, scalar, in1, op0, op1, eng="dve"):
        self.S.op(eng, lambda e: e.scalar_tensor_tensor(out=out.ap, in0=in0.ap, scalar=_ap(scalar), in1=in1.ap,
                                                        op0=op0, op1=op1),
                  reads=_keys(in0, scalar, in1), writes=_keys(out))

    def cp(self, eng, out, in_):
        if eng == "act":
            self.S.op("act", lambda e: e.activation(out=out.ap, in_=in_.ap, func=AF.Copy),
                      reads=_keys(in_), writes=_keys(out))
        else:
            self.S.op(eng, lambda e: e.tensor_copy(out=out.ap, in_=in_.ap), reads=_keys(in_), writes=_keys(out))

    def memset(self, eng, out, val):
        self.S.op(eng, lambda e: e.memset(out.ap, val), writes=_keys(out))

    def recip(self, out, in_):
        self.S.op("dve", lambda e: e.reciprocal(out=out.ap, in_=in_.ap), reads=_keys(in_), writes=_keys(out))

    def scan(self, out, d0, d1, init, op0, op1):
        self.S.op("dve", lambda e: e.tensor_tensor_scan(out=out.ap, data0=d0.ap, data1=d1.ap, initial=_ap(init),
                                                        op0=op0, op1=op1),
                  reads=_keys(d0, d1, init), writes=_keys(out))

    def max8(self, out, in_):
        self.S.op("dve", lambda e: e.max(out=out.ap, in_=in_.ap), reads=_keys(in_), writes=_keys(out))

    def reduce(self, out, in_, op, axis=AX.X):
        self.S.op("dve", lambda e: e.tensor_reduce(out=out.ap, in_=in_.ap, axis=axis, op=op),
                  reads=_keys(in_), writes=_keys(out))

    def dma(self, q, out, in_, final=False):
        o_ap, i_ap = _ap(out), _ap(in_)
        self.S.dma(q, lambda e: e.dma_start(out=o_ap, in_=i_ap), reads=_keys(in_), writes=_keys(out), final=final)


def build(dbg=None):
    nc = bass.Bass("TRN2", target_bir_lowering=False)
    es = contextlib.ExitStack()
    with es:
        _build(nc, es, dbg or set())
    return nc


def _dram_in(nc, name, shape, dt=F32):
    return nc.dram_tensor(name, list(shape), dt, kind="ExternalInput").ap()


GELU_C0 = 1.5957691216057308
GELU_C1 = 1.5957691216057308 * 0.044715


def _build(nc, es, dbg):
    S = Sched(nc, es)
    O = Ops(S)
    x_all = _dram_in(nc, "x_all", [L, D])
    x_own = _dram_in(nc, "x_own", [NOWN * 128, D])
    c_in = _dram_in(nc, "c", [D])
    w_mod = _dram_in(nc, "w_mod", [D, 6 * D])
    b_mod = _dram_in(nc, "b_mod", [6 * D])
    norm1_g = _dram_in(nc, "norm1_g", [D])
    w_in = _dram_in(nc, "w_in", [D, 3524])
    w_glu = _dram_in(nc, "w_ssm_glu", [512, 2048])
    b_glu = _dram_in(nc, "b_ssm_glu", [2048])
    kv_g = _dram_in(nc, "kv_norm_g", [128])
    ik_g = _dram_in(nc, "idx_k_norm_g", [64])
    w_ukT = _dram_in(nc, "w_ukT", [128, 4, 128])
    w_uv = _dram_in(nc, "w_uv", [128, 512])
    w_ap = _dram_in(nc, "w_attn_proj", [512, D])
    w_out = _dram_in(nc, "w_out", [D, D])
    norm2_g = _dram_in(nc, "norm2_g", [D])
    w_fg = _dram_in(nc, "w_ffn_gate", [D, FF])
    w_fu = _dram_in(nc, "w_ffn_up", [D, FF])
    w_fd = _dram_in(nc, "w_ffn_down", [FF, D])
    final_g = _dram_in(nc, "final_g", [D])
    sl_are = _dram_in(nc, "sl_are", [128, 16])
    sl_aim = _dram_in(nc, "sl_aim", [128, 16])
    sl_ldt = _dram_in(nc, "sl_ldt", [128, 16])
    fl_are = _dram_in(nc, "fl_are", [128, 256])
    fl_aim = _dram_in(nc, "fl_aim", [128, 256])
    fl_ldt = _dram_in(nc, "fl_ldt", [128, 256])
    sl_bre = _dram_in(nc, "sl_bre", [128, 16, 16])
    sl_bim = _dram_in(nc, "sl_bim", [128, 16, 16])
    sl_cre = _dram_in(nc, "sl_cre", [128, 16, 16])
    sl_cim = _dram_in(nc, "sl_cim", [128, 16, 16])
    fl_bre = _dram_in(nc, "fl_bre", [128, 256])
    fl_bim = _dram_in(nc, "fl_bim", [128, 256])
    d_dsk = _dram_in(nc, "dsk", [128, 4])
    k_identb = _dram_in(nc, "k_identb", [128, 128], BF16)
    k_identf = _dram_in(nc, "k_identf", [128, 128])
    k_negi8 = _dram_in(nc, "k_negi8", [128, 1024], BF16)
    k_cm2 = _dram_in(nc, "k_cm2", [128, 256])
    k_cm2p = _dram_in(nc, "k_cm2p", [128, 256])
    k_sel = _dram_in(nc, "k_sel", [128, 2])
    k_maskf = _dram_in(nc, "k_maskf", [128, 4])
    k_masks = _dram_in(nc, "k_masks", [128, 2])
    k_pow2 = _dram_in(nc, "k_pow2", [128, NBIS])
    k_jv = _dram_in(nc, "k_jv", [128, 9])
    k_iv = _dram_in(nc, "k_iv", [128, 64])

    out = nc.dram_tensor("out", [NOWN * 128, D], F32, kind="ExternalOutput").ap()
    yg_d = nc.dram_tensor("yg_scratch", [NOWN, 128, 512], BF16, kind="Internal").ap()
    h_d = nc.dram_tensor("h_scratch", [NOWN * 128, D], F32, kind="Internal").ap()
    mod_d = nc.dram_tensor("mod_scratch", [128, 6 * D], F32, kind="Internal").ap()

    def dbg_out(name, shape, dt=F32):
        return nc.dram_tensor("dbg_" + name, list(shape), dt, kind="ExternalOutput").ap()

    def sb(name, shape, dt=F32, stack=es, key=None):
        t = stack.enter_context(nc.sbuf_tensor(name, list(shape), dt))
        return Tl(t, key or name)

    PS = [Tl(es.enter_context(nc.psum_tensor(f"psb{i}", [128, 512], F32)), f"ps{i}") for i in range(8)]

    ncd = nc.allow_non_contiguous_dma(reason="small parameter loads")
    ncd.__enter__()

    identb = sb("identb", [128, 128], BF16)
    identf = sb("identf", [128, 128], F32)
    O.dma("sp", identb.v, k_identb[:, :])
    O.dma("sp", identf.v, k_identf[:, :])
    sel = sb("sel", [128, 2], F32)
    O.dma("sp", sel.v, k_sel[:, :])

    with contextlib.ExitStack() as p0:
        mod = sb("mod", [128, 6 * D], F32, p0)
        SH1, A1, G1, SH2, A2, G2 = [mod[:, i * D:(i + 1) * D].k(("mod", 2 * i), ("mod", 2 * i + 1))
                                    for i in range(6)]
        cT = sb("cT", [128, 8], F32, p0)
        condT = sb("condT", [128, 8], F32, p0)
        crep = sb("crep", [128, 8, 128], F32, p0)
        bmod = sb("bmodbc", [128, 6 * D], F32, p0)
        gbc = sb("gbc", [128, 2, D], F32, p0)
        wm = [sb(f"wm{i}", [128, 8, 512], F32, p0) for i in range(2)]
        O.dma("sp", cT.v, c_in.rearrange("(kt p) -> p kt", p=128))
        O.dma("sp", bmod.v, b_mod.partition_broadcast(128))
        O.dma("sp", gbc[:, 0, :].k("gbc0"), norm1_g.partition_broadcast(128))
        O.dma("sp", gbc[:, 1, :].k("gbc1"), norm2_g.partition_broadcast(128))
        O.act(condT.v, cT.v, AF.Silu)
        O.cp("dve", crep.v, condT.v.unsqueeze(2).broadcast_to([128, 8, 128]))
        for n in range(12):
            wt = wm[n % 2]
            O.dma("sp", wt.v, w_mod[:, n * 512:(n + 1) * 512].rearrange("(kt p) c -> p kt c", p=128))
            bank = PS[n % 2]
            for kt in range(8):
                O.mm(bank.v, crep[:, kt, :], wt[:, kt, :], kt == 0, kt == 7)
            O.tt("dve", mod[:, n * 512:(n + 1) * 512].k(("mod", n)), bank.v, bmod[:, n * 512:(n + 1) * 512], ALU.add)
        for which, Av in ((0, A1), (1, A2)):
            O.stt(Av, Av, 1.0, gbc[:, which, :].k(f"gbc{which}"), ALU.add, ALU.mult)
        O.dma("sp", mod_d[:, :], mod.v.k(*[("mod", n) for n in range(12)]))
        if "mod" in dbg:
            O.dma("sp", dbg_out("mod", [128, 6 * D])[:, :], mod.v.k(*[("mod", n) for n in range(12)]), final=True)
        S.barrier()
        S.emit()

    def norm_block(xsrc, A, SHv, uT_dst, W, idx):
        xt = W["x"][idx % W["nbuf"]]
        O.dma("sp", xt.v, xsrc)
        ss = W["ss"][:, idx % 2:idx % 2 + 1].k(("ss", idx % 2))
        O.act(W["junk"].v, xt.v, AF.Square, accum=ss)
        var = W["var"][:, idx % 2:idx % 2 + 1].k(("var", idx % 2))
        O.ts("dve", var, ss, 1.0 / D, ALU.mult, EPS, ALU.add)
        O.act(var, var, AF.Sqrt)
        rstd = W["rstd"][:, idx % 2:idx % 2 + 1].k(("rstd", idx % 2))
        O.recip(rstd, var)
        O.stt(W["t1"].v, xt.v, rstd, A, ALU.mult, ALU.mult)
        ub = W["ub"][idx % W["nbuf"]]
        O.tt("pool", ub.v, W["t1"].v, SHv, ALU.add)
        pst = PS[0].v.bitcast(BF16)
        for kt in range(8):
            O.tr(pst[:, kt * 128:(kt + 1) * 128], ub[:, kt * 128:(kt + 1) * 128], identb.v)
        O.cp("act", uT_dst, pst.rearrange("p (k t) -> p k t", k=8))

    def load_mod(stack, pfx, idxs):
        outv = []
        for i in idxs:
            t = sb(f"{pfx}mod{i}", [128, D], F32, stack)
            O.dma("sp", t.v, mod_d[:, i * D:(i + 1) * D])
            outv.append(t.v)
        return outv

    def norm_work(stack, pfx, nbuf=2):
        return dict(
            nbuf=nbuf,
            x=[sb(f"{pfx}x{i}", [128, D], F32, stack) for i in range(nbuf)],
            ss=sb(f"{pfx}ss", [128, 2], F32, stack), var=sb(f"{pfx}var", [128, 2], F32, stack),
            rstd=sb(f"{pfx}rstd", [128, 2], F32, stack),
            junk=sb(f"{pfx}junk", [128, D], BF16, stack), t1=sb(f"{pfx}t1", [128, D], F32, stack),
            ub=[sb(f"{pfx}ub{i}", [128, D], BF16, stack) for i in range(nbuf)],
        )

    ckv_d = nc.dram_tensor("ckv_scratch", [128, NB, 129], BF16, kind="Internal").ap()
    ckvT_d = nc.dram_tensor("ckvT_scratch", [128, L], BF16, kind="Internal").ap()
    kiT_d = nc.dram_tensor("kiT_scratch", [128, L], BF16, kind="Internal").ap()

    with contextlib.ExitStack() as pA:
        W = norm_work(pA, "a_")
        SH1, A1 = load_mod(pA, "a_", [0, 1])
        wsh = sb("wsh", [128, 8, 704], BF16, pA)
        for (c0, c1, o0) in ((OFF["xs"], OFF["xs"] + 512, 0), (OFF["ckv"], OFF["ckv"] + 128, 512),
                             (OFF["ki"], OFF["ki"] + 64, 640)):
            O.dma("pool", wsh[:, :, o0:o0 + (c1 - c0)], w_in[:, c0:c1].rearrange("(kt p) c -> p kt c", p=128))
        gkv = sb("gkv", [128, 128], F32, pA)
        gik = sb("gik", [128, 64], F32, pA)
        O.dma("sp", gkv.v, kv_g.partition_broadcast(128))
        O.dma("sp", gik.v, ik_g.partition_broadcast(128))
        cks = sb("a_cks", [128, 4, 129], BF16, pA)
        ckTs = sb("a_ckTs", [128, 512], BF16, pA)
        kiTs = sb("a_kiTs", [128, 512], BF16, pA)
        O.memset("pool", cks[:, :, 128:129].k("ckv_ones"), 1.0)
        uT = [sb(f"a_uT{i}", [128, 8, 512], BF16, pA) for i in range(1)]
        ssk = sb("a_ssk", [128, 2], F32, pA)
        rsk = sb("a_rsk", [128, 2], F32, pA)
        junk2 = sb("a_junk2", [128, 128], BF16, pA)
        kin2 = sb("a_kin2", [128, 2, 64], BF16, pA)

        S5 = _s5_prepare(nc, S, O, sb, pA, PS, identf, locals())
        xsT = [sb(f"a_xsT{i}", [128, 4, 8, 64], BF16, pA) for i in range(2)]
        BR = sb("a_BR", [128, 16, 64], F32, pA)
        BI = sb("a_BI", [128, 16, 64], F32, pA)
        T1 = sb("a_T1", [128, 8, 64], F32, pA)
        T2 = sb("a_T2", [128, 8, 64], F32, pA)
        STr = sb("a_STr", [128, 16, 64], F32, pA)
        STi = sb("a_STi", [128, 16, 64], F32, pA)
        inj = sb("a_inj", [128, 2, 16], F32, pA)
        itmp = sb("a_itmp", [128, 2, 16], F32, pA)
        Sbf = [[sb(f"a_Sbf{i}{c}", [128, 16, 65], BF16, pA) for c in range(2)] for i in range(2)]
        gx2 = sb("a_gx2", [128, 512], F32, pA)
        ygs = sb("a_ygs", [128, 4, 512], BF16, pA)
        ygt = sb("a_ygt", [128, 4, 128], BF16, pA)
        ygo = [sb(f"a_ygo{i}", [128, 4, 128], BF16, pA) for i in range(2)]
        O.memset("pool", Sbf[1][0][:, :, 64:65], 0.0)
        O.memset("pool", Sbf[1][1][:, :, 64:65], 0.0)

        ygdbg = dbg_out("yg", [NOWN, 128, 512], BF16) if "s5" in dbg else None
        for sbi in range(0 if DEV_SKIP_A else NB // 4):
            u = uT[0]
            for bl in range(4):
                blk = 4 * sbi + bl
                norm_block(x_all[blk * 128:(blk + 1) * 128, :], A1, SH1,
                           u[:, :, bl * 128:(bl + 1) * 128].k((u.key, bl)), W, blk)
            ukeys = [(u.key, bl) for bl in range(4)]
            xs = xsT[sbi % 2]
            for c4 in range(4):
                bank = PS[1 + c4 % 2]
                for kt in range(8):
                    O.mm(bank.v, wsh[:, kt, c4 * 128:(c4 + 1) * 128], u[:, kt, :].k(*ukeys), kt == 0, kt == 7)
                O.cp("act", xs[:, c4, :, :].k((xs.key, c4)), bank.v.rearrange("p (m r) -> p r m", r=8))
            for bl in range(4):
                blk = 4 * sbi + bl
                bank = PS[3]
                for kt in range(8):
                    O.mm(bank[:, 0:192], u[:, kt, bl * 128:(bl + 1) * 128].k((u.key, bl)), wsh[:, kt, 512:704],
                         kt == 0, kt == 7)
                O.act(junk2.v, bank[:, 0:128], AF.Square, accum=ssk[:, 0:1].k("ssk0"))
                O.act(junk2[:, 0:64], bank[:, 128:192], AF.Square, accum=ssk[:, 1:2].k("ssk1"))
                O.ts("dve", rsk[:, 0:1].k("rsk0"), ssk[:, 0:1].k("ssk0"), 1.0 / 128, ALU.mult, EPS, ALU.add)
                O.ts("dve", rsk[:, 1:2].k("rsk1"), ssk[:, 1:2].k("ssk1"), 1.0 / 64, ALU.mult, EPS, ALU.add)
                O.act(rsk.v.k("rsk0", "rsk1"), rsk.v.k("rsk0", "rsk1"), AF.Sqrt)
                O.recip(rsk.v.k("rsk0", "rsk1"), rsk.v.k("rsk0", "rsk1"))
                ckb = cks[:, bl, 0:128].k(("cks", bl))
                O.stt(ckb, bank[:, 0:128], rsk[:, 0:1].k("rsk0"), gkv.v, ALU.mult, ALU.mult)
                for dup in range(2):
                    O.stt(kin2[:, dup, :], bank[:, 128:192], rsk[:, 1:2].k("rsk1"), gik.v, ALU.mult, ALU.mult)
                pst = PS[4].v.bitcast(BF16)
                O.tr(pst[:, 0:128], ckb, identb.v)
                O.tr(pst[:, 128:256], kin2.v.rearrange("p a b -> p (a b)"), identb.v)
                O.cp("dve", ckTs[:, bl * 128:(bl + 1) * 128].k(("ckTs", bl)), pst[:, 0:128])
                O.cp("dve", kiTs[:, bl * 128:(bl + 1) * 128].k(("kiTs", bl)), pst[:, 128:256])
            O.dma("sp", ckv_d[:, 4 * sbi:4 * sbi + 4, :], cks.v.k("ckv_ones", *[("cks", bl) for bl in range(4)]))
            O.dma("sp", ckvT_d[:, sbi * 512:(sbi + 1) * 512], ckTs.v.k(*[("ckTs", bl) for bl in range(4)]))
            O.dma("sp", kiT_d[:, sbi * 512:(sbi + 1) * 512], kiTs.v.k(*[("kiTs", bl) for bl in range(4)]))
            _s5_superblock(S, O, PS, S5, sbi, xs, BR, BI, T1, T2, STr, STi, inj, itmp, Sbf,
                           gx2, ygs, ygt, ygo, sel, yg_d, ygdbg)
        if "ckv" in dbg:
            S.barrier()
            dk = dbg_out("ckvT", [128, L], BF16)
            S.dma("sp", lambda e: e.dma_start(out=dk[:, :], in_=ckvT_d[:, :]), final=True)
            dk2 = dbg_out("kiT2", [128, L], BF16)
            S.dma("sp", lambda e: e.dma_start(out=dk2[:, :], in_=kiT_d[:, :]), final=True)
            dk3 = dbg_out("ckv_sb", [128, NB, 129], BF16)
            S.dma("sp", lambda e: e.dma_start(out=dk3[:, :, :], in_=ckv_d[:, :, :]), final=True)
        if "s5" in dbg:
            for nm in S5["dbg"]:
                t = S5["dbg"][nm]
                shp = list(t.t.shape)
                O.dma("sp", dbg_out(nm, [128, int(np.prod(shp[1:]))], t.t.dtype)[:, :],
                      t.v.rearrange("p a b c -> p (a b c)") if len(shp) == 4 else
                      (t.v.rearrange("p a b -> p (a b)") if len(shp) == 3 else t.v), final=True)
        S.barrier()
        S.emit()
    if "stopA" in dbg:
        ncd.__exit__(None, None, None)
        return

    _phase_b1_b2_c(nc, S, O, sb, PS, dbg, dbg_out, norm_block, norm_work, load_mod, locals())
    ncd.__exit__(None, None, None)
def _phasor(O, cyc, outc, outs, tmps):
    ri, rf, s1, q = tmps
    O.cp("dve", ri, cyc)
    O.cp("dve", rf, ri)
    O.tt("dve", rf, cyc, rf, ALU.subtract)
    O.act(s1, rf, AF.Sin, scale=math.pi)
    O.act(q, rf, AF.Sin, scale=math.pi / 2)
    O.tt("dve", q, q, q, ALU.mult)
    O.ts("dve", q, q, -2.0, ALU.mult, 1.0, ALU.add)
    O.stt(outs, s1, 2.0, q, ALU.mult, ALU.mult)
    O.tt("dve", s1, s1, s1, ALU.mult)
    O.ts("dve", outc, s1, -2.0, ALU.mult, 1.0, ALU.add)


def _s5_prepare(nc, S, O, sb, st, PS, identf, g):
    R = {}
    KT = sb("s5_KT", [128, 8, 4, 128], BF16, st)
    Fm = [sb(f"s5_Fm{c}", [128, 8, 4, 2, 2, 64], BF16, st) for c in range(2)]
    Em = [sb(f"s5_Em{c}", [128, 8, 8, 2, 2, 2, 16], BF16, st) for c in range(2)]
    Dc = sb("s5_Dc", [128, 16, 64], F32, st)
    Ds = sb("s5_Ds", [128, 16, 64], F32, st)
    rho = sb("s5_rho", [128, 16, 64], F32, st)
    Lam = sb("s5_Lam", [128, 2, 16], F32, st)
    R.update(KT=KT, Fm=Fm, Em=Em, Dc=Dc, Ds=Ds, rho=rho, Lam=Lam)
    R["dbg"] = dict(s5_KT=KT, s5_Fm0=Fm[0], s5_Fm1=Fm[1], s5_Em0=Em[0], s5_Em1=Em[1], s5_Dc=Dc, s5_Ds=Ds,
                    s5_rho=rho, s5_Lam=Lam)
    holder = {}

    def ld(name, src, shape):
        t = sb("s5t_" + name, shape, F32, holder["tp"])
        O.dma("sp", t.v, src)
        return t

    def tmp(name, shape, dt=F32):
        return sb("s5t_" + name, shape, dt, holder["tp"])

    with contextlib.ExitStack() as tp:
        holder["tp"] = tp

        masks = ld("masks", g["k_masks"][:, :], [128, 2])
        jv = ld("jv", g["k_jv"][:, :], [128, 9])
        iv = ld("iv", g["k_iv"][:, :], [128, 64])
        dsk = ld("dsk", g["d_dsk"][:, :], [128, 4])

        def lam_common(pfx, are_d, aim_d, ldt_d, Wd):
            are = ld(pfx + "are", are_d[:, :], [128, Wd])
            aim = ld(pfx + "aim", aim_d[:, :], [128, Wd])
            dt = ld(pfx + "ldt", ldt_d[:, :], [128, Wd])
            O.act(dt.v, dt.v, AF.Exp)
            x1 = tmp(pfx + "x1", [128, Wd])
            angc = tmp(pfx + "angc", [128, Wd])
            O.tt("dve", x1.v, are.v, dt.v, ALU.mult)
            O.tt("dve", angc.v, aim.v, dt.v, ALU.mult)
            O.ts("dve", angc.v, angc.v, 1.0 / (2 * math.pi), ALU.mult)
            ph = (tmp(pfx + "ri", [128, Wd], I32).v, tmp(pfx + "rf", [128, Wd]).v, tmp(pfx + "s1", [128, Wd]).v,
                  tmp(pfx + "q", [128, Wd]).v)
            rj = tmp(pfx + "rj", [128, Wd])
            uc = tmp(pfx + "uc", [128, Wd])
            us = tmp(pfx + "us", [128, Wd])
            mg = tmp(pfx + "mg", [128, Wd])

            def lam_pow(j, lr, li):
                O.ts("dve", rj.v, angc.v, float(j), ALU.mult)
                _phasor(O, rj.v, uc.v, us.v, ph)
                O.act(mg.v, x1.v, AF.Exp, scale=float(j))
                O.tt("dve", lr, mg.v, uc.v, ALU.mult)
                O.tt("dve", li, mg.v, us.v, ALU.mult)

            l1r = tmp(pfx + "l1r", [128, Wd])
            l1i = tmp(pfx + "l1i", [128, Wd])
            lam_pow(1, l1r.v, l1i.v)
            den = tmp(pfx + "den", [128, Wd])
            t0 = tmp(pfx + "t0", [128, Wd])
            cre = tmp(pfx + "cfr", [128, Wd])
            cim = tmp(pfx + "cfi", [128, Wd])
            O.tt("dve", den.v, are.v, are.v, ALU.mult)
            O.tt("dve", t0.v, aim.v, aim.v, ALU.mult)
            O.tt("dve", den.v, den.v, t0.v, ALU.add)
            O.recip(den.v, den.v)
            O.ts("dve", l1r.v, l1r.v, -1.0, ALU.add)
            O.tt("dve", cre.v, l1r.v, are.v, ALU.mult)
            O.tt("dve", t0.v, l1i.v, aim.v, ALU.mult)
            O.tt("dve", cre.v, cre.v, t0.v, ALU.add)
            O.tt("dve", cre.v, cre.v, den.v, ALU.mult)
            O.tt("dve", cim.v, l1i.v, are.v, ALU.mult)
            O.tt("dve", t0.v, l1r.v, aim.v, ALU.mult)
            O.tt("dve", cim.v, cim.v, t0.v, ALU.subtract)
            O.tt("dve", cim.v, cim.v, den.v, ALU.mult)
            return lam_pow, cre, cim, x1, angc, ph

        lam_pow, cre, cim, x1, angc, ph = lam_common("sl_", g["sl_are"], g["sl_aim"], g["sl_ldt"], 16)
        Bre = ld("sl_bre", g["sl_bre"][:, :, :], [128, 16, 16])
        Bim = ld("sl_bim", g["sl_bim"][:, :, :], [128, 16, 16])
        Cre = ld("sl_cre", g["sl_cre"][:, :, :], [128, 16, 16])
        Cim = ld("sl_cim", g["sl_cim"][:, :, :], [128, 16, 16])
        bc = lambda t: t.v.unsqueeze(2).broadcast_to([128, 16, 16])
        bcv = lambda v: v.unsqueeze(2).broadcast_to([128, 16, 16])
        Bbr = tmp("sl_Bbr", [128, 16, 16])
        Bbi = tmp("sl_Bbi", [128, 16, 16])
        ta = tmp("sl_ta", [128, 16, 16])
        tb = tmp("sl_tb", [128, 16, 16])
        O.tt("dve", Bbr.v, Bre.v, bc(cre), ALU.mult)
        O.tt("dve", ta.v, Bim.v, bc(cim), ALU.mult)
        O.tt("dve", Bbr.v, Bbr.v, ta.v, ALU.subtract)
        O.tt("dve", Bbi.v, Bim.v, bc(cre), ALU.mult)
        O.tt("dve", ta.v, Bre.v, bc(cim), ALU.mult)
        O.tt("dve", Bbi.v, Bbi.v, ta.v, ALU.add)
        CM = [tmp("sl_CMr", [128, 8, 2, 2, 2, 16], BF16), tmp("sl_CMi", [128, 8, 2, 2, 2, 16], BF16)]
        GM = [tmp("sl_GMr", [128, 8, 2, 2, 2, 16], BF16), tmp("sl_GMi", [128, 8, 2, 2, 2, 16], BF16)]
        for tl in CM + GM + Em:
            O.memset("pool", tl.v, 0.0)
        gs = lambda t, s: t.v.rearrange("p (gq s) c -> p gq s c", s=2)[:, :, s, :]
        for s in range(2):
            for g2 in range(2):
                O.ts("dve", CM[0][:, :, s, s, g2, :], gs(Cre, s), masks[:, g2:g2 + 1], ALU.mult)
                O.ts("dve", CM[1][:, :, s, s, g2, :], gs(Cim, s), masks[:, g2:g2 + 1], ALU.mult, -1.0, ALU.mult)
        ljr = tmp("sl_ljr", [128, 16])
        lji = tmp("sl_lji", [128, 16])
        O.memset("pool", KT.v, 0.0)
        for j in range(9):
            lam_pow(j, ljr.v, lji.v)
            if j < 8:
                O.tt("dve", ta.v, Bbr.v, bcv(ljr.v), ALU.mult)
                O.tt("dve", tb.v, Bbi.v, bcv(lji.v), ALU.mult)
                O.tt("dve", ta.v, ta.v, tb.v, ALU.subtract)
                for s in range(2):
                    for g2 in range(2):
                        O.ts("dve", GM[0][:, :, s, s, g2, :], gs(ta, s), masks[:, g2:g2 + 1], ALU.mult)
                O.tt("dve", ta.v, Bbi.v, bcv(ljr.v), ALU.mult)
                O.tt("dve", tb.v, Bbr.v, bcv(lji.v), ALU.mult)
                O.tt("dve", ta.v, ta.v, tb.v, ALU.add)
                for s in range(2):
                    for g2 in range(2):
                        O.ts("dve", GM[1][:, :, s, s, g2, :], gs(ta, s), masks[:, g2:g2 + 1], ALU.mult)
                bank = PS[4 + j // 2]
                f64 = lambda v: v.rearrange("p a b c -> p (a b c)")
                for gh in range(16):
                    c4, pair = gh // 4, gh % 4
                    q = pair // 2
                    col = ((j % 2) * 4 + c4) * 64
                    o = bank[64 * q:64 * q + 64, col:col + 64]
                    O.mm(o, f64(GM[0][:, gh // 2, gh % 2, :, :, :]), f64(CM[0][:, gh // 2, gh % 2, :, :, :]),
                         pair % 2 == 0, False)
                    O.mm(o, f64(GM[1][:, gh // 2, gh % 2, :, :, :]), f64(CM[1][:, gh // 2, gh % 2, :, :, :]),
                         False, pair % 2 == 1)
                if j % 2 == 1:
                    jh = j // 2
                    for q in range(2):
                        O.cp("dve", KT[64 * q:64 * q + 64, 2 * jh:2 * jh + 2, :, 64 * q:64 * q + 64],
                             bank[64 * q:64 * q + 64, :].rearrange("p (j c k) -> p j c k", j=2, c=4))
            if j >= 1:
                r = j - 1
                O.tt("dve", ta.v, Cre.v, bcv(ljr.v), ALU.mult)
                O.tt("dve", tb.v, Cim.v, bcv(lji.v), ALU.mult)
                O.tt("dve", ta.v, ta.v, tb.v, ALU.subtract)
                for s in range(2):
                    for g2 in range(2):
                        O.ts("dve", Em[0][:, r, :, s, s, g2, :], gs(ta, s), masks[:, g2:g2 + 1], ALU.mult)
                O.tt("dve", ta.v, Cre.v, bcv(lji.v), ALU.mult)
                O.tt("dve", tb.v, Cim.v, bcv(ljr.v), ALU.mult)
                O.tt("dve", ta.v, ta.v, tb.v, ALU.add)
                for s in range(2):
                    for g2 in range(2):
                        O.ts("dve", Em[1][:, r, :, s, s, g2, :], gs(ta, s), masks[:, g2:g2 + 1], ALU.mult, -1.0,
                             ALU.mult)
            if j == 8:
                O.cp("dve", Lam[:, 0, :], ljr.v)
                O.cp("dve", Lam[:, 1, :], lji.v)
        for c4 in range(4):
            O.stt(KT[:, 0, c4, :], identf.v, dsk[:, c4:c4 + 1], KT[:, 0, c4, :], ALU.mult, ALU.add)
        r8 = tmp("sl_r8", [128, 16])
        O.ts("dve", r8.v, angc.v, 8.0, ALU.mult)
        O.cp("dve", ph[0], r8.v)
        O.cp("dve", ph[1], ph[0])
        O.tt("dve", r8.v, r8.v, ph[1], ALU.subtract)
        RT = tmp("sl_RT", [128, 16, 64])
        O.tt("dve", RT.v, r8.v.unsqueeze(2).broadcast_to([128, 16, 64]),
             iv.v.unsqueeze(1).broadcast_to([128, 16, 64]), ALU.mult)
        ph2 = (tmp("sl_ri2", [128, 512], I32).v, tmp("sl_rf2", [128, 512]).v, tmp("sl_s12", [128, 512]).v,
               tmp("sl_q2", [128, 512]).v)
        fl = lambda t, h: t[:, 8 * h:8 * h + 8, :].rearrange("p a b -> p (a b)")
        for h in range(2):
            _phasor(O, fl(RT, h), fl(Dc, h), fl(Ds, h), ph2)
        rh = tmp("sl_rh", [128, 16])
        O.act(rh.v, x1.v, AF.Exp, scale=8.0)
        O.cp("dve", rho.v, rh.v.unsqueeze(2).broadcast_to([128, 16, 64]))
        O.memset("dve", rho[:, :, 0:1], 0.0)

        S.barrier()
        S.emit()
    with contextlib.ExitStack() as tp:
        holder["tp"] = tp
        maskf = ld("maskf", g["k_maskf"][:, :], [128, 4])
        lam_pow, cre, cim, x1, angc, ph = lam_common("fl_", g["fl_are"], g["fl_aim"], g["fl_ldt"], 256)
        Bre = ld("fl_bre", g["fl_bre"][:, :], [128, 256])
        Bim = ld("fl_bim", g["fl_bim"][:, :], [128, 256])
        Bbr = tmp("fl_Bbr", [128, 256])
        Bbi = tmp("fl_Bbi", [128, 256])
        ta = tmp("fl_ta", [128, 256])
        tb = tmp("fl_tb", [128, 256])
        O.tt("dve", Bbr.v, Bre.v, cre.v, ALU.mult)
        O.tt("dve", ta.v, Bim.v, cim.v, ALU.mult)
        O.tt("dve", Bbr.v, Bbr.v, ta.v, ALU.subtract)
        O.tt("dve", Bbi.v, Bim.v, cre.v, ALU.mult)
        O.tt("dve", ta.v, Bre.v, cim.v, ALU.mult)
        O.tt("dve", Bbi.v, Bbi.v, ta.v, ALU.add)
        ljr = tmp("fl_ljr", [128, 256])
        lji = tmp("fl_lji", [128, 256])
        v4 = lambda t: t.v.rearrange("p (c n) -> p c n", c=4)
        for j in range(8):
            lam_pow(j, ljr.v, lji.v)
            O.tt("dve", ta.v, Bbr.v, ljr.v, ALU.mult)
            O.tt("dve", tb.v, Bbi.v, lji.v, ALU.mult)
            O.tt("dve", ta.v, ta.v, tb.v, ALU.subtract)
            for wg in range(4):
                O.ts("dve", Fm[0][:, j, :, wg // 2, wg % 2, :], v4(ta), maskf[:, wg:wg + 1], ALU.mult)
            O.tt("dve", ta.v, Bbi.v, ljr.v, ALU.mult)
            O.tt("dve", tb.v, Bbr.v, lji.v, ALU.mult)
            O.tt("dve", ta.v, ta.v, tb.v, ALU.add)
            for wg in range(4):
                O.ts("dve", Fm[1][:, j, :, wg // 2, wg % 2, :], v4(ta), maskf[:, wg:wg + 1], ALU.mult)
        S.barrier()
        S.emit()
    return R


def _s5_superblock(S, O, PS, S5, sbi, xs, BR, BI, T1, T2, STr, STi, inj, itmp, Sbf,
                   gx2, ygs, ygt, ygo, sel, yg_d, ygdbg=None):
    KT, Fm, Em, Dc, Ds, rho, Lam = S5["KT"], S5["Fm"], S5["Em"], S5["Dc"], S5["Ds"], S5["rho"], S5["Lam"]
    cur, prv = Sbf[sbi % 2], Sbf[(sbi + 1) % 2]
    xkeys = [(xs.key, c4) for c4 in range(4)]
    for half in range(2):
        for ghl in range(8):
            gh = half * 8 + ghl
            c4, pair = gh // 4, gh % 4
            for c in range(2):
                o = PS[5 + c][:, ghl * 64:(ghl + 1) * 64]
                q = pair // 2
                for k in range(8):
                    O.mm(o, Fm[c][64 * q:64 * q + 64, 7 - k, c4, pair % 2, :, :].rearrange("p a b -> p (a b)"),
                         xs[64 * q:64 * q + 64, c4, k, :].k((xs.key, c4)), k == 0, k == 7)
        hs = slice(half * 8, half * 8 + 8)
        pr = PS[5].v.rearrange("p (a b) -> p a b", a=8)
        pi = PS[6].v.rearrange("p (a b) -> p a b", a=8)
        O.tt("dve", T1.v, pr, Dc[:, hs, :], ALU.mult)
        O.tt("dve", T2.v, pi, Ds[:, hs, :], ALU.mult)
        O.tt("pool", BR[:, hs, :].k(("BR", half)), T1.v, T2.v, ALU.add)
        O.tt("dve", T1.v, pi, Dc[:, hs, :], ALU.mult)
        O.tt("dve", T2.v, pr, Ds[:, hs, :], ALU.mult)
        O.tt("pool", BI[:, hs, :].k(("BI", half)), T1.v, T2.v, ALU.subtract)
    BRk = BR.v.k(("BR", 0), ("BR", 1))
    BIk = BI.v.k(("BI", 0), ("BI", 1))
    if sbi > 0:
        O.tt("pool", BRk[:, :, 0], BRk[:, :, 0], inj[:, 0, :], ALU.add)
        O.tt("pool", BIk[:, :, 0], BIk[:, :, 0], inj[:, 1, :], ALU.add)
    f2 = lambda v: v.rearrange("p a b -> p (a b)")
    O.scan(f2(STr.v), f2(rho.v), f2(BRk), 0.0, ALU.mult, ALU.add)
    O.scan(f2(STi.v), f2(rho.v), f2(BIk), 0.0, ALU.mult, ALU.add)
    O.tt("dve", BRk, STr.v, Dc.v, ALU.mult)
    O.tt("pool", BIk, STi.v, Ds.v, ALU.mult)
    O.tt("dve", BRk, BRk, BIk, ALU.subtract)
    O.tt("pool", BIk, STr.v, Ds.v, ALU.mult)
    O.tt("dve", STi.v, STi.v, Dc.v, ALU.mult)
    O.tt("pool", BIk, BIk, STi.v, ALU.add)
    SRr, SRi = BRk, BIk
    O.tt("pool", inj[:, 0, :], SRr[:, :, 63], Lam[:, 0, :], ALU.mult)
    O.tt("pool", itmp[:, 0, :], SRi[:, :, 63], Lam[:, 1, :], ALU.mult)
    O.tt("pool", inj[:, 0, :], inj[:, 0, :], itmp[:, 0, :], ALU.subtract)
    O.tt("pool", inj[:, 1, :], SRr[:, :, 63], Lam[:, 1, :], ALU.mult)
    O.tt("pool", itmp[:, 1, :], SRi[:, :, 63], Lam[:, 0, :], ALU.mult)
    O.tt("pool", inj[:, 1, :], inj[:, 1, :], itmp[:, 1, :], ALU.add)
    for c, SR in ((0, SRr), (1, SRi)):
        O.cp("pool", cur[c][:, :, 0:1], prv[c][:, :, 64:65])
        O.cp("act", cur[c][:, :, 1:65], SR)
    for c4 in range(4):
        bank = PS[7]
        for r in range(8):
            o = bank[:, r * 64:(r + 1) * 64]
            for j in range(r + 1):
                O.mm(o, KT[:, j, c4, :], xs[:, c4, r - j, :].k((xs.key, c4)), j == 0, False)
            for pair in range(4):
                gh = c4 * 4 + pair
                q = pair // 2
                o2 = bank[64 * q:64 * q + 64, r * 64:(r + 1) * 64]
                O.mm(o2, Em[0][:, r, gh // 2, gh % 2, :, :, :].rearrange("p a b c -> p (a b c)"),
                     cur[0][:, gh, 0:64], False, False)
                O.mm(o2, Em[1][:, r, gh // 2, gh % 2, :, :, :].rearrange("p a b c -> p (a b c)"),
                     cur[1][:, gh, 0:64], False, pair == 3)
        O.act(gx2.v, bank.v, AF.Square)
        O.ts("dve", gx2.v, gx2.v, GELU_C1, ALU.mult, GELU_C0, ALU.add)
        O.tt("dve", gx2.v, gx2.v, bank.v, ALU.mult)
        O.act(gx2.v, gx2.v, AF.Sigmoid)
        O.tt("dve", ygs[:, c4, :].k((ygs.key, c4)).rearrange("p (m r) -> p r m", r=8),
             gx2.v.rearrange("p (r m) -> p r m", r=8), bank.v.rearrange("p (r m) -> p r m", r=8), ALU.mult)
    ygk = ygs.v.k(*[(ygs.key, c4) for c4 in range(4)])
    for i2 in range(2):
        i = 2 * sbi + i2
        a0, b0 = (2 * i2) * 128, (2 * i2 + 1) * 128
        yo = ygo[i2]
        O.ts("pool", ygt.v, ygk[:, :, b0:b0 + 128], sel[:, 1:2], ALU.mult)
        O.stt(yo.v, ygk[:, :, a0:a0 + 128], sel[:, 0:1], ygt.v, ALU.mult, ALU.add)
        O.dma("sp", yg_d[i].rearrange("p (c t) -> p c t", c=4), yo.v)
        if ygdbg is not None:
            O.dma("sp", ygdbg[i].rearrange("p (c t) -> p c t", c=4), yo.v, final=True)
def _phase_b1_b2_c(nc, S, O, sb, PS, dbg, dbg_out, norm_block, norm_work, load_mod, g):
    x_own, w_in, out = g["x_own"], g["w_in"], g["out"]
    identb, identf, sel = g["identb"], g["identf"], g["sel"]
    ckv_d, ckvT_d, kiT_d, yg_d, h_d = g["ckv_d"], g["ckvT_d"], g["kiT_d"], g["yg_d"], g["h_d"]
    att_d = nc.dram_tensor("att_scratch", [NOWN, 128, 512], BF16, kind="Internal").ap()
    nblk = DEV_NBLK or NOWN
    wview = lambda w, c0, c1: w[:, c0:c1].rearrange("(kt p) c -> p kt c", p=128)

    with contextlib.ExitStack() as pB:
        ckv_sb = sb("ckv_sb", [128, NB, 129], BF16, pB)
        ckvT = sb("ckvT", [128, L], BF16, pB)
        kiT2 = sb("kiT2", [128, L], BF16, pB)
        O.dma("sp", ckv_sb.v, ckv_d[:, :, :])
        O.dma("sp", ckvT.v, ckvT_d[:, :])
        O.dma("sp", kiT2.v, kiT_d[:, :])
        W = norm_work(pB, "b_")
        SH1, A1 = load_mod(pB, "b_", [0, 1])
        wq = sb("b_wq", [128, 8, 512], BF16, pB)
        wqi = sb("b_wqi", [128, 8, 260], BF16, pB)
        O.dma("pool", wq.v, wview(w_in, OFF["q"], OFF["q"] + 512))
        O.dma("pool", wqi[:, :, 0:256], wview(w_in, OFF["qi"], OFF["qi"] + 256))
        O.dma("pool", wqi[:, :, 256:260], wview(w_in, OFF["wi"], OFF["wi"] + 4))
        wukT = sb("b_wukT", [128, 4, 128], BF16, pB)
        wuv = sb("b_wuv", [128, 512], BF16, pB)
        O.dma("pool", wukT.v, g["w_ukT"][:, :, :])
        O.dma("pool", wuv.v, g["w_uv"][:, :])
        negi8 = sb("b_negi8", [128, 1024], BF16, pB)
        cm2 = sb("b_cm2", [128, 256], F32, pB)
        cm2p = sb("b_cm2p", [128, 256], F32, pB)
        pow2 = sb("b_pow2", [128, NBIS], F32, pB)
        O.dma("sp", negi8.v, g["k_negi8"][:, :])
        O.dma("sp", cm2.v, g["k_cm2"][:, :])
        O.dma("sp", cm2p.v, g["k_cm2p"][:, :])
        O.dma("sp", pow2.v, g["k_pow2"][:, :])
        uT1 = sb("b_uT1", [128, 8, 128], BF16, pB)
        qT = sb("b_qT", [128, 4, 128], BF16, pB)
        qlatT = sb("b_qlatT", [128, 1024], BF16, pB)
        absw = sb("b_absw", [128, 4], F32, pB)
        sgn = sb("b_sgn", [128, 4], F32, pB)
        qis = sb("b_qis", [128, 4, 64], BF16, pB)
        qiT = sb("b_qiT", [128, 2, 128], BF16, pB)
        Dg = sb("b_Dg", [128, 4, 128], BF16, pB)
        Rsb = [sb(f"b_R{i}", [128, 4, 512], BF16, pB) for i in range(2)]
        Ibuf = sb("b_Ibuf", [128, L], F32, pB)
        nm = sb("b_nm", [128, L], BF16, pB)
        t256 = sb("b_t256", [128, 256], F32, pB)
        mx8 = sb("b_mx8", [128, 8], F32, pB)
        bs = sb("b_bs", [128, 8], F32, pB)
        WK = sb("b_WK", [128, NBIS], F32, pB)
        pT = [sb(f"b_pT{i}", [128, 1024], BF16, pB) for i in range(2)]
        rden = sb("b_rden", [128, 8], F32, pB)
        o_n = sb("b_on", [128, 8, 128], BF16, pB)
        onT = sb("b_onT", [128, 8, 128], BF16, pB)
        attT = [sb(f"b_attT{i}", [128, 4, 128], BF16, pB) for i in range(2)]
        col = lambda n: bs[:, n:n + 1].k(("bs", n))
        M1, M2, LO, W0, MID, CNT, TMP = [col(n) for n in range(7)]
        d_att = dbg_out("att", [NOWN, 128, 512], BF16) if "att" in dbg else None
        d_thr = dbg_out("thr", [NOWN, 128, 8]) if "att" in dbg else None

        for i in range(nblk):
            nkt = 2 * i + 2
            Lq = nkt * 128
            if DEV_STAGE == 10:
                continue
            norm_block(x_own[i * 128:(i + 1) * 128, :], A1, SH1, uT1.v, W, i)
            if DEV_STAGE == 11:
                continue
            for hp in range(4):
                for kt in range(8):
                    O.mm(PS[1][:, hp * 128:(hp + 1) * 128], wq[:, kt, hp * 128:(hp + 1) * 128], uT1[:, kt, :],
                         kt == 0, kt == 7)
            O.cp("act", qT.v, PS[1].v.rearrange("p (a b) -> p a b", a=4))
            if DEV_STAGE == 12:
                continue
            for h in range(8):
                hp, hl = h // 2, h % 2
                O.mm(PS[2 + hl][:, hp * 128:(hp + 1) * 128], wukT[hl * 64:(hl + 1) * 64, hp, :],
                     qT[hl * 64:(hl + 1) * 64, hp, :], True, True)
            for hl in range(2):
                O.act(qlatT.v.rearrange("p (a b c) -> p a b c", a=4, b=2)[:, :, hl, :].k(("qlat", hl)),
                      PS[2 + hl].v.rearrange("p (a c) -> p a c", a=4), AF.Copy, scale=0.125)
            qlk = [("qlat", 0), ("qlat", 1)]
            if DEV_STAGE == 13:
                continue
            for kt in range(8):
                O.mm(PS[4][:, 0:260], uT1[:, kt, :], wqi[:, kt, :], kt == 0, kt == 7)
            O.act(absw.v, PS[4][:, 256:260], AF.Abs, scale=1.0 / 16)
            O.act(sgn.v, PS[4][:, 256:260], AF.Sign)
            O.tt("dve", qis.v, PS[4][:, 0:256].rearrange("p (h d) -> p h d", h=4),
                 absw.v.unsqueeze(2).broadcast_to([128, 4, 64]), ALU.mult)
            O.tt("dve", Dg.v, identf.v.unsqueeze(1).broadcast_to([128, 4, 128]),
                 sgn.v.unsqueeze(2).broadcast_to([128, 4, 128]), ALU.mult)
            pst = PS[0].v.bitcast(BF16)
            for hp2 in range(2):
                O.tr(pst[:, hp2 * 128:(hp2 + 1) * 128], qis[:, 2 * hp2:2 * hp2 + 2, :].rearrange("p a b -> p (a b)"),
                     identb.v)
            O.cp("act", qiT.v, pst[:, 0:256].rearrange("p (a b) -> p a b", a=2))
            if DEV_STAGE == 1:
                continue
            ngr = (Lq + 511) // 512
            Ik = []
            for kg in range(ngr):
                nk = min(512, Lq - kg * 512)
                k0 = kg * 512
                R = Rsb[kg % 2]
                for h in range(4):
                    hl = h % 2
                    O.mm(PS[1 + h][:, 0:nk], qiT[hl * 64:(hl + 1) * 64, h // 2, :], kiT2[hl * 64:(hl + 1) * 64, k0:k0 + nk],
                         True, True)
                    O.act(R[:, h, 0:nk].k((R.key, h)), PS[1 + h][:, 0:nk], AF.Relu)
                ib = PS[5 + kg % 2]
                for h in range(4):
                    O.mm(ib[:, 0:nk], Dg[:, h, :], R[:, h, 0:nk].k((R.key, h)), h == 0, h == 3)
                Ik.append(("I", kg))
                if kg == ngr - 1:
                    if nk > 256:
                        O.cp("act", Ibuf[:, k0:k0 + nk - 256].k(("I", kg)), ib[:, 0:nk - 256])
                    O.tt("dve", Ibuf[:, Lq - 256:Lq].k(("I", kg)), ib[:, nk - 256:nk], cm2.v, ALU.add)
                    O.tt("dve", t256.v, ib[:, nk - 256:nk], cm2p.v, ALU.add)
                    O.reduce(M2, t256.v, ALU.min)
                else:
                    O.cp("act", Ibuf[:, k0:k0 + nk].k(("I", kg)), ib[:, 0:nk])
            Iall = Ibuf[:, 0:Lq].k(*Ik)
            if DEV_STAGE == 2:
                continue
            O.max8(mx8.v, Iall)
            if Lq > 256:
                O.reduce(M1, Ibuf[:, 0:Lq - 256].k(*Ik), ALU.min)
                O.tt("dve", LO, M1, M2, ALU.min)
            else:
                O.cp("dve", LO, M2)
            O.tt("dve", W0, mx8[:, 0:1], LO, ALU.subtract)
            O.ts("dve", WK.v, pow2.v, W0, ALU.mult)
            for k in range(NBIS):
                O.tt("dve", MID, LO, WK[:, k:k + 1], ALU.add)
                O.ts("dve", nm[:, 0:Lq], Iall, MID, ALU.is_ge, 0.0, ALU.add, accum=CNT)
                O.stt(TMP, CNT, TOPK - 0.5, WK[:, k:k + 1], ALU.is_ge, ALU.mult)
                O.tt("dve", LO, LO, TMP, ALU.add)
            O.ts("dve", nm[:, 0:Lq], Iall, LO, ALU.is_lt)
            if d_thr is not None:
                O.dma("sp", d_thr[i], bs.v.k(*[("bs", n) for n in range(7)]), final=True)
            if DEV_STAGE == 3:
                continue
            for kt in range(nkt):
                lb = (PS[1], PS[2]) if kt % 2 == 0 else (PS[3], PS[4])
                p = pT[kt % 2]
                for half in range(2):
                    O.mm(lb[half].v, ckvT[:, kt * 128:(kt + 1) * 128], qlatT[:, half * 512:(half + 1) * 512].k(*qlk),
                         True, False)
                    O.mm(lb[half].v, nm[:, kt * 128:(kt + 1) * 128], negi8[:, half * 512:(half + 1) * 512], False, True)
                    O.act(p[:, half * 512:(half + 1) * 512].k((p.key, half)), lb[half].v, AF.Exp)
                for h in range(8):
                    bank, off = PS[5 + h // 3], (h % 3) * 129
                    O.mm(bank[:, off:off + 129], p[:, h * 128:(h + 1) * 128].k((p.key, h // 4)), ckv_sb[:, kt, :],
                         kt == 0 and h % 3 == 0, kt == nkt - 1)
            if DEV_STAGE == 4:
                continue
            for b3 in range(3):
                nh = 3 if b3 < 2 else 2
                v3 = PS[5 + b3][:, 0:nh * 129].rearrange("p (a b) -> p a b", a=nh)
                O.recip(rden[:, 3 * b3:3 * b3 + nh].k(("rden", b3)), v3[:, :, 128])
                O.tt("dve", o_n[:, 3 * b3:3 * b3 + nh, :].k(("on", b3)), v3[:, :, 0:128],
                     rden[:, 3 * b3:3 * b3 + nh].k(("rden", b3)).unsqueeze(2).broadcast_to([128, nh, 128]), ALU.mult)
            for h in range(8):
                O.tr(pst[:, h * 128:(h + 1) * 128], o_n[:, h, :].k(("on", h // 3)), identb.v)
            O.cp("act", onT.v, pst.rearrange("p (a b) -> p a b", a=8))
            for h in range(8):
                hp, hl = h // 2, h % 2
                O.mm(PS[1][hl * 64:(hl + 1) * 64, hp * 128:(hp + 1) * 128], wuv[:, h * 64:(h + 1) * 64], onT[:, h, :],
                     True, True)
            at = attT[i % 2]
            O.cp("act", at.v, PS[1].v.rearrange("p (a b) -> p a b", a=4))
            O.dma("sp", att_d[i].rearrange("p (a b) -> p a b", a=4), at.v)
            if d_att is not None:
                O.dma("sp", d_att[i].rearrange("p (a b) -> p a b", a=4), at.v, final=True)
        S.barrier()
        S.emit()
    if "stopB1" in dbg:
        return

    ngrp = (nblk + 3) // 4
    mod_d, final_g = g["mod_d"], g["final_g"]
    with contextlib.ExitStack() as pC:
        W = norm_work(pC, "c_")
        SH1, A1, G1 = load_mod(pC, "c_", [0, 1, 2])
        wg = sb("c_wg", [128, 8, 2048], BF16, pC)
        O.dma("pool", wg[:, :, 0:1024], wview(w_in, OFF["ga"], OFF["ga"] + 1024))
        O.dma("pool", wg[:, :, 1024:2048], wview(w_in, OFF["gb"], OFF["gb"] + 1024))
        wglu = sb("c_wglu", [128, 4, 2048], BF16, pC)
        O.dma("pool", wglu.v, g["w_glu"].rearrange("(kt p) c -> p kt c", p=128))
        bglu = sb("c_bglu", [128, 16], F32, pC)
        O.dma("sp", bglu.v, g["b_glu"].rearrange("(ft p) -> p ft", p=128))
        wap = sb("c_wap", [128, 4, 1024], BF16, pC)
        O.dma("pool", wap.v, g["w_ap"].rearrange("(kt p) c -> p kt c", p=128))
        wout = sb("c_wout", [128, 8, 1024], BF16, pC)
        O.dma("pool", wout.v, g["w_out"].rearrange("(kt p) c -> p kt c", p=128))
        uT4 = sb("c_uT4", [128, 8, 512], BF16, pC)
        sg = [sb(f"c_sg{w}", [128, 8, 512], BF16, pC) for w in range(2)]
        ygl = sb("c_ygl", [128, 4, 512], BF16, pC)
        attl = sb("c_attl", [128, 4, 512], BF16, pC)
        sgt = sb("c_sgt", [128, 512], BF16, pC)
        ys = sb("c_ys", [128, 512], BF16, pC)
        m1 = sb("c_m1", [128, 8, 512], BF16, pC)
        mT = sb("c_mT", [128, 8, 512], BF16, pC)
        ta = sb("c_ta", [128, 512], F32, pC)
        xr = [sb(f"c_xr{n}", [128, D], F32, pC) for n in range(2)]
        hout = [sb(f"c_hout{n}", [128, D], F32, pC) for n in range(2)]
        d_h = dbg_out("h", [NOWN * 128, D]) if "h" in dbg else None
        for gi in range(ngrp):
            for bl in range(4):
                i = 4 * gi + bl
                norm_block(x_own[i * 128:(i + 1) * 128, :], A1, SH1, uT4[:, :, bl * 128:(bl + 1) * 128].k(("uT4", bl)),
                           W, i)
                O.dma("sp", ygl[:, :, bl * 128:(bl + 1) * 128].k(("ygl", bl)), yg_d[i].rearrange("p (c t) -> p c t", c=4))
                O.dma("sp", attl[:, :, bl * 128:(bl + 1) * 128].k(("attl", bl)),
                      att_d[i].rearrange("p (c t) -> p c t", c=4))
            uk = [("uT4", bl) for bl in range(4)]
            yk = [("ygl", bl) for bl in range(4)]
            ak = [("attl", bl) for bl in range(4)]
            for w in range(2):
                for ft in range(8):
                    bank = PS[1 + ft % 2]
                    for kt in range(8):
                        O.mm(bank.v, wg[:, kt, w * 1024 + ft * 128:w * 1024 + (ft + 1) * 128], uT4[:, kt, :].k(*uk),
                             kt == 0, kt == 7)
                    O.act(sg[w][:, ft, :].k((sg[w].key, ft)), bank.v, AF.Sigmoid)
            for ft in range(8):
                for c4 in range(4):
                    O.mm(PS[3].v, wglu[:, c4, ft * 128:(ft + 1) * 128], ygl[:, c4, :].k(*yk), c4 == 0, c4 == 3)
                for c4 in range(4):
                    O.mm(PS[4].v, wglu[:, c4, 1024 + ft * 128:1024 + (ft + 1) * 128], ygl[:, c4, :].k(*yk),
                         c4 == 0, c4 == 3)
                O.act(sgt.v, PS[4].v, AF.Sigmoid, bias=bglu[:, 8 + ft:9 + ft])
                O.stt(ys.v, PS[3].v, bglu[:, ft:ft + 1], sgt.v, ALU.add, ALU.mult)
                O.tt("pool", m1[:, ft, :].k(("m1", ft)), ys.v, sg[0][:, ft, :].k((sg[0].key, ft)), ALU.mult)
            for ft in range(8):
                bank = PS[5 + ft % 2]
                for hp in range(4):
                    O.mm(bank.v, wap[:, hp, ft * 128:(ft + 1) * 128], attl[:, hp, :].k(*ak), hp == 0, hp == 3)
                O.tt("dve", ta.v, bank.v, sg[1][:, ft, :].k((sg[1].key, ft)), ALU.mult)
                O.tt("pool", mT[:, ft, :].k(("mT", ft)), ta.v, m1[:, ft, :].k(("m1", ft)), ALU.add)
            mk = [("mT", ft) for ft in range(8)]
            for bl in range(4):
                i = 4 * gi + bl
                xt = xr[i % 2]
                O.dma("sp", xt.v, x_own[i * 128:(i + 1) * 128, :])
                ho = hout[i % 2]
                for dn in range(2):
                    bank = PS[1 + dn]
                    for ft in range(8):
                        O.mm(bank.v, mT[:, ft, bl * 128:(bl + 1) * 128].k(*mk), wout[:, ft, dn * 512:(dn + 1) * 512],
                             ft == 0, ft == 7)
                    O.tt("dve", ta.v, bank.v, G1[:, dn * 512:(dn + 1) * 512], ALU.mult)
                    O.tt("pool", ho[:, dn * 512:(dn + 1) * 512].k((ho.key, dn)), ta.v, xt[:, dn * 512:(dn + 1) * 512],
                         ALU.add)
                hk = ho.v.k((ho.key, 0), (ho.key, 1))
                O.dma("sp", h_d[i * 128:(i + 1) * 128, :], hk)
                if d_h is not None:
                    O.dma("sp", d_h[i * 128:(i + 1) * 128, :], hk, final=True)
        S.barrier()
        S.emit()
    if "stopB2" in dbg:
        return

    with contextlib.ExitStack() as pD:
        W = norm_work(pD, "d_", nbuf=1)
        SH2, A2, G2 = load_mod(pD, "d_", [3, 4, 5])
        fgb = sb("d_fgb", [128, D], F32, pD)
        O.dma("sp", fgb.v, final_g.partition_broadcast(128))
        wfg = sb("d_wfg", [128, 8, FF], BF16, pD)
        wfu = sb("d_wfu", [128, 8, FF], BF16, pD)
        wfd = sb("d_wfd", [128, FF // 128, D], BF16, pD)
        for kt0 in range(0, 8, 4):
            O.dma("pool", wfg[:, kt0:kt0 + 4, :], g["w_fg"][kt0 * 128:(kt0 + 4) * 128, :].rearrange("(kt p) c -> p kt c", p=128))
            O.dma("pool", wfu[:, kt0:kt0 + 4, :], g["w_fu"][kt0 * 128:(kt0 + 4) * 128, :].rearrange("(kt p) c -> p kt c", p=128))
        for f0 in range(0, 22, 11):
            O.dma("pool", wfd[:, f0:f0 + 11, :], g["w_fd"][f0 * 128:(f0 + 11) * 128, :].rearrange("(kt p) c -> p kt c", p=128))
        u2T = sb("d_u2T", [128, 8, 512], BF16, pD)
        hid = sb("d_hid", [128, FF // 128, 512], BF16, pD)
        sil = sb("d_sil", [128, 512], BF16, pD)
        ta = sb("d_ta", [128, 512], F32, pD)
        hr = W["x"][0]
        h2 = sb("d_h2", [128, D], F32, pD)
        ot = [sb(f"d_ot{n}", [128, D], F32, pD) for n in range(1)]
        st = sb("d_st", [128, 4], F32, pD)
        for gi in range(ngrp):
            for bl in range(4):
                i = 4 * gi + bl
                norm_block(h_d[i * 128:(i + 1) * 128, :], A2, SH2, u2T[:, :, bl * 128:(bl + 1) * 128].k(("u2T", bl)), W, i)
            uk = [("u2T", bl) for bl in range(4)]
            for ft in range(FF // 128):
                bg, bu = PS[1 + ft % 2], PS[3 + ft % 2]
                for kt in range(8):
                    O.mm(bg.v, wfg[:, kt, ft * 128:(ft + 1) * 128], u2T[:, kt, :].k(*uk), kt == 0, kt == 7)
                for kt in range(8):
                    O.mm(bu.v, wfu[:, kt, ft * 128:(ft + 1) * 128], u2T[:, kt, :].k(*uk), kt == 0, kt == 7)
                O.act(sil.v, bg.v, AF.Silu)
                O.tt("dve", hid[:, ft, :].k(("hid", ft)), bu.v, sil.v, ALU.mult)
            hk = [("hid", ft) for ft in range(FF // 128)]
            for bl in range(4):
                i = 4 * gi + bl
                O.dma("sp", hr.v, h_d[i * 128:(i + 1) * 128, :])
                for dn in range(2):
                    bank = PS[5 + dn]
                    for ft in range(FF // 128):
                        O.mm(bank.v, hid[:, ft, bl * 128:(bl + 1) * 128].k(*hk), wfd[:, ft, dn * 512:(dn + 1) * 512],
                             ft == 0, ft == FF // 128 - 1)
                    O.tt("dve", ta.v, bank.v, G2[:, dn * 512:(dn + 1) * 512], ALU.mult)
                    O.tt("pool", h2[:, dn * 512:(dn + 1) * 512].k(("h2", dn)), ta.v, hr[:, dn * 512:(dn + 1) * 512],
                         ALU.add)
                h2k = h2.v.k(("h2", 0), ("h2", 1))
                o_t = ot[0]
                O.act(o_t.v, h2k, AF.Square, accum=st[:, 0:1].k("st0"))
                O.ts("dve", st[:, 1:2].k("st1"), st[:, 0:1].k("st0"), 1.0 / D, ALU.mult, EPS, ALU.add)
                O.act(st[:, 1:2].k("st1"), st[:, 1:2].k("st1"), AF.Sqrt)
                O.recip(st[:, 2:3].k("st2"), st[:, 1:2].k("st1"))
                O.stt(o_t.v, h2k, st[:, 2:3].k("st2"), fgb.v, ALU.mult, ALU.mult)
                O.dma("sp", out[i * 128:(i + 1) * 128, :], o_t.v, final=True)
        S.barrier()
        S.emit()
def _consts(p):
    bf = ml_dtypes.bfloat16
    ident = np.eye(128, dtype=np.float32)
    negi8 = np.tile(-BIGM * ident, (1, 8)).astype(bf)
    t = np.arange(128)[:, None]
    s = np.arange(128)[None, :]
    tri = np.where(s <= t, 0.0, NEG).astype(np.float32)
    full = np.full((128, 128), NEG, np.float32)
    zero = np.zeros((128, 128), np.float32)
    cm2 = np.concatenate([tri, full], 1) if p == 0 else np.concatenate([zero, tri], 1)
    cm2p = np.where(cm2 < 0, 1.0e30, 0.0).astype(np.float32)
    selv = np.zeros((128, 2), np.float32)
    selv[:, p] = 1.0
    pp = np.arange(128)
    maskf = np.stack([((pp // 32) % 2 == w) & ((pp // 16) % 2 == g2) for w in range(2) for g2 in range(2)],
                     1).astype(np.float32)
    masks = np.stack([(pp // 64 == 0), (pp // 64 == 1)], 1).astype(np.float32)
    pow2 = np.tile((0.5 ** np.arange(1, NBIS + 1))[None, :], (128, 1)).astype(np.float32)
    jv = np.tile(np.arange(9, dtype=np.float32)[None, :], (128, 1))
    iv = np.tile(np.arange(64, dtype=np.float32)[None, :], (128, 1))
    return dict(k_jv=jv, k_iv=iv, k_identb=ident.astype(bf), k_identf=ident, k_negi8=negi8, k_cm2=cm2, k_cm2p=cm2p,
                k_sel=selv, k_maskf=maskf, k_masks=masks, k_pow2=pow2)


def make_in_maps(inputs, cores=range(8)):
    f = lambda a: np.ascontiguousarray(np.asarray(a, dtype=np.float32))
    c_ = np.ascontiguousarray
    x = f(inputs["x"])
    shared = dict(
        w_mod=f(inputs["w_mod"])[0], b_mod=f(inputs["b_mod"])[0], norm1_g=f(inputs["norm1_g"])[0],
        w_in=f(inputs["w_in"])[0],
        w_ssm_glu=f(inputs["w_ssm_glu"])[0], b_ssm_glu=f(inputs["b_ssm_glu"])[0],
        kv_norm_g=f(inputs["kv_norm_g"])[0], idx_k_norm_g=f(inputs["idx_k_norm_g"])[0],
        w_ukT=c_(f(inputs["w_uk"])[0].reshape(128, 4, 2, 64).transpose(2, 3, 1, 0).reshape(128, 4, 128)),
        w_uv=f(inputs["w_uv"])[0].reshape(128, 512),
        w_attn_proj=f(inputs["w_attn_proj"])[0], w_out=f(inputs["w_out"])[0], norm2_g=f(inputs["norm2_g"])[0],
        w_ffn_gate=f(inputs["w_ffn_gate"])[0], w_ffn_up=f(inputs["w_ffn_up"])[0],
        w_ffn_down=f(inputs["w_ffn_down"])[0], final_g=f(inputs["final_g"]),
    )
    are, aim, ldt = f(inputs["ssm_a_re"])[0], f(inputs["ssm_a_im"])[0], f(inputs["ssm_log_dt"])[0]
    bre, bim = f(inputs["ssm_b_re"])[0], f(inputs["ssm_b_im"])[0]
    cre, cim = f(inputs["ssm_c_re"])[0], f(inputs["ssm_c_im"])[0]
    sl_a = lambda a: c_(a.reshape(16, 2, 64).transpose(1, 2, 0).reshape(128, 16))
    fl_a = lambda a: c_(np.broadcast_to(a.reshape(4, 8, 64).transpose(1, 0, 2)[:, None], (8, 16, 4, 64)).reshape(128, 256))
    shared.update(
        sl_are=sl_a(are), sl_aim=sl_a(aim),
        sl_ldt=c_(np.broadcast_to(ldt.reshape(16, 2).T[:, None, :], (2, 64, 16)).reshape(128, 16)),
        fl_are=fl_a(are), fl_aim=fl_a(aim),
        fl_ldt=c_(np.broadcast_to(ldt.reshape(4, 8).T[:, None, :, None], (8, 16, 4, 64)).reshape(128, 256)),
        sl_bre=c_(bre.reshape(16, 2, 64, 16).transpose(1, 2, 0, 3).reshape(128, 16, 16)),
        sl_bim=c_(bim.reshape(16, 2, 64, 16).transpose(1, 2, 0, 3).reshape(128, 16, 16)),
        sl_cre=c_(cre.reshape(16, 2, 16, 64).transpose(1, 3, 0, 2).reshape(128, 16, 16)),
        sl_cim=c_(cim.reshape(16, 2, 16, 64).transpose(1, 3, 0, 2).reshape(128, 16, 16)),
        fl_bre=c_(bre.reshape(4, 8, 64, 16).transpose(1, 3, 0, 2).reshape(128, 256)),
        fl_bim=c_(bim.reshape(4, 8, 64, 16).transpose(1, 3, 0, 2).reshape(128, 256)),
        dsk=c_(f(inputs["ssm_d"])[0].reshape(4, 128).T),
    )
    maps = []
    for core in cores:
        b, p = core // 2, core % 2
        xb = x[b]
        x_own = np.ascontiguousarray(xb.reshape(NB, 128, D)[p::2].reshape(NOWN * 128, D))
        m = dict(shared)
        m.update(x_all=xb, x_own=x_own, c=f(inputs["c"])[b])
        m.update(_consts(p))
        maps.append(m)
    return maps


def kernel(**inputs):
    nc = build()
    maps = make_in_maps(inputs)
    res = run_bass_kernel_spmd(nc, maps, core_ids=list(range(8)))
    x = np.asarray(inputs["x"])
    outp = np.empty(x.shape, np.float32)
    for core in range(8):
        b, p = core // 2, core % 2
        o = np.asarray(res.results[core]["out"]).reshape(NOWN, 128, D)
        outp[b].reshape(NB, 128, D)[p::2] = o
    return outp
```

```python
import contextlib
import math
import numpy as np
import ml_dtypes
import concourse.bass as bass
import concourse.mybir as mybir
from concourse.bass_utils import run_bass_kernel_spmd

F32 = mybir.dt.float32
BF16 = mybir.dt.bfloat16
I32 = mybir.dt.int32
AF = mybir.ActivationFunctionType
ALU = mybir.AluOpType
AX = mybir.AxisListType

D = 1024
L = 8192
NB = 64
NOWN = 32
TC = 8
OFF = dict(xs=0, q=512, ckv=1024, qi=1152, ki=1408, wi=1472, ga=1476, gb=2500)
FF = 2816
EPS = 1e-6
NEG = -1.0e30
BIGM = 30000.0
NBIS = 14
TOPK = 256
DEV_NBLK = None
DEV_STAGE = 0
DEV_SKIP_A = False
SELF_SYNC = True
NO_SELF_SYNC = ()


class _Op:
    __slots__ = ("eng", "fn", "deps", "inc", "tok", "kind", "uid")


class Sched:
    NDMA = 8

    def __init__(self, nc, es, self_sync=SELF_SYNC):
        self.nc = nc
        self.E = {"pe": nc.tensor, "act": nc.scalar, "dve": nc.vector, "pool": nc.gpsimd, "sp": nc.sync}
        self.sem = {k: es.enter_context(nc.semaphore("sem_" + k)) for k in self.E}
        self.dsem = {q: [es.enter_context(nc.semaphore(f"dsem_{q}_{i}")) for i in range(self.NDMA)]
                     for q in ("sp", "pool", "act")}
        self.cnt = {k: 0 for k in self.E}
        self.dcnt = {q: 0 for q in self.dsem}
        self.seen = {k: {} for k in self.E}
        self.lw = {}
        self.lr = {}
        self.ops = []
        self.self_sync = self_sync
        self.uid = 0
        self.final = []
        self.last = {}
        self.pend = []

    def _add(self, o, reads, writes):
        deps = {}
        for b in reads:
            w = self.lw.get(b)
            if w is not None:
                deps[w.uid] = w
        for b in writes:
            w = self.lw.get(b)
            if w is not None:
                deps[w.uid] = w
            for r in self.lr.get(b, {}).values():
                deps[r.uid] = r
        out = []
        for d in deps.values():
            if d is o:
                continue
            if d.kind == "c" and o.kind == "c" and d.eng == o.eng:
                if o.eng == "pe" or o.eng in NO_SELF_SYNC:
                    continue
            out.append(d)
            d.inc = True
        o.deps = out
        for b in reads:
            key = o.eng if o.kind == "c" else ("d", o.uid)
            self.lr.setdefault(b, {})[key] = o
        for b in writes:
            self.lw[b] = o
            self.lr[b] = {}
        self.ops.append(o)
        if o.kind == "c":
            self.last[o.eng] = o
        else:
            self.pend.append(o)
        return o

    def op(self, eng, fn, reads=(), writes=()):
        o = _Op()
        o.eng, o.fn, o.kind, o.inc, o.tok = eng, fn, "c", False, None
        self.uid += 1
        o.uid = self.uid
        return self._add(o, reads, writes)

    def dma(self, q, fn, reads=(), writes=(), final=False):
        o = _Op()
        o.eng, o.fn, o.kind, o.inc, o.tok = q, fn, "d", True, None
        self.uid += 1
        o.uid = self.uid
        if final:
            self.final.append(o)
        return self._add(o, reads, writes)

    def _wait(self, eng, tok):
        sem, val = tok
        if self.seen[eng].get(id(sem), 0) < val:
            self.E[eng].wait_ge(sem, val)
            self.seen[eng][id(sem)] = val

    def _wait_all(self, eng, toks):
        best = {}
        for sem, val in toks:
            if best.get(id(sem), (None, 0))[1] < val:
                best[id(sem)] = (sem, val)
        for tok in best.values():
            self._wait(eng, tok)

    def barrier(self):
        lasts = [o for o in self.last.values()] + list(self.pend)
        for d in lasts:
            d.inc = True
        for eng in self.E:
            o = _Op()
            o.eng, o.fn, o.kind, o.inc, o.tok = eng, None, "w", False, None
            self.uid += 1
            o.uid = self.uid
            o.deps = [d for d in lasts if not (d.kind == "c" and d.eng == eng)]
            self.ops.append(o)
        self.pend = []
        self.lw = {}
        self.lr = {}

    def emit(self):
        for o in self.ops:
            e = self.E[o.eng]
            self._wait_all(o.eng, [d.tok for d in o.deps])
            if o.kind == "w":
                continue
            if o.kind == "d":
                k = self.dcnt[o.eng]
                self.dcnt[o.eng] += 1
                slot, gen = k % self.NDMA, k // self.NDMA
                sem = self.dsem[o.eng][slot]
                if gen > 0:
                    self._wait(o.eng, (sem, 16 * gen))
                o.fn(e).then_inc(sem, 16)
                o.tok = (sem, 16 * (gen + 1))
            else:
                inst = o.fn(e)
                if o.inc:
                    self.cnt[o.eng] += 1
                    inst.then_inc(self.sem[o.eng], 1)
                    o.tok = (self.sem[o.eng], self.cnt[o.eng])
        for o in self.final:
            self._wait("sp", o.tok)
        self.ops = []


class Vw:
    __slots__ = ("ap", "keys")

    def __init__(self, ap, keys):
        self.ap = ap
        self.keys = keys if isinstance(keys, list) else [keys]

    def k(self, *keys):
        return Vw(self.ap, list(keys))

    def __getitem__(self, idx):
        return Vw(self.ap[idx], self.keys)

    def rearrange(self, *a, **kw):
        return Vw(self.ap.rearrange(*a, **kw), self.keys)

    def bitcast(self, dt):
        return Vw(self.ap.bitcast(dt), self.keys)

    def broadcast_to(self, shape):
        return Vw(self.ap.broadcast_to(list(shape)), self.keys)

    def unsqueeze(self, ax):
        return Vw(self.ap.unsqueeze(ax), self.keys)


class Tl:
    def __init__(self, t, key):
        self.t = t
        self.key = key

    def __getitem__(self, idx):
        return Vw(self.t[idx], self.key)

    @property
    def v(self):
        return Vw(self.t[:], self.key)


def _keys(*vs):
    out = []
    for v in vs:
        if isinstance(v, Vw):
            out.extend(v.keys)
    return out


def _ap(v):
    return v.ap if isinstance(v, Vw) else v


class Ops:
    def __init__(self, S):
        self.S = S

    def mm(self, out, lhsT, rhs, start, stop):
        self.S.op("pe", lambda e: e.matmul(out.ap, lhsT.ap, rhs.ap, start=start, stop=stop, skip_group_check=True),
                  reads=_keys(lhsT, rhs), writes=_keys(out))

    def tr(self, out, in_, ident):
        self.S.op("pe", lambda e: e.transpose(out.ap, in_.ap, ident.ap), reads=_keys(in_, ident), writes=_keys(out))

    def act(self, out, in_, func, bias=None, scale=None, accum=None, eng="act"):
        kw = {}
        if bias is not None:
            kw["bias"] = _ap(bias)
        if scale is not None:
            kw["scale"] = _ap(scale)
        if accum is not None:
            kw["accum_out"] = accum.ap
        self.S.op("act", lambda e: e.activation(out=out.ap, in_=in_.ap, func=func, **kw),
                  reads=_keys(in_, bias, scale), writes=_keys(out, accum))

    def ts(self, eng, out, in0, s1, op0, s2=None, op1=None, accum=None):
        kw = dict(scalar1=_ap(s1), scalar2=_ap(s2) if s2 is not None else None, op0=op0)
        if op1 is not None:
            kw["op1"] = op1
        if accum is not None:
            kw["accum_out"] = accum.ap
        self.S.op(eng, lambda e: e.tensor_scalar(out=out.ap, in0=in0.ap, **kw),
                  reads=_keys(in0, s1, s2), writes=_keys(out, accum))

    def tt(self, eng, out, in0, in1, op):
        self.S.op(eng, lambda e: e.tensor_tensor(out=out.ap, in0=in0.ap, in1=in1.ap, op=op),
                  reads=_keys(in0, in1), writes=_keys(out))

    def stt(self, out, in0, scalar, in1, op0, op1, eng="dve"):
        self.S.op(eng, lambda e: e.scalar_tensor_tensor(out=out.ap, in0=in0.ap, scalar=_ap(scalar), in1=in1.ap,
                                                        op0=op0, op1=op1),
                  reads=_keys(in0, scalar, in1), writes=_keys(out))

    def cp(self, eng, out, in_):
        if eng == "act":
            self.S.op("act", lambda e: e.activation(out=out.ap, in_=in_.ap, func=AF.Copy),
                      reads=_keys(in_), writes=_keys(out))
        else:
            self.S.op(eng, lambda e: e.tensor_copy(out=out.ap, in_=in_.ap), reads=_keys(in_), writes=_keys(out))

    def memset(self, eng, out, val):
        self.S.op(eng, lambda e: e.memset(out.ap, val), writes=_keys(out))

    def recip(self, out, in_):
        self.S.op("dve", lambda e: e.reciprocal(out=out.ap, in_=in_.ap), reads=_keys(in_), writes=_keys(out))

    def scan(self, out, d0, d1, init, op0, op1):
        self.S.op("dve", lambda e: e.tensor_tensor_scan(out=out.ap, data0=d0.ap, data1=d1.ap, initial=_ap(init),
                                                        op0=op0, op1=op1),
                  reads=_keys(d0, d1, init), writes=_keys(out))

    def max8(self, out, in_):
        self.S.op("dve", lambda e: e.max(out=out.ap, in_=in_.ap), reads=_keys(in_), writes=_keys(out))

    def reduce(self, out, in_, op, axis=AX.X):
        self.S.op("dve", lambda e: e.tensor_reduce(out=out.ap, in_=in_.ap, axis=axis, op=op),
                  reads=_keys(in_), writes=_keys(out))

    def dma(self, q, out, in_, final=False):
        o_ap, i_ap = _ap(out), _ap(in_)
        self.S.dma(q, lambda e: e.dma_start(out=o_ap, in_=i_ap), reads=_keys(in_), writes=_keys(out), final=final)


def build(dbg=None):
    nc = bass.Bass("TRN2", target_bir_lowering=False)
    es = contextlib.ExitStack()
    with es:
        _build(nc, es, dbg or set())
    return nc


def _dram_in(nc, name, shape, dt=F32):
    return nc.dram_tensor(name, list(shape), dt, kind="ExternalInput").ap()


GELU_C0 = 1.5957691216057308
GELU_C1 = 1.5957691216057308 * 0.044715


def _build(nc, es, dbg):
    S = Sched(nc, es)
    O = Ops(S)
    x_all = _dram_in(nc, "x_all", [L, D])
    x_own = _dram_in(nc, "x_own", [NOWN * 128, D])
    c_in = _dram_in(nc, "c", [D])
    w_mod = _dram_in(nc, "w_mod", [D, 6 * D])
    b_mod = _dram_in(nc, "b_mod", [6 * D])
    norm1_g = _dram_in(nc, "norm1_g", [D])
    w_in = _dram_in(nc, "w_in", [D, 3524])
    w_glu = _dram_in(nc, "w_ssm_glu", [512, 2048])
    b_glu = _dram_in(nc, "b_ssm_glu", [2048])
    kv_g = _dram_in(nc, "kv_norm_g", [128])
    ik_g = _dram_in(nc, "idx_k_norm_g", [64])
    w_ukT = _dram_in(nc, "w_ukT", [128, 4, 128])
    w_uv = _dram_in(nc, "w_uv", [128, 512])
    w_ap = _dram_in(nc, "w_attn_proj", [512, D])
    w_out = _dram_in(nc, "w_out", [D, D])
    norm2_g = _dram_in(nc, "norm2_g", [D])
    w_fg = _dram_in(nc, "w_ffn_gate", [D, FF])
    w_fu = _dram_in(nc, "w_ffn_up", [D, FF])
    w_fd = _dram_in(nc, "w_ffn_down", [FF, D])
    final_g = _dram_in(nc, "final_g", [D])
    sl_are = _dram_in(nc, "sl_are", [128, 16])
    sl_aim = _dram_in(nc, "sl_aim", [128, 16])
    sl_ldt = _dram_in(nc, "sl_ldt", [128, 16])
    fl_are = _dram_in(nc, "fl_are", [128, 256])
    fl_aim = _dram_in(nc, "fl_aim", [128, 256])
    fl_ldt = _dram_in(nc, "fl_ldt", [128, 256])
    sl_bre = _dram_in(nc, "sl_bre", [128, 16, 16])
    sl_bim = _dram_in(nc, "sl_bim", [128, 16, 16])
    sl_cre = _dram_in(nc, "sl_cre", [128, 16, 16])
    sl_cim = _dram_in(nc, "sl_cim", [128, 16, 16])
    fl_bre = _dram_in(nc, "fl_bre", [128, 256])
    fl_bim = _dram_in(nc, "fl_bim", [128, 256])
    d_dsk = _dram_in(nc, "dsk", [128, 4])
    k_identb = _dram_in(nc, "k_identb", [128, 128], BF16)
    k_identf = _dram_in(nc, "k_identf", [128, 128])
    k_negi8 = _dram_in(nc, "k_negi8", [128, 1024], BF16)
    k_cm2 = _dram_in(nc, "k_cm2", [128, 256])
    k_cm2p = _dram_in(nc, "k_cm2p", [128, 256])
    k_sel = _dram_in(nc, "k_sel", [128, 2])
    k_maskf = _dram_in(nc, "k_maskf", [128, 4])
    k_masks = _dram_in(nc, "k_masks", [128, 2])
    k_pow2 = _dram_in(nc, "k_pow2", [128, NBIS])
    k_jv = _dram_in(nc, "k_jv", [128, 9])
    k_iv = _dram_in(nc, "k_iv", [128, 64])

    out = nc.dram_tensor("out", [NOWN * 128, D], F32, kind="ExternalOutput").ap()
    yg_d = nc.dram_tensor("yg_scratch", [NOWN, 128, 512], BF16, kind="Internal").ap()
    h_d = nc.dram_tensor("h_scratch", [NOWN * 128, D], F32, kind="Internal").ap()
    mod_d = nc.dram_tensor("mod_scratch", [128, 6 * D], F32, kind="Internal").ap()

    def dbg_out(name, shape, dt=F32):
        return nc.dram_tensor("dbg_" + name, list(shape), dt, kind="ExternalOutput").ap()

    def sb(name, shape, dt=F32, stack=es, key=None):
        t = stack.enter_context(nc.sbuf_tensor(name, list(shape), dt))
        return Tl(t, key or name)

    PS = [Tl(es.enter_context(nc.psum_tensor(f"psb{i}", [128, 512], F32)), f"ps{i}") for i in range(8)]

    ncd = nc.allow_non_contiguous_dma(reason="small parameter loads")
    ncd.__enter__()

    identb = sb("identb", [128, 128], BF16)
    identf = sb("identf", [128, 128], F32)
    O.dma("sp", identb.v, k_identb[:, :])
    O.dma("sp", identf.v, k_identf[:, :])
    sel = sb("sel", [128, 2], F32)
    O.dma("sp", sel.v, k_sel[:, :])

    with contextlib.ExitStack() as p0:
        mod = sb("mod", [128, 6 * D], F32, p0)
        SH1, A1, G1, SH2, A2, G2 = [mod[:, i * D:(i + 1) * D].k(("mod", 2 * i), ("mod", 2 * i + 1))
                                    for i in range(6)]
        cT = sb("cT", [128, 8], F32, p0)
        condT = sb("condT", [128, 8], F32, p0)
        crep = sb("crep", [128, 8, 128], F32, p0)
        bmod = sb("bmodbc", [128, 6 * D], F32, p0)
        gbc = sb("gbc", [128, 2, D], F32, p0)
        wm = [sb(f"wm{i}", [128, 8, 512], F32, p0) for i in range(2)]
        O.dma("sp", cT.v, c_in.rearrange("(kt p) -> p kt", p=128))
        O.dma("sp", bmod.v, b_mod.partition_broadcast(128))
        O.dma("sp", gbc[:, 0, :].k("gbc0"), norm1_g.partition_broadcast(128))
        O.dma("sp", gbc[:, 1, :].k("gbc1"), norm2_g.partition_broadcast(128))
        O.act(condT.v, cT.v, AF.Silu)
        O.cp("dve", crep.v, condT.v.unsqueeze(2).broadcast_to([128, 8, 128]))
        for n in range(12):
            wt = wm[n % 2]
            O.dma("sp", wt.v, w_mod[:, n * 512:(n + 1) * 512].rearrange("(kt p) c -> p kt c", p=128))
            bank = PS[n % 2]
            for kt in range(8):
                O.mm(bank.v, crep[:, kt, :], wt[:, kt, :], kt == 0, kt == 7)
            O.tt("dve", mod[:, n * 512:(n + 1) * 512].k(("mod", n)), bank.v, bmod[:, n * 512:(n + 1) * 512], ALU.add)
        for which, Av in ((0, A1), (1, A2)):
            O.stt(Av, Av, 1.0, gbc[:, which, :].k(f"gbc{which}"), ALU.add, ALU.mult)
        O.dma("sp", mod_d[:, :], mod.v.k(*[("mod", n) for n in range(12)]))
        if "mod" in dbg:
            O.dma("sp", dbg_out("mod", [128, 6 * D])[:, :], mod.v.k(*[("mod", n) for n in range(12)]), final=True)
        S.barrier()
        S.emit()

    def norm_block(xsrc, A, SHv, uT_dst, W, idx):
        xt = W["x"][idx % W["nbuf"]]
        O.dma("sp", xt.v, xsrc)
        ss = W["ss"][:, idx % 2:idx % 2 + 1].k(("ss", idx % 2))
        O.act(W["junk"].v, xt.v, AF.Square, accum=ss)
        var = W["var"][:, idx % 2:idx % 2 + 1].k(("var", idx % 2))
        O.ts("dve", var, ss, 1.0 / D, ALU.mult, EPS, ALU.add)
        O.act(var, var, AF.Sqrt)
        rstd = W["rstd"][:, idx % 2:idx % 2 + 1].k(("rstd", idx % 2))
        O.recip(rstd, var)
        O.stt(W["t1"].v, xt.v, rstd, A, ALU.mult, ALU.mult)
        ub = W["ub"][idx % W["nbuf"]]
        O.tt("pool", ub.v, W["t1"].v, SHv, ALU.add)
        pst = PS[0].v.bitcast(BF16)
        for kt in range(8):
            O.tr(pst[:, kt * 128:(kt + 1) * 128], ub[:, kt * 128:(kt + 1) * 128], identb.v)
        O.cp("act", uT_dst, pst.rearrange("p (k t) -> p k t", k=8))

    def load_mod(stack, pfx, idxs):
        outv = []
        for i in idxs:
            t = sb(f"{pfx}mod{i}", [128, D], F32, stack)
            O.dma("sp", t.v, mod_d[:, i * D:(i + 1) * D])
            outv.append(t.v)
        return outv

    def norm_work(stack, pfx, nbuf=2):
        return dict(
            nbuf=nbuf,
            x=[sb(f"{pfx}x{i}", [128, D], F32, stack) for i in range(nbuf)],
            ss=sb(f"{pfx}ss", [128, 2], F32, stack), var=sb(f"{pfx}var", [128, 2], F32, stack),
            rstd=sb(f"{pfx}rstd", [128, 2], F32, stack),
            junk=sb(f"{pfx}junk", [128, D], BF16, stack), t1=sb(f"{pfx}t1", [128, D], F32, stack),
            ub=[sb(f"{pfx}ub{i}", [128, D], BF16, stack) for i in range(nbuf)],
        )

    ckv_d = nc.dram_tensor("ckv_scratch", [128, NB, 129], BF16, kind="Internal").ap()
    ckvT_d = nc.dram_tensor("ckvT_scratch", [128, L], BF16, kind="Internal").ap()
    kiT_d = nc.dram_tensor("kiT_scratch", [128, L], BF16, kind="Internal").ap()

    with contextlib.ExitStack() as pA:
        W = norm_work(pA, "a_")
        SH1, A1 = load_mod(pA, "a_", [0, 1])
        wsh = sb("wsh", [128, 8, 704], BF16, pA)
        for (c0, c1, o0) in ((OFF["xs"], OFF["xs"] + 512, 0), (OFF["ckv"], OFF["ckv"] + 128, 512),
                             (OFF["ki"], OFF["ki"] + 64, 640)):
            O.dma("pool", wsh[:, :, o0:o0 + (c1 - c0)], w_in[:, c0:c1].rearrange("(kt p) c -> p kt c", p=128))
        gkv = sb("gkv", [128, 128], F32, pA)
        gik = sb("gik", [128, 64], F32, pA)
        O.dma("sp", gkv.v, kv_g.partition_broadcast(128))
        O.dma("sp", gik.v, ik_g.partition_broadcast(128))
        cks = sb("a_cks", [128, 4, 129], BF16, pA)
        ckTs = sb("a_ckTs", [128, 512], BF16, pA)
        kiTs = sb("a_kiTs", [128, 512], BF16, pA)
        O.memset("pool", cks[:, :, 128:129].k("ckv_ones"), 1.0)
        uT = [sb(f"a_uT{i}", [128, 8, 512], BF16, pA) for i in range(2)]
        ssk = sb("a_ssk", [128, 2], F32, pA)
        rsk = sb("a_rsk", [128, 2], F32, pA)
        junk2 = sb("a_junk2", [128, 128], BF16, pA)
        kin2 = sb("a_kin2", [128, 2, 64], BF16, pA)

        S5 = _s5_prepare(nc, S, O, sb, pA, PS, identf, locals())
        xsT = [sb(f"a_xsT{i}", [128, 4, 8, 64], BF16, pA) for i in range(2)]
        BR = sb("a_BR", [128, 16, 64], F32, pA)
        BI = sb("a_BI", [128, 16, 64], F32, pA)
        T1 = [sb(f"a_T1{n}", [128, 8, 64], F32, pA) for n in range(2)]
        T2 = [sb(f"a_T2{n}", [128, 8, 64], F32, pA) for n in range(2)]
        STr = sb("a_STr", [128, 16, 64], F32, pA)
        STi = sb("a_STi", [128, 16, 64], F32, pA)
        inj = sb("a_inj", [128, 2, 16], F32, pA)
        itmp = sb("a_itmp", [128, 2, 16], F32, pA)
        Sbf = [[sb(f"a_Sbf{i}{c}", [128, 16, 65], BF16, pA) for c in range(2)] for i in range(2)]
        gx2 = [sb(f"a_gx2{n}", [128, 512], F32, pA) for n in range(2)]
        ygs = sb("a_ygs", [128, 4, 512], BF16, pA)
        ygt = sb("a_ygt", [128, 4, 128], BF16, pA)
        ygo = [sb(f"a_ygo{i}", [128, 4, 128], BF16, pA) for i in range(2)]
        O.memset("pool", Sbf[1][0][:, :, 64:65], 0.0)
        O.memset("pool", Sbf[1][1][:, :, 64:65], 0.0)

        ygdbg = dbg_out("yg", [NOWN, 128, 512], BF16) if "s5" in dbg else None
        def front(sbi):
            u = uT[sbi % 2]
            xs = xsT[sbi % 2]
            ukeys = [(u.key, bl) for bl in range(4)]
            ch = []
            for bl in range(4):
                blk = 4 * sbi + bl
                ch.append(lambda bl=bl, blk=blk: norm_block(
                    x_all[blk * 128:(blk + 1) * 128, :], A1, SH1, u[:, :, bl * 128:(bl + 1) * 128].k((u.key, bl)), W, blk))

            def xs_proj(c4):
                bank = PS[1 + c4 % 2]
                for kt in range(8):
                    O.mm(bank.v, wsh[:, kt, c4 * 128:(c4 + 1) * 128], u[:, kt, :].k(*ukeys), kt == 0, kt == 7)
                O.cp("act", xs[:, c4, :, :].k((xs.key, c4)), bank.v.rearrange("p (m r) -> p r m", r=8))
            for c4 in range(4):
                ch.append(lambda c4=c4: xs_proj(c4))

            def keys(bl):
                bank = PS[3]
                for kt in range(8):
                    O.mm(bank[:, 0:192], u[:, kt, bl * 128:(bl + 1) * 128].k((u.key, bl)), wsh[:, kt, 512:704],
                         kt == 0, kt == 7)
                O.act(junk2.v, bank[:, 0:128], AF.Square, accum=ssk[:, 0:1].k("ssk0"))
                O.act(junk2[:, 0:64], bank[:, 128:192], AF.Square, accum=ssk[:, 1:2].k("ssk1"))
                O.ts("dve", rsk[:, 0:1].k("rsk0"), ssk[:, 0:1].k("ssk0"), 1.0 / 128, ALU.mult, EPS, ALU.add)
                O.ts("dve", rsk[:, 1:2].k("rsk1"), ssk[:, 1:2].k("ssk1"), 1.0 / 64, ALU.mult, EPS, ALU.add)
                O.act(rsk.v.k("rsk0", "rsk1"), rsk.v.k("rsk0", "rsk1"), AF.Sqrt)
                O.recip(rsk.v.k("rsk0", "rsk1"), rsk.v.k("rsk0", "rsk1"))
                ckb = cks[:, bl, 0:128].k(("cks", bl))
                O.stt(ckb, bank[:, 0:128], rsk[:, 0:1].k("rsk0"), gkv.v, ALU.mult, ALU.mult)
                for dup in range(2):
                    O.stt(kin2[:, dup, :], bank[:, 128:192], rsk[:, 1:2].k("rsk1"), gik.v, ALU.mult, ALU.mult)
                pst = PS[4].v.bitcast(BF16)
                O.tr(pst[:, 0:128], ckb, identb.v)
                O.tr(pst[:, 128:256], kin2.v.rearrange("p a b -> p (a b)"), identb.v)
                O.cp("dve", ckTs[:, bl * 128:(bl + 1) * 128].k(("ckTs", bl)), pst[:, 0:128])
                O.cp("dve", kiTs[:, bl * 128:(bl + 1) * 128].k(("kiTs", bl)), pst[:, 128:256])
            for bl in range(4):
                ch.append(lambda bl=bl: keys(bl))

            def spill():
                O.dma("sp", ckv_d[:, 4 * sbi:4 * sbi + 4, :], cks.v.k("ckv_ones", *[("cks", bl) for bl in range(4)]))
                O.dma("sp", ckvT_d[:, sbi * 512:(sbi + 1) * 512], ckTs.v.k(*[("ckTs", bl) for bl in range(4)]))
                O.dma("sp", kiT_d[:, sbi * 512:(sbi + 1) * 512], kiTs.v.k(*[("kiTs", bl) for bl in range(4)]))
            ch.append(spill)
            return ch

        def merge(ca, cb):
            na, nb = len(ca), len(cb)
            ia = ib = 0
            while ia < na or ib < nb:
                if ib >= nb or (ia < na and ia * nb <= ib * na):
                    ca[ia]()
                    ia += 1
                else:
                    cb[ib]()
                    ib += 1

        nsb = 0 if DEV_SKIP_A else NB // 4
        if nsb:
            for c in front(0):
                c()
        for sbi in range(nsb):
            s5c = _s5_superblock(S, O, PS, S5, sbi, xsT[sbi % 2], BR, BI, T1, T2, STr, STi, inj, itmp, Sbf,
                                 gx2, ygs, ygt, ygo, sel, yg_d, ygdbg)
            merge(s5c, front(sbi + 1) if sbi + 1 < nsb else [])
        if "ckv" in dbg:
            S.barrier()
            dk = dbg_out("ckvT", [128, L], BF16)
            S.dma("sp", lambda e: e.dma_start(out=dk[:, :], in_=ckvT_d[:, :]), final=True)
            dk2 = dbg_out("kiT2", [128, L], BF16)
            S.dma("sp", lambda e: e.dma_start(out=dk2[:, :], in_=kiT_d[:, :]), final=True)
            dk3 = dbg_out("ckv_sb", [128, NB, 129], BF16)
            S.dma("sp", lambda e: e.dma_start(out=dk3[:, :, :], in_=ckv_d[:, :, :]), final=True)
        if "s5" in dbg:
            for nm in S5["dbg"]:
                t = S5["dbg"][nm]
                shp = list(t.t.shape)
                O.dma("sp", dbg_out(nm, [128, int(np.prod(shp[1:]))], t.t.dtype)[:, :],
                      t.v.rearrange("p a b c -> p (a b c)") if len(shp) == 4 else
                      (t.v.rearrange("p a b -> p (a b)") if len(shp) == 3 else t.v), final=True)
        S.barrier()
        S.emit()
    if "stopA" in dbg:
        ncd.__exit__(None, None, None)
        return

    _phase_b1_b2_c(nc, S, O, sb, PS, dbg, dbg_out, norm_block, norm_work, load_mod, locals())
    ncd.__exit__(None, None, None)
def _phasor(O, cyc, outc, outs, tmps):
    ri, rf, s1, q = tmps
    O.cp("dve", ri, cyc)
    O.cp("dve", rf, ri)
    O.tt("dve", rf, cyc, rf, ALU.subtract)
    O.act(s1, rf, AF.Sin, scale=math.pi)
    O.act(q, rf, AF.Sin, scale=math.pi / 2)
    O.tt("dve", q, q, q, ALU.mult)
    O.ts("dve", q, q, -2.0, ALU.mult, 1.0, ALU.add)
    O.stt(outs, s1, 2.0, q, ALU.mult, ALU.mult)
    O.tt("dve", s1, s1, s1, ALU.mult)
    O.ts("dve", outc, s1, -2.0, ALU.mult, 1.0, ALU.add)


def _s5_prepare(nc, S, O, sb, st, PS, identf, g):
    R = {}
    KT = sb("s5_KT", [128, 8, 4, 128], BF16, st)
    Fm = [sb(f"s5_Fm{c}", [128, 8, 4, 2, 2, 64], BF16, st) for c in range(2)]
    Em = [sb(f"s5_Em{c}", [128, 8, 8, 2, 2, 2, 16], BF16, st) for c in range(2)]
    Dc = sb("s5_Dc", [128, 16, 64], F32, st)
    Ds = sb("s5_Ds", [128, 16, 64], F32, st)
    rho = sb("s5_rho", [128, 16, 64], F32, st)
    Lam = sb("s5_Lam", [128, 2, 16], F32, st)
    R.update(KT=KT, Fm=Fm, Em=Em, Dc=Dc, Ds=Ds, rho=rho, Lam=Lam)
    R["dbg"] = dict(s5_KT=KT, s5_Fm0=Fm[0], s5_Fm1=Fm[1], s5_Em0=Em[0], s5_Em1=Em[1], s5_Dc=Dc, s5_Ds=Ds,
                    s5_rho=rho, s5_Lam=Lam)
    holder = {}

    def ld(name, src, shape):
        t = sb("s5t_" + name, shape, F32, holder["tp"])
        O.dma("sp", t.v, src)
        return t

    def tmp(name, shape, dt=F32):
        return sb("s5t_" + name, shape, dt, holder["tp"])

    with contextlib.ExitStack() as tp:
        holder["tp"] = tp

        masks = ld("masks", g["k_masks"][:, :], [128, 2])
        jv = ld("jv", g["k_jv"][:, :], [128, 9])
        iv = ld("iv", g["k_iv"][:, :], [128, 64])
        dsk = ld("dsk", g["d_dsk"][:, :], [128, 4])

        def lam_common(pfx, are_d, aim_d, ldt_d, Wd):
            are = ld(pfx + "are", are_d[:, :], [128, Wd])
            aim = ld(pfx + "aim", aim_d[:, :], [128, Wd])
            dt = ld(pfx + "ldt", ldt_d[:, :], [128, Wd])
            O.act(dt.v, dt.v, AF.Exp)
            x1 = tmp(pfx + "x1", [128, Wd])
            angc = tmp(pfx + "angc", [128, Wd])
            O.tt("dve", x1.v, are.v, dt.v, ALU.mult)
            O.tt("dve", angc.v, aim.v, dt.v, ALU.mult)
            O.ts("dve", angc.v, angc.v, 1.0 / (2 * math.pi), ALU.mult)
            ph = (tmp(pfx + "ri", [128, Wd], I32).v, tmp(pfx + "rf", [128, Wd]).v, tmp(pfx + "s1", [128, Wd]).v,
                  tmp(pfx + "q", [128, Wd]).v)
            rj = tmp(pfx + "rj", [128, Wd])
            uc = tmp(pfx + "uc", [128, Wd])
            us = tmp(pfx + "us", [128, Wd])
            mg = tmp(pfx + "mg", [128, Wd])

            def lam_pow(j, lr, li):
                O.ts("dve", rj.v, angc.v, float(j), ALU.mult)
                _phasor(O, rj.v, uc.v, us.v, ph)
                O.act(mg.v, x1.v, AF.Exp, scale=float(j))
                O.tt("dve", lr, mg.v, uc.v, ALU.mult)
                O.tt("dve", li, mg.v, us.v, ALU.mult)

            l1r = tmp(pfx + "l1r", [128, Wd])
            l1i = tmp(pfx + "l1i", [128, Wd])
            lam_pow(1, l1r.v, l1i.v)
            den = tmp(pfx + "den", [128, Wd])
            t0 = tmp(pfx + "t0", [128, Wd])
            cre = tmp(pfx + "cfr", [128, Wd])
            cim = tmp(pfx + "cfi", [128, Wd])
            O.tt("dve", den.v, are.v, are.v, ALU.mult)
            O.tt("dve", t0.v, aim.v, aim.v, ALU.mult)
            O.tt("dve", den.v, den.v, t0.v, ALU.add)
            O.recip(den.v, den.v)
            O.ts("dve", l1r.v, l1r.v, -1.0, ALU.add)
            O.tt("dve", cre.v, l1r.v, are.v, ALU.mult)
            O.tt("dve", t0.v, l1i.v, aim.v, ALU.mult)
            O.tt("dve", cre.v, cre.v, t0.v, ALU.add)
            O.tt("dve", cre.v, cre.v, den.v, ALU.mult)
            O.tt("dve", cim.v, l1i.v, are.v, ALU.mult)
            O.tt("dve", t0.v, l1r.v, aim.v, ALU.mult)
            O.tt("dve", cim.v, cim.v, t0.v, ALU.subtract)
            O.tt("dve", cim.v, cim.v, den.v, ALU.mult)
            return lam_pow, cre, cim, x1, angc, ph

        lam_pow, cre, cim, x1, angc, ph = lam_common("sl_", g["sl_are"], g["sl_aim"], g["sl_ldt"], 16)
        Bre = ld("sl_bre", g["sl_bre"][:, :, :], [128, 16, 16])
        Bim = ld("sl_bim", g["sl_bim"][:, :, :], [128, 16, 16])
        Cre = ld("sl_cre", g["sl_cre"][:, :, :], [128, 16, 16])
        Cim = ld("sl_cim", g["sl_cim"][:, :, :], [128, 16, 16])
        bc = lambda t: t.v.unsqueeze(2).broadcast_to([128, 16, 16])
        bcv = lambda v: v.unsqueeze(2).broadcast_to([128, 16, 16])
        Bbr = tmp("sl_Bbr", [128, 16, 16])
        Bbi = tmp("sl_Bbi", [128, 16, 16])
        ta = tmp("sl_ta", [128, 16, 16])
        tb = tmp("sl_tb", [128, 16, 16])
        O.tt("dve", Bbr.v, Bre.v, bc(cre), ALU.mult)
        O.tt("dve", ta.v, Bim.v, bc(cim), ALU.mult)
        O.tt("dve", Bbr.v, Bbr.v, ta.v, ALU.subtract)
        O.tt("dve", Bbi.v, Bim.v, bc(cre), ALU.mult)
        O.tt("dve", ta.v, Bre.v, bc(cim), ALU.mult)
        O.tt("dve", Bbi.v, Bbi.v, ta.v, ALU.add)
        CM = [tmp("sl_CMr", [128, 8, 2, 2, 2, 16], BF16), tmp("sl_CMi", [128, 8, 2, 2, 2, 16], BF16)]
        GM = [tmp("sl_GMr", [128, 8, 2, 2, 2, 16], BF16), tmp("sl_GMi", [128, 8, 2, 2, 2, 16], BF16)]
        for tl in CM + GM + Em:
            O.memset("pool", tl.v, 0.0)
        gs = lambda t, s: t.v.rearrange("p (gq s) c -> p gq s c", s=2)[:, :, s, :]
        for s in range(2):
            for g2 in range(2):
                O.ts("dve", CM[0][:, :, s, s, g2, :], gs(Cre, s), masks[:, g2:g2 + 1], ALU.mult)
                O.ts("dve", CM[1][:, :, s, s, g2, :], gs(Cim, s), masks[:, g2:g2 + 1], ALU.mult, -1.0, ALU.mult)
        ljr = tmp("sl_ljr", [128, 16])
        lji = tmp("sl_lji", [128, 16])
        O.memset("pool", KT.v, 0.0)
        for j in range(9):
            lam_pow(j, ljr.v, lji.v)
            if j < 8:
                O.tt("dve", ta.v, Bbr.v, bcv(ljr.v), ALU.mult)
                O.tt("dve", tb.v, Bbi.v, bcv(lji.v), ALU.mult)
                O.tt("dve", ta.v, ta.v, tb.v, ALU.subtract)
                for s in range(2):
                    for g2 in range(2):
                        O.ts("dve", GM[0][:, :, s, s, g2, :], gs(ta, s), masks[:, g2:g2 + 1], ALU.mult)
                O.tt("dve", ta.v, Bbi.v, bcv(ljr.v), ALU.mult)
                O.tt("dve", tb.v, Bbr.v, bcv(lji.v), ALU.mult)
                O.tt("dve", ta.v, ta.v, tb.v, ALU.add)
                for s in range(2):
                    for g2 in range(2):
                        O.ts("dve", GM[1][:, :, s, s, g2, :], gs(ta, s), masks[:, g2:g2 + 1], ALU.mult)
                bank = PS[4 + j // 2]
                f64 = lambda v: v.rearrange("p a b c -> p (a b c)")
                for gh in range(16):
                    c4, pair = gh // 4, gh % 4
                    q = pair // 2
                    col = ((j % 2) * 4 + c4) * 64
                    o = bank[64 * q:64 * q + 64, col:col + 64]
                    O.mm(o, f64(GM[0][:, gh // 2, gh % 2, :, :, :]), f64(CM[0][:, gh // 2, gh % 2, :, :, :]),
                         pair % 2 == 0, False)
                    O.mm(o, f64(GM[1][:, gh // 2, gh % 2, :, :, :]), f64(CM[1][:, gh // 2, gh % 2, :, :, :]),
                         False, pair % 2 == 1)
                if j % 2 == 1:
                    jh = j // 2
                    for q in range(2):
                        O.cp("dve", KT[64 * q:64 * q + 64, 2 * jh:2 * jh + 2, :, 64 * q:64 * q + 64],
                             bank[64 * q:64 * q + 64, :].rearrange("p (j c k) -> p j c k", j=2, c=4))
            if j >= 1:
                r = j - 1
                O.tt("dve", ta.v, Cre.v, bcv(ljr.v), ALU.mult)
                O.tt("dve", tb.v, Cim.v, bcv(lji.v), ALU.mult)
                O.tt("dve", ta.v, ta.v, tb.v, ALU.subtract)
                for s in range(2):
                    for g2 in range(2):
                        O.ts("dve", Em[0][:, r, :, s, s, g2, :], gs(ta, s), masks[:, g2:g2 + 1], ALU.mult)
                O.tt("dve", ta.v, Cre.v, bcv(lji.v), ALU.mult)
                O.tt("dve", tb.v, Cim.v, bcv(ljr.v), ALU.mult)
                O.tt("dve", ta.v, ta.v, tb.v, ALU.add)
                for s in range(2):
                    for g2 in range(2):
                        O.ts("dve", Em[1][:, r, :, s, s, g2, :], gs(ta, s), masks[:, g2:g2 + 1], ALU.mult, -1.0,
                             ALU.mult)
            if j == 8:
                O.cp("dve", Lam[:, 0, :], ljr.v)
                O.cp("dve", Lam[:, 1, :], lji.v)
        for c4 in range(4):
            O.stt(KT[:, 0, c4, :], identf.v, dsk[:, c4:c4 + 1], KT[:, 0, c4, :], ALU.mult, ALU.add)
        r8 = tmp("sl_r8", [128, 16])
        O.ts("dve", r8.v, angc.v, 8.0, ALU.mult)
        O.cp("dve", ph[0], r8.v)
        O.cp("dve", ph[1], ph[0])
        O.tt("dve", r8.v, r8.v, ph[1], ALU.subtract)
        RT = tmp("sl_RT", [128, 16, 64])
        O.tt("dve", RT.v, r8.v.unsqueeze(2).broadcast_to([128, 16, 64]),
             iv.v.unsqueeze(1).broadcast_to([128, 16, 64]), ALU.mult)
        ph2 = (tmp("sl_ri2", [128, 512], I32).v, tmp("sl_rf2", [128, 512]).v, tmp("sl_s12", [128, 512]).v,
               tmp("sl_q2", [128, 512]).v)
        fl = lambda t, h: t[:, 8 * h:8 * h + 8, :].rearrange("p a b -> p (a b)")
        for h in range(2):
            _phasor(O, fl(RT, h), fl(Dc, h), fl(Ds, h), ph2)
        rh = tmp("sl_rh", [128, 16])
        O.act(rh.v, x1.v, AF.Exp, scale=8.0)
        O.cp("dve", rho.v, rh.v.unsqueeze(2).broadcast_to([128, 16, 64]))
        O.memset("dve", rho[:, :, 0:1], 0.0)

        S.barrier()
        S.emit()
    with contextlib.ExitStack() as tp:
        holder["tp"] = tp
        maskf = ld("maskf", g["k_maskf"][:, :], [128, 4])
        lam_pow, cre, cim, x1, angc, ph = lam_common("fl_", g["fl_are"], g["fl_aim"], g["fl_ldt"], 256)
        Bre = ld("fl_bre", g["fl_bre"][:, :], [128, 256])
        Bim = ld("fl_bim", g["fl_bim"][:, :], [128, 256])
        Bbr = tmp("fl_Bbr", [128, 256])
        Bbi = tmp("fl_Bbi", [128, 256])
        ta = tmp("fl_ta", [128, 256])
        tb = tmp("fl_tb", [128, 256])
        O.tt("dve", Bbr.v, Bre.v, cre.v, ALU.mult)
        O.tt("dve", ta.v, Bim.v, cim.v, ALU.mult)
        O.tt("dve", Bbr.v, Bbr.v, ta.v, ALU.subtract)
        O.tt("dve", Bbi.v, Bim.v, cre.v, ALU.mult)
        O.tt("dve", ta.v, Bre.v, cim.v, ALU.mult)
        O.tt("dve", Bbi.v, Bbi.v, ta.v, ALU.add)
        ljr = tmp("fl_ljr", [128, 256])
        lji = tmp("fl_lji", [128, 256])
        v4 = lambda t: t.v.rearrange("p (c n) -> p c n", c=4)
        for j in range(8):
            lam_pow(j, ljr.v, lji.v)
            O.tt("dve", ta.v, Bbr.v, ljr.v, ALU.mult)
            O.tt("dve", tb.v, Bbi.v, lji.v, ALU.mult)
            O.tt("dve", ta.v, ta.v, tb.v, ALU.subtract)
            for wg in range(4):
                O.ts("dve", Fm[0][:, j, :, wg // 2, wg % 2, :], v4(ta), maskf[:, wg:wg + 1], ALU.mult)
            O.tt("dve", ta.v, Bbi.v, ljr.v, ALU.mult)
            O.tt("dve", tb.v, Bbr.v, lji.v, ALU.mult)
            O.tt("dve", ta.v, ta.v, tb.v, ALU.add)
            for wg in range(4):
                O.ts("dve", Fm[1][:, j, :, wg // 2, wg % 2, :], v4(ta), maskf[:, wg:wg + 1], ALU.mult)
        S.barrier()
        S.emit()
    return R


def _s5_superblock(S, O, PS, S5, sbi, xs, BR, BI, T1, T2, STr, STi, inj, itmp, Sbf,
                   gx2, ygs, ygt, ygo, sel, yg_d, ygdbg=None):
    KT, Fm, Em, Dc, Ds, rho, Lam = S5["KT"], S5["Fm"], S5["Em"], S5["Dc"], S5["Ds"], S5["rho"], S5["Lam"]
    cur, prv = Sbf[sbi % 2], Sbf[(sbi + 1) % 2]
    f2 = lambda v: v.rearrange("p a b -> p (a b)")
    BRk = BR.v.k(("BR", 0), ("BR", 1))
    BIk = BI.v.k(("BI", 0), ("BI", 1))
    SRr, SRi = BRk, BIk

    def snew(half):
        for ghl in range(8):
            gh = half * 8 + ghl
            c4, pair = gh // 4, gh % 4
            q = pair // 2
            for c in range(2):
                o = PS[5 + c][:, ghl * 64:(ghl + 1) * 64]
                for k in range(8):
                    O.mm(o, Fm[c][64 * q:64 * q + 64, 7 - k, c4, pair % 2, :, :].rearrange("p a b -> p (a b)"),
                         xs[64 * q:64 * q + 64, c4, k, :].k((xs.key, c4)), k == 0, k == 7)

    def demod(half):
        hs = slice(half * 8, half * 8 + 8)
        pr = PS[5].v.rearrange("p (a b) -> p a b", a=8)
        pi = PS[6].v.rearrange("p (a b) -> p a b", a=8)
        O.tt("dve", T1[0].v, pr, Dc[:, hs, :], ALU.mult)
        O.tt("dve", T2[0].v, pi, Ds[:, hs, :], ALU.mult)
        O.tt("pool", BR[:, hs, :].k(("BR", half)), T1[0].v, T2[0].v, ALU.add)
        O.tt("dve", T1[1].v, pi, Dc[:, hs, :], ALU.mult)
        O.tt("dve", T2[1].v, pr, Ds[:, hs, :], ALU.mult)
        O.tt("pool", BI[:, hs, :].k(("BI", half)), T1[1].v, T2[1].v, ALU.subtract)

    def scan_stage():
        if sbi > 0:
            O.tt("pool", BRk[:, :, 0], BRk[:, :, 0], inj[:, 0, :], ALU.add)
            O.tt("pool", BIk[:, :, 0], BIk[:, :, 0], inj[:, 1, :], ALU.add)
        O.scan(f2(STr.v), f2(rho.v), f2(BRk), 0.0, ALU.mult, ALU.add)
        O.scan(f2(STi.v), f2(rho.v), f2(BIk), 0.0, ALU.mult, ALU.add)

    def remod_stage():
        O.tt("dve", BRk, STr.v, Dc.v, ALU.mult)
        O.tt("pool", BIk, STi.v, Ds.v, ALU.mult)
        O.tt("dve", BRk, BRk, BIk, ALU.subtract)
        O.tt("pool", BIk, STr.v, Ds.v, ALU.mult)
        O.tt("dve", STi.v, STi.v, Dc.v, ALU.mult)
        O.tt("pool", BIk, BIk, STi.v, ALU.add)
        O.tt("pool", inj[:, 0, :], SRr[:, :, 63], Lam[:, 0, :], ALU.mult)
        O.tt("pool", itmp[:, 0, :], SRi[:, :, 63], Lam[:, 1, :], ALU.mult)
        O.tt("pool", inj[:, 0, :], inj[:, 0, :], itmp[:, 0, :], ALU.subtract)
        O.tt("pool", inj[:, 1, :], SRr[:, :, 63], Lam[:, 1, :], ALU.mult)
        O.tt("pool", itmp[:, 1, :], SRi[:, :, 63], Lam[:, 0, :], ALU.mult)
        O.tt("pool", inj[:, 1, :], inj[:, 1, :], itmp[:, 1, :], ALU.add)
        for c, SR in ((0, SRr), (1, SRi)):
            O.cp("pool", cur[c][:, :, 0:1], prv[c][:, :, 64:65])
            O.cp("act", cur[c][:, :, 1:65], SR)

    def out_stage(c4):
        bank = PS[7]
        for r in range(8):
            o = bank[:, r * 64:(r + 1) * 64]
            for j in range(r + 1):
                O.mm(o, KT[:, j, c4, :], xs[:, c4, r - j, :].k((xs.key, c4)), j == 0, False)
            for pair in range(4):
                gh = c4 * 4 + pair
                q = pair // 2
                o2 = bank[64 * q:64 * q + 64, r * 64:(r + 1) * 64]
                O.mm(o2, Em[0][:, r, gh // 2, gh % 2, :, :, :].rearrange("p a b c -> p (a b c)"),
                     cur[0][:, gh, 0:64], False, False)
                O.mm(o2, Em[1][:, r, gh // 2, gh % 2, :, :, :].rearrange("p a b c -> p (a b c)"),
                     cur[1][:, gh, 0:64], False, pair == 3)
        gx = gx2[c4 % 2]
        O.act(gx.v, bank.v, AF.Square)
        O.ts("dve", gx.v, gx.v, GELU_C1, ALU.mult, GELU_C0, ALU.add)
        O.tt("dve", gx.v, gx.v, bank.v, ALU.mult)
        O.act(gx.v, gx.v, AF.Sigmoid)
        O.tt("dve", ygs[:, c4, :].k((ygs.key, c4)).rearrange("p (m r) -> p r m", r=8),
             gx.v.rearrange("p (r m) -> p r m", r=8), bank.v.rearrange("p (r m) -> p r m", r=8), ALU.mult)

    def blend_stage():
        ygk = ygs.v.k(*[(ygs.key, c4) for c4 in range(4)])
        for i2 in range(2):
            i = 2 * sbi + i2
            a0, b0 = (2 * i2) * 128, (2 * i2 + 1) * 128
            yo = ygo[i2]
            O.ts("pool", ygt.v, ygk[:, :, b0:b0 + 128], sel[:, 1:2], ALU.mult)
            O.stt(yo.v, ygk[:, :, a0:a0 + 128], sel[:, 0:1], ygt.v, ALU.mult, ALU.add)
            O.dma("sp", yg_d[i].rearrange("p (c t) -> p c t", c=4), yo.v)
            if ygdbg is not None:
                O.dma("sp", ygdbg[i].rearrange("p (c t) -> p c t", c=4), yo.v, final=True)

    chunks = []
    for half in range(2):
        chunks.append(lambda half=half: snew(half))
        chunks.append(lambda half=half: demod(half))
    chunks.append(scan_stage)
    chunks.append(remod_stage)
    for c4 in range(4):
        chunks.append(lambda c4=c4: out_stage(c4))
    chunks.append(blend_stage)
    return chunks
def _phase_b1_b2_c(nc, S, O, sb, PS, dbg, dbg_out, norm_block, norm_work, load_mod, g):
    x_own, w_in, out = g["x_own"], g["w_in"], g["out"]
    identb, identf, sel = g["identb"], g["identf"], g["sel"]
    ckv_d, ckvT_d, kiT_d, yg_d, h_d = g["ckv_d"], g["ckvT_d"], g["kiT_d"], g["yg_d"], g["h_d"]
    att_d = nc.dram_tensor("att_scratch", [NOWN, 128, 512], BF16, kind="Internal").ap()
    nblk = DEV_NBLK or NOWN
    wview = lambda w, c0, c1: w[:, c0:c1].rearrange("(kt p) c -> p kt c", p=128)

    with contextlib.ExitStack() as pB:
        ckv_sb = sb("ckv_sb", [128, NB, 129], BF16, pB)
        ckvT = sb("ckvT", [128, L], BF16, pB)
        kiT2 = sb("kiT2", [128, L], BF16, pB)
        O.dma("sp", ckv_sb.v, ckv_d[:, :, :])
        O.dma("sp", ckvT.v, ckvT_d[:, :])
        O.dma("sp", kiT2.v, kiT_d[:, :])
        W = norm_work(pB, "b_")
        SH1, A1 = load_mod(pB, "b_", [0, 1])
        wq = sb("b_wq", [128, 8, 512], BF16, pB)
        wqi = sb("b_wqi", [128, 8, 260], BF16, pB)
        O.dma("pool", wq.v, wview(w_in, OFF["q"], OFF["q"] + 512))
        O.dma("pool", wqi[:, :, 0:256], wview(w_in, OFF["qi"], OFF["qi"] + 256))
        O.dma("pool", wqi[:, :, 256:260], wview(w_in, OFF["wi"], OFF["wi"] + 4))
        wukT = sb("b_wukT", [128, 4, 128], BF16, pB)
        wuv = sb("b_wuv", [128, 512], BF16, pB)
        O.dma("pool", wukT.v, g["w_ukT"][:, :, :])
        O.dma("pool", wuv.v, g["w_uv"][:, :])
        negi8 = sb("b_negi8", [128, 1024], BF16, pB)
        cm2 = sb("b_cm2", [128, 256], F32, pB)
        cm2p = sb("b_cm2p", [128, 256], F32, pB)
        pow2 = sb("b_pow2", [128, NBIS], F32, pB)
        O.dma("sp", negi8.v, g["k_negi8"][:, :])
        O.dma("sp", cm2.v, g["k_cm2"][:, :])
        O.dma("sp", cm2p.v, g["k_cm2p"][:, :])
        O.dma("sp", pow2.v, g["k_pow2"][:, :])
        uT1 = sb("b_uT1", [128, 8, 128], BF16, pB)
        qT = sb("b_qT", [128, 4, 128], BF16, pB)
        qlat2 = [sb(f"b_qlatT{n}", [128, 1024], BF16, pB) for n in range(2)]
        absw = sb("b_absw", [128, 4], F32, pB)
        sgn = sb("b_sgn", [128, 4], F32, pB)
        qis = sb("b_qis", [128, 4, 64], BF16, pB)
        qiT = sb("b_qiT", [128, 2, 128], BF16, pB)
        Dg = sb("b_Dg", [128, 4, 128], BF16, pB)
        Rsb = [sb(f"b_R{i}", [128, 4, 512], BF16, pB) for i in range(2)]
        Ibuf = sb("b_Ibuf", [128, L], F32, pB)
        nm2 = [sb(f"b_nm{n}", [128, L], BF16, pB) for n in range(2)]
        NW2 = sb("b_NW2", [128, NBIS], F32, pB)
        t256 = sb("b_t256", [128, 256], F32, pB)
        mx8 = sb("b_mx8", [128, 8], F32, pB)
        bs = sb("b_bs", [128, 8], F32, pB)
        WK = sb("b_WK", [128, NBIS], F32, pB)
        pT = [sb(f"b_pT{i}", [128, 1024], BF16, pB) for i in range(2)]
        rden = sb("b_rden", [128, 8], F32, pB)
        o_n = sb("b_on", [128, 8, 128], BF16, pB)
        onT = sb("b_onT", [128, 8, 128], BF16, pB)
        attT = [sb(f"b_attT{i}", [128, 4, 128], BF16, pB) for i in range(2)]
        col = lambda n: bs[:, n:n + 1].k(("bs", n))
        M1, M2, LO, W0, MID, CNT, TMP = [col(n) for n in range(7)]
        d_att = dbg_out("att", [NOWN, 128, 512], BF16) if "att" in dbg else None

        def prologue_indexer(i):
            nkt = 2 * i + 2
            Lq = nkt * 128
            qlatT = qlat2[i % 2]
            norm_block(x_own[i * 128:(i + 1) * 128, :], A1, SH1, uT1.v, W, i)
            for hp in range(4):
                for kt in range(8):
                    O.mm(PS[1][:, hp * 128:(hp + 1) * 128], wq[:, kt, hp * 128:(hp + 1) * 128], uT1[:, kt, :],
                         kt == 0, kt == 7)
            O.cp("act", qT.v, PS[1].v.rearrange("p (a b) -> p a b", a=4))
            for h in range(8):
                hp, hl = h // 2, h % 2
                O.mm(PS[2 + hl][:, hp * 128:(hp + 1) * 128], wukT[hl * 64:(hl + 1) * 64, hp, :],
                     qT[hl * 64:(hl + 1) * 64, hp, :], True, True)
            for hl in range(2):
                O.act(qlatT.v.rearrange("p (a b c) -> p a b c", a=4, b=2)[:, :, hl, :].k((qlatT.key, hl)),
                      PS[2 + hl].v.rearrange("p (a c) -> p a c", a=4), AF.Copy, scale=0.125)
            for kt in range(8):
                O.mm(PS[4][:, 0:260], uT1[:, kt, :], wqi[:, kt, :], kt == 0, kt == 7)
            O.act(absw.v, PS[4][:, 256:260], AF.Abs, scale=1.0 / 16)
            O.act(sgn.v, PS[4][:, 256:260], AF.Sign)
            O.tt("dve", qis.v, PS[4][:, 0:256].rearrange("p (h d) -> p h d", h=4),
                 absw.v.unsqueeze(2).broadcast_to([128, 4, 64]), ALU.mult)
            O.tt("pool", Dg.v, identf.v.unsqueeze(1).broadcast_to([128, 4, 128]),
                 sgn.v.unsqueeze(2).broadcast_to([128, 4, 128]), ALU.mult)
            pst = PS[0].v.bitcast(BF16)
            for hp2 in range(2):
                O.tr(pst[:, hp2 * 128:(hp2 + 1) * 128], qis[:, 2 * hp2:2 * hp2 + 2, :].rearrange("p a b -> p (a b)"),
                     identb.v)
            O.cp("act", qiT.v, pst[:, 0:256].rearrange("p (a b) -> p a b", a=2))
            ngr = (Lq + 511) // 512
            Ik = []
            for kg in range(ngr):
                nk = min(512, Lq - kg * 512)
                k0 = kg * 512
                R = Rsb[kg % 2]
                for h in range(4):
                    hl = h % 2
                    O.mm(PS[1 + h][:, 0:nk], qiT[hl * 64:(hl + 1) * 64, h // 2, :], kiT2[hl * 64:(hl + 1) * 64, k0:k0 + nk],
                         True, True)
                    O.act(R[:, h, 0:nk].k((R.key, h)), PS[1 + h][:, 0:nk], AF.Relu)
                ib = PS[5 + kg % 2]
                for h in range(4):
                    O.mm(ib[:, 0:nk], Dg[:, h, :], R[:, h, 0:nk].k((R.key, h)), h == 0, h == 3)
                Ik.append(("I", kg))
                if kg == ngr - 1:
                    if nk > 256:
                        O.cp("act", Ibuf[:, k0:k0 + nk - 256].k(("I", kg)), ib[:, 0:nk - 256])
                    O.tt("dve", Ibuf[:, Lq - 256:Lq].k(("I", kg)), ib[:, nk - 256:nk], cm2.v, ALU.add)
                    O.tt("dve", t256.v, ib[:, nk - 256:nk], cm2p.v, ALU.add)
                    O.reduce(M2, t256.v, ALU.min)
                else:
                    O.cp("act", Ibuf[:, k0:k0 + nk].k(("I", kg)), ib[:, 0:nk])
            return Ik

        def bisect_chunks(i, Ik):
            Lq = (2 * i + 2) * 128
            Iall = Ibuf[:, 0:Lq].k(*Ik)
            nmv = nm2[i % 2]
            ch = []

            def init():
                O.max8(mx8.v, Iall)
                if Lq > 256:
                    O.reduce(M1, Ibuf[:, 0:Lq - 256].k(*Ik), ALU.min)
                    O.tt("dve", LO, M1, M2, ALU.min)
                else:
                    O.cp("dve", LO, M2)
                O.tt("dve", W0, mx8[:, 0:1], LO, ALU.subtract)
                O.ts("dve", WK.v, pow2.v, W0, ALU.mult)
                O.ts("dve", NW2[:, 0:NBIS - 1], WK[:, 1:NBIS], -1.0, ALU.mult)
                O.ts("dve", NW2[:, NBIS - 1:NBIS], WK[:, NBIS - 1:NBIS], -1.0, ALU.mult)
                O.stt(NW2[:, NBIS - 1:NBIS], W0, -(2.0 ** -20), NW2[:, NBIS - 1:NBIS], ALU.mult, ALU.add)
                O.tt("dve", MID, LO, WK[:, 0:1], ALU.add)
            ch.append(init)

            def it(k):
                O.ts("dve", nmv[:, 0:Lq], Iall, MID, ALU.is_ge, 0.0, ALU.add, accum=CNT)
                O.stt(TMP, CNT, TOPK - 0.5, WK[:, k:k + 1], ALU.is_ge, ALU.mult)
                O.stt(MID, TMP, NW2[:, k:k + 1], MID, ALU.add, ALU.add)
            for k in range(NBIS):
                ch.append(lambda k=k: it(k))
            ch.append(lambda: O.ts("dve", nmv[:, 0:Lq], Iall, MID, ALU.is_lt))
            return ch

        def attention_chunks(i):
            nkt = 2 * i + 2
            qlatT = qlat2[i % 2]
            qlk = [(qlatT.key, 0), (qlatT.key, 1)]
            nmv = nm2[i % 2]
            ch = []

            def tile(kt):
                lb = (PS[1], PS[2]) if kt % 2 == 0 else (PS[3], PS[4])
                p = pT[kt % 2]
                for half in range(2):
                    O.mm(lb[half].v, ckvT[:, kt * 128:(kt + 1) * 128], qlatT[:, half * 512:(half + 1) * 512].k(*qlk),
                         True, False)
                    O.mm(lb[half].v, nmv[:, kt * 128:(kt + 1) * 128], negi8[:, half * 512:(half + 1) * 512], False, True)
                    O.act(p[:, half * 512:(half + 1) * 512].k((p.key, half)), lb[half].v, AF.Exp)
                for h in range(8):
                    bank, off = PS[5 + h // 3], (h % 3) * 129
                    O.mm(bank[:, off:off + 129], p[:, h * 128:(h + 1) * 128].k((p.key, h // 4)), ckv_sb[:, kt, :],
                         kt == 0 and h % 3 == 0, kt == nkt - 1)
            for kt in range(nkt):
                ch.append(lambda kt=kt: tile(kt))
            return ch

        def epilogue(i):
            pst = PS[0].v.bitcast(BF16)
            for b3 in range(3):
                nh = 3 if b3 < 2 else 2
                v3 = PS[5 + b3][:, 0:nh * 129].rearrange("p (a b) -> p a b", a=nh)
                O.recip(rden[:, 3 * b3:3 * b3 + nh].k(("rden", b3)), v3[:, :, 128])
                O.tt("dve", o_n[:, 3 * b3:3 * b3 + nh, :].k(("on", b3)), v3[:, :, 0:128],
                     rden[:, 3 * b3:3 * b3 + nh].k(("rden", b3)).unsqueeze(2).broadcast_to([128, nh, 128]), ALU.mult)
            for h in range(8):
                O.tr(pst[:, h * 128:(h + 1) * 128], o_n[:, h, :].k(("on", h // 3)), identb.v)
            O.cp("act", onT.v, pst.rearrange("p (a b) -> p a b", a=8))
            for h in range(8):
                hp, hl = h // 2, h % 2
                O.mm(PS[1][hl * 64:(hl + 1) * 64, hp * 128:(hp + 1) * 128], wuv[:, h * 64:(h + 1) * 64], onT[:, h, :],
                     True, True)
            at = attT[i % 2]
            O.cp("act", at.v, PS[1].v.rearrange("p (a b) -> p a b", a=4))
            O.dma("sp", att_d[i].rearrange("p (a b) -> p a b", a=4), at.v)
            if d_att is not None:
                O.dma("sp", d_att[i].rearrange("p (a b) -> p a b", a=4), at.v, final=True)

        def merge(ca, cb):
            na, nb = len(ca), len(cb)
            ia = ib = 0
            while ia < na or ib < nb:
                if ib >= nb or (ia < na and ia * nb <= ib * na):
                    ca[ia]()
                    ia += 1
                else:
                    cb[ib]()
                    ib += 1

        Ik0 = prologue_indexer(0)
        for c in bisect_chunks(0, Ik0):
            c()
        for i in range(nblk):
            bc = []
            if i + 1 < nblk:
                Ik1 = prologue_indexer(i + 1)
                bc = bisect_chunks(i + 1, Ik1)
            merge(bc, attention_chunks(i))
            epilogue(i)
        S.barrier()
        S.emit()
    if "stopB1" in dbg:
        return

    ngrp = (nblk + 3) // 4
    mod_d, final_g = g["mod_d"], g["final_g"]
    with contextlib.ExitStack() as pC:
        W = norm_work(pC, "c_")
        SH1, A1, G1 = load_mod(pC, "c_", [0, 1, 2])
        wg = sb("c_wg", [128, 8, 2048], BF16, pC)
        O.dma("pool", wg[:, :, 0:1024], wview(w_in, OFF["ga"], OFF["ga"] + 1024))
        O.dma("pool", wg[:, :, 1024:2048], wview(w_in, OFF["gb"], OFF["gb"] + 1024))
        wglu = sb("c_wglu", [128, 4, 2048], BF16, pC)
        O.dma("pool", wglu.v, g["w_glu"].rearrange("(kt p) c -> p kt c", p=128))
        bglu = sb("c_bglu", [128, 16], F32, pC)
        O.dma("sp", bglu.v, g["b_glu"].rearrange("(ft p) -> p ft", p=128))
        wap = sb("c_wap", [128, 4, 1024], BF16, pC)
        O.dma("pool", wap.v, g["w_ap"].rearrange("(kt p) c -> p kt c", p=128))
        wout = sb("c_wout", [128, 8, 1024], BF16, pC)
        O.dma("pool", wout.v, g["w_out"].rearrange("(kt p) c -> p kt c", p=128))
        uT4 = sb("c_uT4", [128, 8, 512], BF16, pC)
        sg = [sb(f"c_sg{w}", [128, 8, 512], BF16, pC) for w in range(2)]
        ygl = sb("c_ygl", [128, 4, 512], BF16, pC)
        attl = sb("c_attl", [128, 4, 512], BF16, pC)
        sgt = sb("c_sgt", [128, 512], BF16, pC)
        ys = sb("c_ys", [128, 512], BF16, pC)
        m1 = sb("c_m1", [128, 8, 512], BF16, pC)
        mT = sb("c_mT", [128, 8, 512], BF16, pC)
        ta = sb("c_ta", [128, 512], F32, pC)
        xr = [sb(f"c_xr{n}", [128, D], F32, pC) for n in range(2)]
        hout = [sb(f"c_hout{n}", [128, D], F32, pC) for n in range(2)]
        d_h = dbg_out("h", [NOWN * 128, D]) if "h" in dbg else None
        for gi in range(ngrp):
            for bl in range(4):
                i = 4 * gi + bl
                norm_block(x_own[i * 128:(i + 1) * 128, :], A1, SH1, uT4[:, :, bl * 128:(bl + 1) * 128].k(("uT4", bl)),
                           W, i)
                O.dma("sp", ygl[:, :, bl * 128:(bl + 1) * 128].k(("ygl", bl)), yg_d[i].rearrange("p (c t) -> p c t", c=4))
                O.dma("sp", attl[:, :, bl * 128:(bl + 1) * 128].k(("attl", bl)),
                      att_d[i].rearrange("p (c t) -> p c t", c=4))
            uk = [("uT4", bl) for bl in range(4)]
            yk = [("ygl", bl) for bl in range(4)]
            ak = [("attl", bl) for bl in range(4)]
            for w in range(2):
                for ft in range(8):
                    bank = PS[1 + ft % 2]
                    for kt in range(8):
                        O.mm(bank.v, wg[:, kt, w * 1024 + ft * 128:w * 1024 + (ft + 1) * 128], uT4[:, kt, :].k(*uk),
                             kt == 0, kt == 7)
                    O.act(sg[w][:, ft, :].k((sg[w].key, ft)), bank.v, AF.Sigmoid)
            for ft in range(8):
                for c4 in range(4):
                    O.mm(PS[3].v, wglu[:, c4, ft * 128:(ft + 1) * 128], ygl[:, c4, :].k(*yk), c4 == 0, c4 == 3)
                for c4 in range(4):
                    O.mm(PS[4].v, wglu[:, c4, 1024 + ft * 128:1024 + (ft + 1) * 128], ygl[:, c4, :].k(*yk),
                         c4 == 0, c4 == 3)
                O.act(sgt.v, PS[4].v, AF.Sigmoid, bias=bglu[:, 8 + ft:9 + ft])
                O.stt(ys.v, PS[3].v, bglu[:, ft:ft + 1], sgt.v, ALU.add, ALU.mult)
                O.tt("pool", m1[:, ft, :].k(("m1", ft)), ys.v, sg[0][:, ft, :].k((sg[0].key, ft)), ALU.mult)
            for ft in range(8):
                bank = PS[5 + ft % 2]
                for hp in range(4):
                    O.mm(bank.v, wap[:, hp, ft * 128:(ft + 1) * 128], attl[:, hp, :].k(*ak), hp == 0, hp == 3)
                O.tt("dve", ta.v, bank.v, sg[1][:, ft, :].k((sg[1].key, ft)), ALU.mult)
                O.tt("pool", mT[:, ft, :].k(("mT", ft)), ta.v, m1[:, ft, :].k(("m1", ft)), ALU.add)
            mk = [("mT", ft) for ft in range(8)]
            for bl in range(4):
                i = 4 * gi + bl
                xt = xr[i % 2]
                O.dma("sp", xt.v, x_own[i * 128:(i + 1) * 128, :])
                ho = hout[i % 2]
                for dn in range(2):
                    bank = PS[1 + dn]
                    for ft in range(8):
                        O.mm(bank.v, mT[:, ft, bl * 128:(bl + 1) * 128].k(*mk), wout[:, ft, dn * 512:(dn + 1) * 512],
                             ft == 0, ft == 7)
                    O.tt("dve", ta.v, bank.v, G1[:, dn * 512:(dn + 1) * 512], ALU.mult)
                    O.tt("pool", ho[:, dn * 512:(dn + 1) * 512].k((ho.key, dn)), ta.v, xt[:, dn * 512:(dn + 1) * 512],
                         ALU.add)
                hk = ho.v.k((ho.key, 0), (ho.key, 1))
                O.dma("sp", h_d[i * 128:(i + 1) * 128, :], hk)
                if d_h is not None:
                    O.dma("sp", d_h[i * 128:(i + 1) * 128, :], hk, final=True)
        S.barrier()
        S.emit()
    if "stopB2" in dbg:
        return

    with contextlib.ExitStack() as pD:
        W = norm_work(pD, "d_", nbuf=1)
        SH2, A2, G2 = load_mod(pD, "d_", [3, 4, 5])
        fgb = sb("d_fgb", [128, D], F32, pD)
        O.dma("sp", fgb.v, final_g.partition_broadcast(128))
        wfg = sb("d_wfg", [128, 8, FF], BF16, pD)
        wfu = sb("d_wfu", [128, 8, FF], BF16, pD)
        wfd = sb("d_wfd", [128, FF // 128, D], BF16, pD)
        for kt0 in range(0, 8, 4):
            O.dma("pool", wfg[:, kt0:kt0 + 4, :], g["w_fg"][kt0 * 128:(kt0 + 4) * 128, :].rearrange("(kt p) c -> p kt c", p=128))
            O.dma("pool", wfu[:, kt0:kt0 + 4, :], g["w_fu"][kt0 * 128:(kt0 + 4) * 128, :].rearrange("(kt p) c -> p kt c", p=128))
        for f0 in range(0, 22, 11):
            O.dma("pool", wfd[:, f0:f0 + 11, :], g["w_fd"][f0 * 128:(f0 + 11) * 128, :].rearrange("(kt p) c -> p kt c", p=128))
        u2T = sb("d_u2T", [128, 8, 512], BF16, pD)
        hid = sb("d_hid", [128, FF // 128, 512], BF16, pD)
        sil = sb("d_sil", [128, 512], BF16, pD)
        ta = sb("d_ta", [128, 512], F32, pD)
        hr = W["x"][0]
        h2 = sb("d_h2", [128, D], F32, pD)
        ot = [sb(f"d_ot{n}", [128, D], F32, pD) for n in range(1)]
        st = sb("d_st", [128, 4], F32, pD)
        for gi in range(ngrp):
            for bl in range(4):
                i = 4 * gi + bl
                norm_block(h_d[i * 128:(i + 1) * 128, :], A2, SH2, u2T[:, :, bl * 128:(bl + 1) * 128].k(("u2T", bl)), W, i)
            uk = [("u2T", bl) for bl in range(4)]
            for ft in range(FF // 128):
                bg, bu = PS[1 + ft % 2], PS[3 + ft % 2]
                for kt in range(8):
                    O.mm(bg.v, wfg[:, kt, ft * 128:(ft + 1) * 128], u2T[:, kt, :].k(*uk), kt == 0, kt == 7)
                for kt in range(8):
                    O.mm(bu.v, wfu[:, kt, ft * 128:(ft + 1) * 128], u2T[:, kt, :].k(*uk), kt == 0, kt == 7)
                O.act(sil.v, bg.v, AF.Silu)
                O.tt("dve", hid[:, ft, :].k(("hid", ft)), bu.v, sil.v, ALU.mult)
            hk = [("hid", ft) for ft in range(FF // 128)]
            for bl in range(4):
                i = 4 * gi + bl
                O.dma("sp", hr.v, h_d[i * 128:(i + 1) * 128, :])
                for dn in range(2):
                    bank = PS[5 + dn]
                    for ft in range(FF // 128):
                        O.mm(bank.v, hid[:, ft, bl * 128:(bl + 1) * 128].k(*hk), wfd[:, ft, dn * 512:(dn + 1) * 512],
                             ft == 0, ft == FF // 128 - 1)
                    O.tt("dve", ta.v, bank.v, G2[:, dn * 512:(dn + 1) * 512], ALU.mult)
                    O.tt("pool", h2[:, dn * 512:(dn + 1) * 512].k(("h2", dn)), ta.v, hr[:, dn * 512:(dn + 1) * 512],
                         ALU.add)
                h2k = h2.v.k(("h2", 0), ("h2", 1))
                o_t = ot[0]
                O.act(o_t.v, h2k, AF.Square, accum=st[:, 0:1].k("st0"))
                O.ts("dve", st[:, 1:2].k("st1"), st[:, 0:1].k("st0"), 1.0 / D, ALU.mult, EPS, ALU.add)
                O.act(st[:, 1:2].k("st1"), st[:, 1:2].k("st1"), AF.Sqrt)
                O.recip(st[:, 2:3].k("st2"), st[:, 1:2].k("st1"))
                O.stt(o_t.v, h2k, st[:, 2:3].k("st2"), fgb.v, ALU.mult, ALU.mult)
                O.dma("sp", out[i * 128:(i + 1) * 128, :], o_t.v, final=True)
        S.barrier()
        S.emit()
def _consts(p):
    bf = ml_dtypes.bfloat16
    ident = np.eye(128, dtype=np.float32)
    negi8 = np.tile(-BIGM * ident, (1, 8)).astype(bf)
    t = np.arange(128)[:, None]
    s = np.arange(128)[None, :]
    tri = np.where(s <= t, 0.0, NEG).astype(np.float32)
    full = np.full((128, 128), NEG, np.float32)
    zero = np.zeros((128, 128), np.float32)
    cm2 = np.concatenate([tri, full], 1) if p == 0 else np.concatenate([zero, tri], 1)
    cm2p = np.where(cm2 < 0, 1.0e30, 0.0).astype(np.float32)
    selv = np.zeros((128, 2), np.float32)
    selv[:, p] = 1.0
    pp = np.arange(128)
    maskf = np.stack([((pp // 32) % 2 == w) & ((pp // 16) % 2 == g2) for w in range(2) for g2 in range(2)],
                     1).astype(np.float32)
    masks = np.stack([(pp // 64 == 0), (pp // 64 == 1)], 1).astype(np.float32)
    pow2 = np.tile((0.5 ** np.arange(1, NBIS + 1))[None, :], (128, 1)).astype(np.float32)
    jv = np.tile(np.arange(9, dtype=np.float32)[None, :], (128, 1))
    iv = np.tile(np.arange(64, dtype=np.float32)[None, :], (128, 1))
    return dict(k_jv=jv, k_iv=iv, k_identb=ident.astype(bf), k_identf=ident, k_negi8=negi8, k_cm2=cm2, k_cm2p=cm2p,
                k_sel=selv, k_maskf=maskf, k_masks=masks, k_pow2=pow2)


def make_in_maps(inputs, cores=range(8)):
    f = lambda a: np.ascontiguousarray(np.asarray(a, dtype=np.float32))
    c_ = np.ascontiguousarray
    x = f(inputs["x"])
    shared = dict(
        w_mod=f(inputs["w_mod"])[0], b_mod=f(inputs["b_mod"])[0], norm1_g=f(inputs["norm1_g"])[0],
        w_in=f(inputs["w_in"])[0],
        w_ssm_glu=f(inputs["w_ssm_glu"])[0], b_ssm_glu=f(inputs["b_ssm_glu"])[0],
        kv_norm_g=f(inputs["kv_norm_g"])[0], idx_k_norm_g=f(inputs["idx_k_norm_g"])[0],
        w_ukT=c_(f(inputs["w_uk"])[0].reshape(128, 4, 2, 64).transpose(2, 3, 1, 0).reshape(128, 4, 128)),
        w_uv=f(inputs["w_uv"])[0].reshape(128, 512),
        w_attn_proj=f(inputs["w_attn_proj"])[0], w_out=f(inputs["w_out"])[0], norm2_g=f(inputs["norm2_g"])[0],
        w_ffn_gate=f(inputs["w_ffn_gate"])[0], w_ffn_up=f(inputs["w_ffn_up"])[0],
        w_ffn_down=f(inputs["w_ffn_down"])[0], final_g=f(inputs["final_g"]),
    )
    are, aim, ldt = f(inputs["ssm_a_re"])[0], f(inputs["ssm_a_im"])[0], f(inputs["ssm_log_dt"])[0]
    bre, bim = f(inputs["ssm_b_re"])[0], f(inputs["ssm_b_im"])[0]
    cre, cim = f(inputs["ssm_c_re"])[0], f(inputs["ssm_c_im"])[0]
    sl_a = lambda a: c_(a.reshape(16, 2, 64).transpose(1, 2, 0).reshape(128, 16))
    fl_a = lambda a: c_(np.broadcast_to(a.reshape(4, 8, 64).transpose(1, 0, 2)[:, None], (8, 16, 4, 64)).reshape(128, 256))
    shared.update(
        sl_are=sl_a(are), sl_aim=sl_a(aim),
        sl_ldt=c_(np.broadcast_to(ldt.reshape(16, 2).T[:, None, :], (2, 64, 16)).reshape(128, 16)),
        fl_are=fl_a(are), fl_aim=fl_a(aim),
        fl_ldt=c_(np.broadcast_to(ldt.reshape(4, 8).T[:, None, :, None], (8, 16, 4, 64)).reshape(128, 256)),
        sl_bre=c_(bre.reshape(16, 2, 64, 16).transpose(1, 2, 0, 3).reshape(128, 16, 16)),
        sl_bim=c_(bim.reshape(16, 2, 64, 16).transpose(1, 2, 0, 3).reshape(128, 16, 16)),
        sl_cre=c_(cre.reshape(16, 2, 16, 64).transpose(1, 3, 0, 2).reshape(128, 16, 16)),
        sl_cim=c_(cim.reshape(16, 2, 16, 64).transpose(1, 3, 0, 2).reshape(128, 16, 16)),
        fl_bre=c_(bre.reshape(4, 8, 64, 16).transpose(1, 3, 0, 2).reshape(128, 256)),
        fl_bim=c_(bim.reshape(4, 8, 64, 16).transpose(1, 3, 0, 2).reshape(128, 256)),
        dsk=c_(f(inputs["ssm_d"])[0].reshape(4, 128).T),
    )
    maps = []
    for core in cores:
        b, p = core // 2, core % 2
        xb = x[b]
        x_own = np.ascontiguousarray(xb.reshape(NB, 128, D)[p::2].reshape(NOWN * 128, D))
        m = dict(shared)
        m.update(x_all=xb, x_own=x_own, c=f(inputs["c"])[b])
        m.update(_consts(p))
        maps.append(m)
    return maps


def kernel(**inputs):
    nc = build()
    maps = make_in_maps(inputs)
    res = run_bass_kernel_spmd(nc, maps, core_ids=list(range(8)))
    x = np.asarray(inputs["x"])
    outp = np.empty(x.shape, np.float32)
    for core in range(8):
        b, p = core // 2, core % 2
        o = np.asarray(res.results[core]["out"]).reshape(NOWN, 128, D)
        outp[b].reshape(NB, 128, D)[p::2] = o
    return outp
```

```python
import contextlib
import math
import numpy as np
import ml_dtypes
import concourse.bass as bass
import concourse.mybir as mybir
from concourse.bass_utils import run_bass_kernel_spmd

F32 = mybir.dt.float32
BF16 = mybir.dt.bfloat16
I32 = mybir.dt.int32
AF = mybir.ActivationFunctionType
ALU = mybir.AluOpType
AX = mybir.AxisListType

D = 1024
L = 8192
NB = 64
NOWN = 32
TC = 8
OFF = dict(xs=0, q=512, ckv=1024, qi=1152, ki=1408, wi=1472, ga=1476, gb=2500)
FF = 2816
EPS = 1e-6
NEG = -1.0e30
BIGM = 30000.0
NBIS = 14
TOPK = 256
DEV_NBLK = None
DEV_STAGE = 0
DEV_SKIP_A = False
SELF_SYNC = True
NO_SELF_SYNC = ()


class _Op:
    __slots__ = ("eng", "fn", "deps", "inc", "tok", "kind", "uid")


class Sched:
    NDMA = 8

    def __init__(self, nc, es, self_sync=SELF_SYNC):
        self.nc = nc
        self.E = {"pe": nc.tensor, "act": nc.scalar, "dve": nc.vector, "pool": nc.gpsimd, "sp": nc.sync}
        self.sem = {k: es.enter_context(nc.semaphore("sem_" + k)) for k in self.E}
        self.dsem = {q: [es.enter_context(nc.semaphore(f"dsem_{q}_{i}")) for i in range(self.NDMA)]
                     for q in ("sp", "pool", "act")}
        self.cnt = {k: 0 for k in self.E}
        self.dcnt = {q: 0 for q in self.dsem}
        self.seen = {k: {} for k in self.E}
        self.lw = {}
        self.lr = {}
        self.ops = []
        self.self_sync = self_sync
        self.uid = 0
        self.final = []
        self.last = {}
        self.pend = []

    def _add(self, o, reads, writes):
        deps = {}
        for b in reads:
            w = self.lw.get(b)
            if w is not None:
                deps[w.uid] = w
        for b in writes:
            w = self.lw.get(b)
            if w is not None:
                deps[w.uid] = w
            for r in self.lr.get(b, {}).values():
                deps[r.uid] = r
        out = []
        for d in deps.values():
            if d is o:
                continue
            if d.kind == "c" and o.kind == "c" and d.eng == o.eng:
                if o.eng == "pe" or o.eng in NO_SELF_SYNC:
                    continue
            out.append(d)
            d.inc = True
        o.deps = out
        for b in reads:
            key = o.eng if o.kind == "c" else ("d", o.uid)
            self.lr.setdefault(b, {})[key] = o
        for b in writes:
            self.lw[b] = o
            self.lr[b] = {}
        self.ops.append(o)
        if o.kind == "c":
            self.last[o.eng] = o
        else:
            self.pend.append(o)
        return o

    def op(self, eng, fn, reads=(), writes=()):
        o = _Op()
        o.eng, o.fn, o.kind, o.inc, o.tok = eng, fn, "c", False, None
        self.uid += 1
        o.uid = self.uid
        return self._add(o, reads, writes)

    def dma(self, q, fn, reads=(), writes=(), final=False):
        o = _Op()
        o.eng, o.fn, o.kind, o.inc, o.tok = q, fn, "d", True, None
        self.uid += 1
        o.uid = self.uid
        if final:
            self.final.append(o)
        return self._add(o, reads, writes)

    def _wait(self, eng, tok):
        sem, val = tok
        if self.seen[eng].get(id(sem), 0) < val:
            self.E[eng].wait_ge(sem, val)
            self.seen[eng][id(sem)] = val

    def _wait_all(self, eng, toks):
        best = {}
        for sem, val in toks:
            if best.get(id(sem), (None, 0))[1] < val:
                best[id(sem)] = (sem, val)
        for tok in best.values():
            self._wait(eng, tok)

    def barrier(self):
        lasts = [o for o in self.last.values()] + list(self.pend)
        for d in lasts:
            d.inc = True
        for eng in self.E:
            o = _Op()
            o.eng, o.fn, o.kind, o.inc, o.tok = eng, None, "w", False, None
            self.uid += 1
            o.uid = self.uid
            o.deps = [d for d in lasts if not (d.kind == "c" and d.eng == eng)]
            self.ops.append(o)
        self.pend = []
        self.lw = {}
        self.lr = {}

    def emit(self):
        for o in self.ops:
            e = self.E[o.eng]
            self._wait_all(o.eng, [d.tok for d in o.deps])
            if o.kind == "w":
                continue
            if o.kind == "d":
                k = self.dcnt[o.eng]
                self.dcnt[o.eng] += 1
                slot, gen = k % self.NDMA, k // self.NDMA
                sem = self.dsem[o.eng][slot]
                if gen > 0:
                    self._wait(o.eng, (sem, 16 * gen))
                o.fn(e).then_inc(sem, 16)
                o.tok = (sem, 16 * (gen + 1))
            else:
                inst = o.fn(e)
                if o.inc:
                    self.cnt[o.eng] += 1
                    inst.then_inc(self.sem[o.eng], 1)
                    o.tok = (self.sem[o.eng], self.cnt[o.eng])
        for o in self.final:
            self._wait("sp", o.tok)
        self.ops = []


class Vw:
    __slots__ = ("ap", "keys")

    def __init__(self, ap, keys):
        self.ap = ap
        self.keys = keys if isinstance(keys, list) else [keys]

    def k(self, *keys):
        return Vw(self.ap, list(keys))

    def __getitem__(self, idx):
        return Vw(self.ap[idx], self.keys)

    def rearrange(self, *a, **kw):
        return Vw(self.ap.rearrange(*a, **kw), self.keys)

    def bitcast(self, dt):
        return Vw(self.ap.bitcast(dt), self.keys)

    def broadcast_to(self, shape):
        return Vw(self.ap.broadcast_to(list(shape)), self.keys)

    def unsqueeze(self, ax):
        return Vw(self.ap.unsqueeze(ax), self.keys)


class Tl:
    def __init__(self, t, key):
        self.t = t
        self.key = key

    def __getitem__(self, idx):
        return Vw(self.t[idx], self.key)

    @property
    def v(self):
        return Vw(self.t[:], self.key)


def _keys(*vs):
    out = []
    for v in vs:
        if isinstance(v, Vw):
            out.extend(v.keys)
    return out


def _ap(v):
    return v.ap if isinstance(v, Vw) else v


class Ops:
    def __init__(self, S):
        self.S = S

    def mm(self, out, lhsT, rhs, start, stop):
        self.S.op("pe", lambda e: e.matmul(out.ap, lhsT.ap, rhs.ap, start=start, stop=stop, skip_group_check=True),
                  reads=_keys(lhsT, rhs), writes=_keys(out))

    def tr(self, out, in_, ident):
        self.S.op("pe", lambda e: e.transpose(out.ap, in_.ap, ident.ap), reads=_keys(in_, ident), writes=_keys(out))

    def act(self, out, in_, func, bias=None, scale=None, accum=None, eng="act"):
        kw = {}
        if bias is not None:
            kw["bias"] = _ap(bias)
        if scale is not None:
            kw["scale"] = _ap(scale)
        if accum is not None:
            kw["accum_out"] = accum.ap
        self.S.op("act", lambda e: e.activation(out=out.ap, in_=in_.ap, func=func, **kw),
                  reads=_keys(in_, bias, scale), writes=_keys(out, accum))

    def ts(self, eng, out, in0, s1, op0, s2=None, op1=None, accum=None):
        kw = dict(scalar1=_ap(s1), scalar2=_ap(s2) if s2 is not None else None, op0=op0)
        if op1 is not None:
            kw["op1"] = op1
        if accum is not None:
            kw["accum_out"] = accum.ap
        self.S.op(eng, lambda e: e.tensor_scalar(out=out.ap, in0=in0.ap, **kw),
                  reads=_keys(in0, s1, s2), writes=_keys(out, accum))

    def tt(self, eng, out, in0, in1, op):
        self.S.op(eng, lambda e: e.tensor_tensor(out=out.ap, in0=in0.ap, in1=in1.ap, op=op),
                  reads=_keys(in0, in1), writes=_keys(out))

    def stt(self, out, in0, scalar, in1, op0, op1, eng="dve"):
        self.S.op(eng, lambda e: e.scalar_tensor_tensor(out=out.ap, in0=in0.ap, scalar=_ap(scalar), in1=in1.ap,
                                                        op0=op0, op1=op1),
                  reads=_keys(in0, scalar, in1), writes=_keys(out))

    def cp(self, eng, out, in_):
        if eng == "act":
            self.S.op("act", lambda e: e.activation(out=out.ap, in_=in_.ap, func=AF.Copy),
                      reads=_keys(in_), writes=_keys(out))
        else:
            self.S.op(eng, lambda e: e.tensor_copy(out=out.ap, in_=in_.ap), reads=_keys(in_), writes=_keys(out))

    def memset(self, eng, out, val):
        self.S.op(eng, lambda e: e.memset(out.ap, val), writes=_keys(out))

    def recip(self, out, in_):
        self.S.op("dve", lambda e: e.reciprocal(out=out.ap, in_=in_.ap), reads=_keys(in_), writes=_keys(out))

    def scan(self, out, d0, d1, init, op0, op1):
        self.S.op("dve", lambda e: e.tensor_tensor_scan(out=out.ap, data0=d0.ap, data1=d1.ap, initial=_ap(init),
                                                        op0=op0, op1=op1),
                  reads=_keys(d0, d1, init), writes=_keys(out))

    def max8(self, out, in_):
        self.S.op("dve", lambda e: e.max(out=out.ap, in_=in_.ap), reads=_keys(in_), writes=_keys(out))

    def reduce(self, out, in_, op, axis=AX.X):
        self.S.op("dve", lambda e: e.tensor_reduce(out=out.ap, in_=in_.ap, axis=axis, op=op),
                  reads=_keys(in_), writes=_keys(out))

    def dma(self, q, out, in_, final=False):
        o_ap, i_ap = _ap(out), _ap(in_)
        self.S.dma(q, lambda e: e.dma_start(out=o_ap, in_=i_ap), reads=_keys(in_), writes=_keys(out), final=final)


def build(dbg=None):
    nc = bass.Bass("TRN2", target_bir_lowering=False)
    es = contextlib.ExitStack()
    with es:
        _build(nc, es, dbg or set())
    return nc


def _dram_in(nc, name, shape, dt=F32):
    return nc.dram_tensor(name, list(shape), dt, kind="ExternalInput").ap()


GELU_C0 = 1.5957691216057308
GELU_C1 = 1.5957691216057308 * 0.044715


def _build(nc, es, dbg):
    S = Sched(nc, es)
    O = Ops(S)
    x_all = _dram_in(nc, "x_all", [L, D])
    x_own = _dram_in(nc, "x_own", [NOWN * 128, D])
    c_in = _dram_in(nc, "c", [D])
    w_mod = _dram_in(nc, "w_mod", [D, 6 * D])
    b_mod = _dram_in(nc, "b_mod", [6 * D])
    norm1_g = _dram_in(nc, "norm1_g", [D])
    w_in = _dram_in(nc, "w_in", [D, 3524])
    w_glu = _dram_in(nc, "w_ssm_glu", [512, 2048])
    b_glu = _dram_in(nc, "b_ssm_glu", [2048])
    kv_g = _dram_in(nc, "kv_norm_g", [128])
    ik_g = _dram_in(nc, "idx_k_norm_g", [64])
    w_ukT = _dram_in(nc, "w_ukT", [128, 4, 128])
    w_uv = _dram_in(nc, "w_uv", [128, 512])
    w_ap = _dram_in(nc, "w_attn_proj", [512, D])
    w_out = _dram_in(nc, "w_out", [D, D])
    norm2_g = _dram_in(nc, "norm2_g", [D])
    w_fg = _dram_in(nc, "w_ffn_gate", [D, FF])
    w_fu = _dram_in(nc, "w_ffn_up", [D, FF])
    w_fd = _dram_in(nc, "w_ffn_down", [FF, D])
    final_g = _dram_in(nc, "final_g", [D])
    sl_are = _dram_in(nc, "sl_are", [128, 16])
    sl_aim = _dram_in(nc, "sl_aim", [128, 16])
    sl_ldt = _dram_in(nc, "sl_ldt", [128, 16])
    fl_are = _dram_in(nc, "fl_are", [128, 256])
    fl_aim = _dram_in(nc, "fl_aim", [128, 256])
    fl_ldt = _dram_in(nc, "fl_ldt", [128, 256])
    sl_bre = _dram_in(nc, "sl_bre", [128, 16, 16])
    sl_bim = _dram_in(nc, "sl_bim", [128, 16, 16])
    sl_cre = _dram_in(nc, "sl_cre", [128, 16, 16])
    sl_cim = _dram_in(nc, "sl_cim", [128, 16, 16])
    fl_bre = _dram_in(nc, "fl_bre", [128, 256])
    fl_bim = _dram_in(nc, "fl_bim", [128, 256])
    d_dsk = _dram_in(nc, "dsk", [128, 4])
    k_identb = _dram_in(nc, "k_identb", [128, 128], BF16)
    k_identf = _dram_in(nc, "k_identf", [128, 128])
    k_negi8 = _dram_in(nc, "k_negi8", [128, 1024], BF16)
    k_cm2 = _dram_in(nc, "k_cm2", [128, 256])
    k_cm2p = _dram_in(nc, "k_cm2p", [128, 256])
    k_sel = _dram_in(nc, "k_sel", [128, 2])
    k_maskf = _dram_in(nc, "k_maskf", [128, 4])
    k_masks = _dram_in(nc, "k_masks", [128, 2])
    k_pow2 = _dram_in(nc, "k_pow2", [128, NBIS])
    k_jv = _dram_in(nc, "k_jv", [128, 9])
    k_iv = _dram_in(nc, "k_iv", [128, 64])

    out = nc.dram_tensor("out", [NOWN * 128, D], F32, kind="ExternalOutput").ap()
    yg_d = nc.dram_tensor("yg_scratch", [NOWN, 128, 512], BF16, kind="Internal").ap()
    h_d = nc.dram_tensor("h_scratch", [NOWN * 128, D], F32, kind="Internal").ap()
    mod_d = nc.dram_tensor("mod_scratch", [128, 6 * D], F32, kind="Internal").ap()

    def dbg_out(name, shape, dt=F32):
        return nc.dram_tensor("dbg_" + name, list(shape), dt, kind="ExternalOutput").ap()

    def sb(name, shape, dt=F32, stack=es, key=None):
        t = stack.enter_context(nc.sbuf_tensor(name, list(shape), dt))
        return Tl(t, key or name)

    PS = [Tl(es.enter_context(nc.psum_tensor(f"psb{i}", [128, 512], F32)), f"ps{i}") for i in range(8)]

    ncd = nc.allow_non_contiguous_dma(reason="small parameter loads")
    ncd.__enter__()

    identb = sb("identb", [128, 128], BF16)
    identf = sb("identf", [128, 128], F32)
    O.dma("sp", identb.v, k_identb[:, :])
    O.dma("sp", identf.v, k_identf[:, :])
    sel = sb("sel", [128, 2], F32)
    O.dma("sp", sel.v, k_sel[:, :])

    with contextlib.ExitStack() as p0:
        mod = sb("mod", [128, 6 * D], F32, p0)
        SH1, A1, G1, SH2, A2, G2 = [mod[:, i * D:(i + 1) * D].k(("mod", 2 * i), ("mod", 2 * i + 1))
                                    for i in range(6)]
        cT = sb("cT", [128, 8], F32, p0)
        condT = sb("condT", [128, 8], F32, p0)
        crep = sb("crep", [128, 8, 128], F32, p0)
        bmod = sb("bmodbc", [128, 6 * D], F32, p0)
        gbc = sb("gbc", [128, 2, D], F32, p0)
        wm = [sb(f"wm{i}", [128, 8, 512], F32, p0) for i in range(4)]
        O.dma("sp", cT.v, c_in.rearrange("(kt p) -> p kt", p=128))
        O.dma("sp", bmod.v, b_mod.partition_broadcast(128))
        O.dma("sp", gbc[:, 0, :].k("gbc0"), norm1_g.partition_broadcast(128))
        O.dma("sp", gbc[:, 1, :].k("gbc1"), norm2_g.partition_broadcast(128))
        O.act(condT.v, cT.v, AF.Silu)
        O.cp("dve", crep.v, condT.v.unsqueeze(2).broadcast_to([128, 8, 128]))
        for n in range(12):
            wt = wm[n % 4]
            O.dma("sp", wt.v, w_mod[:, n * 512:(n + 1) * 512].rearrange("(kt p) c -> p kt c", p=128))
            bank = PS[n % 2]
            for kt in range(8):
                O.mm(bank.v, crep[:, kt, :], wt[:, kt, :], kt == 0, kt == 7)
            O.tt("dve", mod[:, n * 512:(n + 1) * 512].k(("mod", n)), bank.v, bmod[:, n * 512:(n + 1) * 512], ALU.add)
        for which, Av in ((0, A1), (1, A2)):
            O.stt(Av, Av, 1.0, gbc[:, which, :].k(f"gbc{which}"), ALU.add, ALU.mult)
        O.dma("sp", mod_d[:, :], mod.v.k(*[("mod", n) for n in range(12)]))
        if "mod" in dbg:
            O.dma("sp", dbg_out("mod", [128, 6 * D])[:, :], mod.v.k(*[("mod", n) for n in range(12)]), final=True)
        S.barrier()
        S.emit()

    def norm_block(xsrc, A, SHv, uT_dst, W, idx):
        xt = W["x"][idx % W["nbuf"]]
        O.dma("sp", xt.v, xsrc)
        ss = W["ss"][:, idx % 2:idx % 2 + 1].k(("ss", idx % 2))
        O.act(W["junk"].v, xt.v, AF.Square, accum=ss)
        var = W["var"][:, idx % 2:idx % 2 + 1].k(("var", idx % 2))
        O.ts("dve", var, ss, 1.0 / D, ALU.mult, EPS, ALU.add)
        O.act(var, var, AF.Sqrt)
        rstd = W["rstd"][:, idx % 2:idx % 2 + 1].k(("rstd", idx % 2))
        O.recip(rstd, var)
        O.stt(W["t1"].v, xt.v, rstd, A, ALU.mult, ALU.mult)
        ub = W["ub"][idx % W["nbuf"]]
        O.tt("pool", ub.v, W["t1"].v, SHv, ALU.add)
        pst = PS[0].v.bitcast(BF16)
        for kt in range(8):
            O.tr(pst[:, kt * 128:(kt + 1) * 128], ub[:, kt * 128:(kt + 1) * 128], identb.v)
        O.cp("act", uT_dst, pst.rearrange("p (k t) -> p k t", k=8))

    def load_mod(stack, pfx, idxs):
        outv = []
        for i in idxs:
            t = sb(f"{pfx}mod{i}", [128, D], F32, stack)
            O.dma("sp", t.v, mod_d[:, i * D:(i + 1) * D])
            outv.append(t.v)
        return outv

    def norm_work(stack, pfx, nbuf=2):
        return dict(
            nbuf=nbuf,
            x=[sb(f"{pfx}x{i}", [128, D], F32, stack) for i in range(nbuf)],
            ss=sb(f"{pfx}ss", [128, 2], F32, stack), var=sb(f"{pfx}var", [128, 2], F32, stack),
            rstd=sb(f"{pfx}rstd", [128, 2], F32, stack),
            junk=sb(f"{pfx}junk", [128, D], BF16, stack), t1=sb(f"{pfx}t1", [128, D], F32, stack),
            ub=[sb(f"{pfx}ub{i}", [128, D], BF16, stack) for i in range(nbuf)],
        )

    ckv_d = nc.dram_tensor("ckv_scratch", [128, NB, 129], BF16, kind="Internal").ap()
    ckvT_d = nc.dram_tensor("ckvT_scratch", [128, L], BF16, kind="Internal").ap()
    kiT_d = nc.dram_tensor("kiT_scratch", [128, L], BF16, kind="Internal").ap()

    with contextlib.ExitStack() as pA:
        W = norm_work(pA, "a_")
        SH1, A1 = load_mod(pA, "a_", [0, 1])
        wsh = sb("wsh", [128, 8, 704], BF16, pA)
        for (c0, c1, o0) in ((OFF["xs"], OFF["xs"] + 512, 0), (OFF["ckv"], OFF["ckv"] + 128, 512),
                             (OFF["ki"], OFF["ki"] + 64, 640)):
            O.dma("pool", wsh[:, :, o0:o0 + (c1 - c0)], w_in[:, c0:c1].rearrange("(kt p) c -> p kt c", p=128))
        gkv = sb("gkv", [128, 128], F32, pA)
        gik = sb("gik", [128, 64], F32, pA)
        O.dma("sp", gkv.v, kv_g.partition_broadcast(128))
        O.dma("sp", gik.v, ik_g.partition_broadcast(128))
        cks = sb("a_cks", [128, 4, 129], BF16, pA)
        ckTs = sb("a_ckTs", [128, 512], BF16, pA)
        kiTs = sb("a_kiTs", [128, 512], BF16, pA)
        O.memset("pool", cks[:, :, 128:129].k("ckv_ones"), 1.0)
        uT = [sb(f"a_uT{i}", [128, 8, 512], BF16, pA) for i in range(2)]
        ssk = sb("a_ssk", [128, 2], F32, pA)
        rsk = sb("a_rsk", [128, 2], F32, pA)
        junk2 = sb("a_junk2", [128, 128], BF16, pA)
        kin2 = sb("a_kin2", [128, 2, 64], BF16, pA)

        S5 = _s5_prepare(nc, S, O, sb, pA, PS, identf, locals())
        xsT = [sb(f"a_xsT{i}", [128, 4, 8, 64], BF16, pA) for i in range(2)]
        BR = sb("a_BR", [128, 16, 64], F32, pA)
        BI = sb("a_BI", [128, 16, 64], F32, pA)
        T1 = [sb(f"a_T1{n}", [128, 8, 64], F32, pA) for n in range(2)]
        T2 = [sb(f"a_T2{n}", [128, 8, 64], F32, pA) for n in range(2)]
        STr = sb("a_STr", [128, 16, 64], F32, pA)
        STi = sb("a_STi", [128, 16, 64], F32, pA)
        inj = sb("a_inj", [128, 2, 16], F32, pA)
        itmp = sb("a_itmp", [128, 2, 16], F32, pA)
        Sbf = [[sb(f"a_Sbf{i}{c}", [128, 16, 65], BF16, pA) for c in range(2)] for i in range(2)]
        gx2 = [sb(f"a_gx2{n}", [128, 512], F32, pA) for n in range(2)]
        ygs = sb("a_ygs", [128, 4, 512], BF16, pA)
        ygt = sb("a_ygt", [128, 4, 128], BF16, pA)
        ygo = [sb(f"a_ygo{i}", [128, 4, 128], BF16, pA) for i in range(2)]
        O.memset("pool", Sbf[1][0][:, :, 64:65], 0.0)
        O.memset("pool", Sbf[1][1][:, :, 64:65], 0.0)

        ygdbg = dbg_out("yg", [NOWN, 128, 512], BF16) if "s5" in dbg else None
        def front(sbi):
            u = uT[sbi % 2]
            xs = xsT[sbi % 2]
            ukeys = [(u.key, bl) for bl in range(4)]
            ch = []
            for bl in range(4):
                blk = 4 * sbi + bl
                ch.append(lambda bl=bl, blk=blk: norm_block(
                    x_all[blk * 128:(blk + 1) * 128, :], A1, SH1, u[:, :, bl * 128:(bl + 1) * 128].k((u.key, bl)), W, blk))

            def xs_proj(c4):
                bank = PS[1 + c4 % 2]
                for kt in range(8):
                    O.mm(bank.v, wsh[:, kt, c4 * 128:(c4 + 1) * 128], u[:, kt, :].k(*ukeys), kt == 0, kt == 7)
                O.cp("act", xs[:, c4, :, :].k((xs.key, c4)), bank.v.rearrange("p (m r) -> p r m", r=8))
            for c4 in range(4):
                ch.append(lambda c4=c4: xs_proj(c4))

            def keys(bl):
                bank = PS[3]
                for kt in range(8):
                    O.mm(bank[:, 0:192], u[:, kt, bl * 128:(bl + 1) * 128].k((u.key, bl)), wsh[:, kt, 512:704],
                         kt == 0, kt == 7)
                O.act(junk2.v, bank[:, 0:128], AF.Square, accum=ssk[:, 0:1].k("ssk0"))
                O.act(junk2[:, 0:64], bank[:, 128:192], AF.Square, accum=ssk[:, 1:2].k("ssk1"))
                O.ts("dve", rsk[:, 0:1].k("rsk0"), ssk[:, 0:1].k("ssk0"), 1.0 / 128, ALU.mult, EPS, ALU.add)
                O.ts("dve", rsk[:, 1:2].k("rsk1"), ssk[:, 1:2].k("ssk1"), 1.0 / 64, ALU.mult, EPS, ALU.add)
                O.act(rsk.v.k("rsk0", "rsk1"), rsk.v.k("rsk0", "rsk1"), AF.Sqrt)
                O.recip(rsk.v.k("rsk0", "rsk1"), rsk.v.k("rsk0", "rsk1"))
                ckb = cks[:, bl, 0:128].k(("cks", bl))
                O.stt(ckb, bank[:, 0:128], rsk[:, 0:1].k("rsk0"), gkv.v, ALU.mult, ALU.mult)
                for dup in range(2):
                    O.stt(kin2[:, dup, :], bank[:, 128:192], rsk[:, 1:2].k("rsk1"), gik.v, ALU.mult, ALU.mult)
                pst = PS[4].v.bitcast(BF16)
                O.tr(pst[:, 0:128], ckb, identb.v)
                O.tr(pst[:, 128:256], kin2.v.rearrange("p a b -> p (a b)"), identb.v)
                O.cp("dve", ckTs[:, bl * 128:(bl + 1) * 128].k(("ckTs", bl)), pst[:, 0:128])
                O.cp("dve", kiTs[:, bl * 128:(bl + 1) * 128].k(("kiTs", bl)), pst[:, 128:256])
            for bl in range(4):
                ch.append(lambda bl=bl: keys(bl))

            def spill():
                O.dma("sp", ckv_d[:, 4 * sbi:4 * sbi + 4, :], cks.v.k("ckv_ones", *[("cks", bl) for bl in range(4)]))
                O.dma("sp", ckvT_d[:, sbi * 512:(sbi + 1) * 512], ckTs.v.k(*[("ckTs", bl) for bl in range(4)]))
                O.dma("sp", kiT_d[:, sbi * 512:(sbi + 1) * 512], kiTs.v.k(*[("kiTs", bl) for bl in range(4)]))
            ch.append(spill)
            return ch

        def merge(ca, cb):
            na, nb = len(ca), len(cb)
            ia = ib = 0
            while ia < na or ib < nb:
                if ib >= nb or (ia < na and ia * nb <= ib * na):
                    ca[ia]()
                    ia += 1
                else:
                    cb[ib]()
                    ib += 1

        nsb = 0 if DEV_SKIP_A else NB // 4
        if nsb:
            for c in front(0):
                c()
        for sbi in range(nsb):
            s5c = _s5_superblock(S, O, PS, S5, sbi, xsT[sbi % 2], BR, BI, T1, T2, STr, STi, inj, itmp, Sbf,
                                 gx2, ygs, ygt, ygo, sel, yg_d, ygdbg)
            merge(s5c, front(sbi + 1) if sbi + 1 < nsb else [])
        if "ckv" in dbg:
            S.barrier()
            dk = dbg_out("ckvT", [128, L], BF16)
            S.dma("sp", lambda e: e.dma_start(out=dk[:, :], in_=ckvT_d[:, :]), final=True)
            dk2 = dbg_out("kiT2", [128, L], BF16)
            S.dma("sp", lambda e: e.dma_start(out=dk2[:, :], in_=kiT_d[:, :]), final=True)
            dk3 = dbg_out("ckv_sb", [128, NB, 129], BF16)
            S.dma("sp", lambda e: e.dma_start(out=dk3[:, :, :], in_=ckv_d[:, :, :]), final=True)
        if "s5" in dbg:
            for nm in S5["dbg"]:
                t = S5["dbg"][nm]
                shp = list(t.t.shape)
                O.dma("sp", dbg_out(nm, [128, int(np.prod(shp[1:]))], t.t.dtype)[:, :],
                      t.v.rearrange("p a b c -> p (a b c)") if len(shp) == 4 else
                      (t.v.rearrange("p a b -> p (a b)") if len(shp) == 3 else t.v), final=True)
        S.barrier()
        S.emit()
    if "stopA" in dbg:
        ncd.__exit__(None, None, None)
        return

    _phase_b1_b2_c(nc, S, O, sb, PS, dbg, dbg_out, norm_block, norm_work, load_mod, locals())
    ncd.__exit__(None, None, None)
def _phasor(O, cyc, outc, outs, tmps):
    ri, rf, s1, q = tmps
    O.cp("dve", ri, cyc)
    O.cp("dve", rf, ri)
    O.tt("dve", rf, cyc, rf, ALU.subtract)
    O.act(s1, rf, AF.Sin, scale=math.pi)
    O.act(q, rf, AF.Sin, scale=math.pi / 2)
    O.tt("dve", q, q, q, ALU.mult)
    O.ts("dve", q, q, -2.0, ALU.mult, 1.0, ALU.add)
    O.stt(outs, s1, 2.0, q, ALU.mult, ALU.mult)
    O.tt("dve", s1, s1, s1, ALU.mult)
    O.ts("dve", outc, s1, -2.0, ALU.mult, 1.0, ALU.add)


def _s5_prepare(nc, S, O, sb, st, PS, identf, g):
    R = {}
    KT = sb("s5_KT", [128, 8, 4, 128], BF16, st)
    Fm = [sb(f"s5_Fm{c}", [128, 8, 4, 2, 2, 64], BF16, st) for c in range(2)]
    Em = [sb(f"s5_Em{c}", [128, 8, 8, 2, 2, 2, 16], BF16, st) for c in range(2)]
    Dc = sb("s5_Dc", [128, 16, 64], F32, st)
    Ds = sb("s5_Ds", [128, 16, 64], F32, st)
    rho = sb("s5_rho", [128, 16, 64], F32, st)
    Lam = sb("s5_Lam", [128, 2, 16], F32, st)
    R.update(KT=KT, Fm=Fm, Em=Em, Dc=Dc, Ds=Ds, rho=rho, Lam=Lam)
    R["dbg"] = dict(s5_KT=KT, s5_Fm0=Fm[0], s5_Fm1=Fm[1], s5_Em0=Em[0], s5_Em1=Em[1], s5_Dc=Dc, s5_Ds=Ds,
                    s5_rho=rho, s5_Lam=Lam)
    holder = {}

    def ld(name, src, shape):
        t = sb("s5t_" + name, shape, F32, holder["tp"])
        O.dma("sp", t.v, src)
        return t

    def tmp(name, shape, dt=F32):
        return sb("s5t_" + name, shape, dt, holder["tp"])

    with contextlib.ExitStack() as tp:
        holder["tp"] = tp

        masks = ld("masks", g["k_masks"][:, :], [128, 2])
        jv = ld("jv", g["k_jv"][:, :], [128, 9])
        iv = ld("iv", g["k_iv"][:, :], [128, 64])
        dsk = ld("dsk", g["d_dsk"][:, :], [128, 4])

        def lam_common(pfx, are_d, aim_d, ldt_d, Wd):
            are = ld(pfx + "are", are_d[:, :], [128, Wd])
            aim = ld(pfx + "aim", aim_d[:, :], [128, Wd])
            dt = ld(pfx + "ldt", ldt_d[:, :], [128, Wd])
            O.act(dt.v, dt.v, AF.Exp)
            x1 = tmp(pfx + "x1", [128, Wd])
            angc = tmp(pfx + "angc", [128, Wd])
            O.tt("dve", x1.v, are.v, dt.v, ALU.mult)
            O.tt("dve", angc.v, aim.v, dt.v, ALU.mult)
            O.ts("dve", angc.v, angc.v, 1.0 / (2 * math.pi), ALU.mult)
            ph = (tmp(pfx + "ri", [128, Wd], I32).v, tmp(pfx + "rf", [128, Wd]).v, tmp(pfx + "s1", [128, Wd]).v,
                  tmp(pfx + "q", [128, Wd]).v)
            rj = tmp(pfx + "rj", [128, Wd])
            uc = tmp(pfx + "uc", [128, Wd])
            us = tmp(pfx + "us", [128, Wd])
            mg = tmp(pfx + "mg", [128, Wd])

            def lam_pow(j, lr, li):
                O.ts("dve", rj.v, angc.v, float(j), ALU.mult)
                _phasor(O, rj.v, uc.v, us.v, ph)
                O.act(mg.v, x1.v, AF.Exp, scale=float(j))
                O.tt("dve", lr, mg.v, uc.v, ALU.mult)
                O.tt("dve", li, mg.v, us.v, ALU.mult)

            l1r = tmp(pfx + "l1r", [128, Wd])
            l1i = tmp(pfx + "l1i", [128, Wd])
            lam_pow(1, l1r.v, l1i.v)
            den = tmp(pfx + "den", [128, Wd])
            t0 = tmp(pfx + "t0", [128, Wd])
            cre = tmp(pfx + "cfr", [128, Wd])
            cim = tmp(pfx + "cfi", [128, Wd])
            O.tt("dve", den.v, are.v, are.v, ALU.mult)
            O.tt("dve", t0.v, aim.v, aim.v, ALU.mult)
            O.tt("dve", den.v, den.v, t0.v, ALU.add)
            O.recip(den.v, den.v)
            O.ts("dve", l1r.v, l1r.v, -1.0, ALU.add)
            O.tt("dve", cre.v, l1r.v, are.v, ALU.mult)
            O.tt("dve", t0.v, l1i.v, aim.v, ALU.mult)
            O.tt("dve", cre.v, cre.v, t0.v, ALU.add)
            O.tt("dve", cre.v, cre.v, den.v, ALU.mult)
            O.tt("dve", cim.v, l1i.v, are.v, ALU.mult)
            O.tt("dve", t0.v, l1r.v, aim.v, ALU.mult)
            O.tt("dve", cim.v, cim.v, t0.v, ALU.subtract)
            O.tt("dve", cim.v, cim.v, den.v, ALU.mult)
            return lam_pow, cre, cim, x1, angc, ph

        lam_pow, cre, cim, x1, angc, ph = lam_common("sl_", g["sl_are"], g["sl_aim"], g["sl_ldt"], 16)
        Bre = ld("sl_bre", g["sl_bre"][:, :, :], [128, 16, 16])
        Bim = ld("sl_bim", g["sl_bim"][:, :, :], [128, 16, 16])
        Cre = ld("sl_cre", g["sl_cre"][:, :, :], [128, 16, 16])
        Cim = ld("sl_cim", g["sl_cim"][:, :, :], [128, 16, 16])
        bc = lambda t: t.v.unsqueeze(2).broadcast_to([128, 16, 16])
        bcv = lambda v: v.unsqueeze(2).broadcast_to([128, 16, 16])
        Bbr = tmp("sl_Bbr", [128, 16, 16])
        Bbi = tmp("sl_Bbi", [128, 16, 16])
        ta = tmp("sl_ta", [128, 16, 16])
        tb = tmp("sl_tb", [128, 16, 16])
        O.tt("dve", Bbr.v, Bre.v, bc(cre), ALU.mult)
        O.tt("dve", ta.v, Bim.v, bc(cim), ALU.mult)
        O.tt("dve", Bbr.v, Bbr.v, ta.v, ALU.subtract)
        O.tt("dve", Bbi.v, Bim.v, bc(cre), ALU.mult)
        O.tt("dve", ta.v, Bre.v, bc(cim), ALU.mult)
        O.tt("dve", Bbi.v, Bbi.v, ta.v, ALU.add)
        CM = [tmp("sl_CMr", [128, 8, 2, 2, 2, 16], BF16), tmp("sl_CMi", [128, 8, 2, 2, 2, 16], BF16)]
        GM = [tmp("sl_GMr", [128, 8, 2, 2, 2, 16], BF16), tmp("sl_GMi", [128, 8, 2, 2, 2, 16], BF16)]
        for tl in CM + GM + Em:
            O.memset("pool", tl.v, 0.0)
        gs = lambda t, s: t.v.rearrange("p (gq s) c -> p gq s c", s=2)[:, :, s, :]
        for s in range(2):
            for g2 in range(2):
                O.ts("dve", CM[0][:, :, s, s, g2, :], gs(Cre, s), masks[:, g2:g2 + 1], ALU.mult)
                O.ts("dve", CM[1][:, :, s, s, g2, :], gs(Cim, s), masks[:, g2:g2 + 1], ALU.mult, -1.0, ALU.mult)
        ljr = tmp("sl_ljr", [128, 16])
        lji = tmp("sl_lji", [128, 16])
        O.memset("pool", KT.v, 0.0)
        for j in range(9):
            lam_pow(j, ljr.v, lji.v)
            if j < 8:
                O.tt("dve", ta.v, Bbr.v, bcv(ljr.v), ALU.mult)
                O.tt("dve", tb.v, Bbi.v, bcv(lji.v), ALU.mult)
                O.tt("dve", ta.v, ta.v, tb.v, ALU.subtract)
                for s in range(2):
                    for g2 in range(2):
                        O.ts("dve", GM[0][:, :, s, s, g2, :], gs(ta, s), masks[:, g2:g2 + 1], ALU.mult)
                O.tt("dve", ta.v, Bbi.v, bcv(ljr.v), ALU.mult)
                O.tt("dve", tb.v, Bbr.v, bcv(lji.v), ALU.mult)
                O.tt("dve", ta.v, ta.v, tb.v, ALU.add)
                for s in range(2):
                    for g2 in range(2):
                        O.ts("dve", GM[1][:, :, s, s, g2, :], gs(ta, s), masks[:, g2:g2 + 1], ALU.mult)
                bank = PS[4 + j // 2]
                f64 = lambda v: v.rearrange("p a b c -> p (a b c)")
                for gh in range(16):
                    c4, pair = gh // 4, gh % 4
                    q = pair // 2
                    col = ((j % 2) * 4 + c4) * 64
                    o = bank[64 * q:64 * q + 64, col:col + 64]
                    O.mm(o, f64(GM[0][:, gh // 2, gh % 2, :, :, :]), f64(CM[0][:, gh // 2, gh % 2, :, :, :]),
                         pair % 2 == 0, False)
                    O.mm(o, f64(GM[1][:, gh // 2, gh % 2, :, :, :]), f64(CM[1][:, gh // 2, gh % 2, :, :, :]),
                         False, pair % 2 == 1)
                if j % 2 == 1:
                    jh = j // 2
                    for q in range(2):
                        O.cp("dve", KT[64 * q:64 * q + 64, 2 * jh:2 * jh + 2, :, 64 * q:64 * q + 64],
                             bank[64 * q:64 * q + 64, :].rearrange("p (j c k) -> p j c k", j=2, c=4))
            if j >= 1:
                r = j - 1
                O.tt("dve", ta.v, Cre.v, bcv(ljr.v), ALU.mult)
                O.tt("dve", tb.v, Cim.v, bcv(lji.v), ALU.mult)
                O.tt("dve", ta.v, ta.v, tb.v, ALU.subtract)
                for s in range(2):
                    for g2 in range(2):
                        O.ts("dve", Em[0][:, r, :, s, s, g2, :], gs(ta, s), masks[:, g2:g2 + 1], ALU.mult)
                O.tt("dve", ta.v, Cre.v, bcv(lji.v), ALU.mult)
                O.tt("dve", tb.v, Cim.v, bcv(ljr.v), ALU.mult)
                O.tt("dve", ta.v, ta.v, tb.v, ALU.add)
                for s in range(2):
                    for g2 in range(2):
                        O.ts("dve", Em[1][:, r, :, s, s, g2, :], gs(ta, s), masks[:, g2:g2 + 1], ALU.mult, -1.0,
                             ALU.mult)
            if j == 8:
                O.cp("dve", Lam[:, 0, :], ljr.v)
                O.cp("dve", Lam[:, 1, :], lji.v)
        for c4 in range(4):
            O.stt(KT[:, 0, c4, :], identf.v, dsk[:, c4:c4 + 1], KT[:, 0, c4, :], ALU.mult, ALU.add)
        r8 = tmp("sl_r8", [128, 16])
        O.ts("dve", r8.v, angc.v, 8.0, ALU.mult)
        O.cp("dve", ph[0], r8.v)
        O.cp("dve", ph[1], ph[0])
        O.tt("dve", r8.v, r8.v, ph[1], ALU.subtract)
        RT = tmp("sl_RT", [128, 16, 64])
        O.tt("dve", RT.v, r8.v.unsqueeze(2).broadcast_to([128, 16, 64]),
             iv.v.unsqueeze(1).broadcast_to([128, 16, 64]), ALU.mult)
        ph2 = (tmp("sl_ri2", [128, 512], I32).v, tmp("sl_rf2", [128, 512]).v, tmp("sl_s12", [128, 512]).v,
               tmp("sl_q2", [128, 512]).v)
        fl = lambda t, h: t[:, 8 * h:8 * h + 8, :].rearrange("p a b -> p (a b)")
        for h in range(2):
            _phasor(O, fl(RT, h), fl(Dc, h), fl(Ds, h), ph2)
        rh = tmp("sl_rh", [128, 16])
        O.act(rh.v, x1.v, AF.Exp, scale=8.0)
        O.cp("dve", rho.v, rh.v.unsqueeze(2).broadcast_to([128, 16, 64]))
        O.memset("dve", rho[:, :, 0:1], 0.0)

        S.barrier()
        S.emit()
    with contextlib.ExitStack() as tp:
        holder["tp"] = tp
        maskf = ld("maskf", g["k_maskf"][:, :], [128, 4])
        lam_pow, cre, cim, x1, angc, ph = lam_common("fl_", g["fl_are"], g["fl_aim"], g["fl_ldt"], 256)
        Bre = ld("fl_bre", g["fl_bre"][:, :], [128, 256])
        Bim = ld("fl_bim", g["fl_bim"][:, :], [128, 256])
        Bbr = tmp("fl_Bbr", [128, 256])
        Bbi = tmp("fl_Bbi", [128, 256])
        ta = tmp("fl_ta", [128, 256])
        tb = tmp("fl_tb", [128, 256])
        O.tt("dve", Bbr.v, Bre.v, cre.v, ALU.mult)
        O.tt("dve", ta.v, Bim.v, cim.v, ALU.mult)
        O.tt("dve", Bbr.v, Bbr.v, ta.v, ALU.subtract)
        O.tt("dve", Bbi.v, Bim.v, cre.v, ALU.mult)
        O.tt("dve", ta.v, Bre.v, cim.v, ALU.mult)
        O.tt("dve", Bbi.v, Bbi.v, ta.v, ALU.add)
        ljr = tmp("fl_ljr", [128, 256])
        lji = tmp("fl_lji", [128, 256])
        v4 = lambda t: t.v.rearrange("p (c n) -> p c n", c=4)
        for j in range(8):
            lam_pow(j, ljr.v, lji.v)
            O.tt("dve", ta.v, Bbr.v, ljr.v, ALU.mult)
            O.tt("dve", tb.v, Bbi.v, lji.v, ALU.mult)
            O.tt("dve", ta.v, ta.v, tb.v, ALU.subtract)
            for wg in range(4):
                O.ts("dve", Fm[0][:, j, :, wg // 2, wg % 2, :], v4(ta), maskf[:, wg:wg + 1], ALU.mult)
            O.tt("dve", ta.v, Bbi.v, ljr.v, ALU.mult)
            O.tt("dve", tb.v, Bbr.v, lji.v, ALU.mult)
            O.tt("dve", ta.v, ta.v, tb.v, ALU.add)
            for wg in range(4):
                O.ts("dve", Fm[1][:, j, :, wg // 2, wg % 2, :], v4(ta), maskf[:, wg:wg + 1], ALU.mult)
        S.barrier()
        S.emit()
    return R


def _s5_superblock(S, O, PS, S5, sbi, xs, BR, BI, T1, T2, STr, STi, inj, itmp, Sbf,
                   gx2, ygs, ygt, ygo, sel, yg_d, ygdbg=None):
    KT, Fm, Em, Dc, Ds, rho, Lam = S5["KT"], S5["Fm"], S5["Em"], S5["Dc"], S5["Ds"], S5["rho"], S5["Lam"]
    cur, prv = Sbf[sbi % 2], Sbf[(sbi + 1) % 2]
    f2 = lambda v: v.rearrange("p a b -> p (a b)")
    BRk = BR.v.k(("BR", 0), ("BR", 1))
    BIk = BI.v.k(("BI", 0), ("BI", 1))
    SRr, SRi = BRk, BIk

    def snew(half):
        for ghl in range(8):
            gh = half * 8 + ghl
            c4, pair = gh // 4, gh % 4
            q = pair // 2
            for c in range(2):
                o = PS[5 + c][:, ghl * 64:(ghl + 1) * 64]
                for k in range(8):
                    O.mm(o, Fm[c][64 * q:64 * q + 64, 7 - k, c4, pair % 2, :, :].rearrange("p a b -> p (a b)"),
                         xs[64 * q:64 * q + 64, c4, k, :].k((xs.key, c4)), k == 0, k == 7)

    def demod(half):
        hs = slice(half * 8, half * 8 + 8)
        pr = PS[5].v.rearrange("p (a b) -> p a b", a=8)
        pi = PS[6].v.rearrange("p (a b) -> p a b", a=8)
        O.tt("dve", T1[0].v, pr, Dc[:, hs, :], ALU.mult)
        O.tt("dve", T2[0].v, pi, Ds[:, hs, :], ALU.mult)
        O.tt("pool", BR[:, hs, :].k(("BR", half)), T1[0].v, T2[0].v, ALU.add)
        O.tt("dve", T1[1].v, pi, Dc[:, hs, :], ALU.mult)
        O.tt("dve", T2[1].v, pr, Ds[:, hs, :], ALU.mult)
        O.tt("pool", BI[:, hs, :].k(("BI", half)), T1[1].v, T2[1].v, ALU.subtract)

    def scan_stage():
        if sbi > 0:
            O.tt("pool", BRk[:, :, 0], BRk[:, :, 0], inj[:, 0, :], ALU.add)
            O.tt("pool", BIk[:, :, 0], BIk[:, :, 0], inj[:, 1, :], ALU.add)
        O.scan(f2(STr.v), f2(rho.v), f2(BRk), 0.0, ALU.mult, ALU.add)
        O.scan(f2(STi.v), f2(rho.v), f2(BIk), 0.0, ALU.mult, ALU.add)

    def remod_stage():
        O.tt("dve", BRk, STr.v, Dc.v, ALU.mult)
        O.tt("pool", BIk, STi.v, Ds.v, ALU.mult)
        O.tt("dve", BRk, BRk, BIk, ALU.subtract)
        O.tt("pool", BIk, STr.v, Ds.v, ALU.mult)
        O.tt("dve", STi.v, STi.v, Dc.v, ALU.mult)
        O.tt("pool", BIk, BIk, STi.v, ALU.add)
        O.tt("pool", inj[:, 0, :], SRr[:, :, 63], Lam[:, 0, :], ALU.mult)
        O.tt("pool", itmp[:, 0, :], SRi[:, :, 63], Lam[:, 1, :], ALU.mult)
        O.tt("pool", inj[:, 0, :], inj[:, 0, :], itmp[:, 0, :], ALU.subtract)
        O.tt("pool", inj[:, 1, :], SRr[:, :, 63], Lam[:, 1, :], ALU.mult)
        O.tt("pool", itmp[:, 1, :], SRi[:, :, 63], Lam[:, 0, :], ALU.mult)
        O.tt("pool", inj[:, 1, :], inj[:, 1, :], itmp[:, 1, :], ALU.add)
        for c, SR in ((0, SRr), (1, SRi)):
            O.cp("pool", cur[c][:, :, 0:1], prv[c][:, :, 64:65])
            O.cp("act", cur[c][:, :, 1:65], SR)

    def out_stage(c4):
        bank = PS[7]
        for r in range(8):
            o = bank[:, r * 64:(r + 1) * 64]
            for j in range(r + 1):
                O.mm(o, KT[:, j, c4, :], xs[:, c4, r - j, :].k((xs.key, c4)), j == 0, False)
            for pair in range(4):
                gh = c4 * 4 + pair
                q = pair // 2
                o2 = bank[64 * q:64 * q + 64, r * 64:(r + 1) * 64]
                O.mm(o2, Em[0][:, r, gh // 2, gh % 2, :, :, :].rearrange("p a b c -> p (a b c)"),
                     cur[0][:, gh, 0:64], False, False)
                O.mm(o2, Em[1][:, r, gh // 2, gh % 2, :, :, :].rearrange("p a b c -> p (a b c)"),
                     cur[1][:, gh, 0:64], False, pair == 3)
        gx = gx2[c4 % 2]
        O.act(gx.v, bank.v, AF.Square)
        O.ts("dve", gx.v, gx.v, GELU_C1, ALU.mult, GELU_C0, ALU.add)
        O.tt("dve", gx.v, gx.v, bank.v, ALU.mult)
        O.act(gx.v, gx.v, AF.Sigmoid)
        O.tt("dve", ygs[:, c4, :].k((ygs.key, c4)).rearrange("p (m r) -> p r m", r=8),
             gx.v.rearrange("p (r m) -> p r m", r=8), bank.v.rearrange("p (r m) -> p r m", r=8), ALU.mult)

    def blend_stage():
        ygk = ygs.v.k(*[(ygs.key, c4) for c4 in range(4)])
        for i2 in range(2):
            i = 2 * sbi + i2
            a0, b0 = (2 * i2) * 128, (2 * i2 + 1) * 128
            yo = ygo[i2]
            O.ts("pool", ygt.v, ygk[:, :, b0:b0 + 128], sel[:, 1:2], ALU.mult)
            O.stt(yo.v, ygk[:, :, a0:a0 + 128], sel[:, 0:1], ygt.v, ALU.mult, ALU.add)
            O.dma("sp", yg_d[i].rearrange("p (c t) -> p c t", c=4), yo.v)
            if ygdbg is not None:
                O.dma("sp", ygdbg[i].rearrange("p (c t) -> p c t", c=4), yo.v, final=True)

    chunks = []
    for half in range(2):
        chunks.append(lambda half=half: snew(half))
        chunks.append(lambda half=half: demod(half))
    chunks.append(scan_stage)
    chunks.append(remod_stage)
    for c4 in range(4):
        chunks.append(lambda c4=c4: out_stage(c4))
    chunks.append(blend_stage)
    return chunks
def _phase_b1_b2_c(nc, S, O, sb, PS, dbg, dbg_out, norm_block, norm_work, load_mod, g):
    x_own, w_in, out = g["x_own"], g["w_in"], g["out"]
    identb, identf, sel = g["identb"], g["identf"], g["sel"]
    ckv_d, ckvT_d, kiT_d, yg_d, h_d = g["ckv_d"], g["ckvT_d"], g["kiT_d"], g["yg_d"], g["h_d"]
    att_d = nc.dram_tensor("att_scratch", [NOWN, 128, 512], BF16, kind="Internal").ap()
    nblk = DEV_NBLK or NOWN
    wview = lambda w, c0, c1: w[:, c0:c1].rearrange("(kt p) c -> p kt c", p=128)

    with contextlib.ExitStack() as pB:
        ckv_sb = sb("ckv_sb", [128, NB, 129], BF16, pB)
        ckvT = sb("ckvT", [128, L], BF16, pB)
        kiT2 = sb("kiT2", [128, L], BF16, pB)
        O.dma("sp", ckv_sb.v, ckv_d[:, :, :])
        O.dma("sp", ckvT.v, ckvT_d[:, :])
        O.dma("sp", kiT2.v, kiT_d[:, :])
        W = norm_work(pB, "b_")
        SH1, A1 = load_mod(pB, "b_", [0, 1])
        wq = sb("b_wq", [128, 8, 512], BF16, pB)
        wqi = sb("b_wqi", [128, 8, 260], BF16, pB)
        O.dma("pool", wq.v, wview(w_in, OFF["q"], OFF["q"] + 512))
        O.dma("pool", wqi[:, :, 0:256], wview(w_in, OFF["qi"], OFF["qi"] + 256))
        O.dma("pool", wqi[:, :, 256:260], wview(w_in, OFF["wi"], OFF["wi"] + 4))
        wukT = sb("b_wukT", [128, 4, 128], BF16, pB)
        wuv = sb("b_wuv", [128, 512], BF16, pB)
        O.dma("pool", wukT.v, g["w_ukT"][:, :, :])
        O.dma("pool", wuv.v, g["w_uv"][:, :])
        negi8 = sb("b_negi8", [128, 1024], BF16, pB)
        cm2 = sb("b_cm2", [128, 256], F32, pB)
        cm2p = sb("b_cm2p", [128, 256], F32, pB)
        pow2 = sb("b_pow2", [128, NBIS], F32, pB)
        O.dma("sp", negi8.v, g["k_negi8"][:, :])
        O.dma("sp", cm2.v, g["k_cm2"][:, :])
        O.dma("sp", cm2p.v, g["k_cm2p"][:, :])
        O.dma("sp", pow2.v, g["k_pow2"][:, :])
        uT1 = sb("b_uT1", [128, 8, 128], BF16, pB)
        qT = sb("b_qT", [128, 4, 128], BF16, pB)
        qlat2 = [sb(f"b_qlatT{n}", [128, 1024], BF16, pB) for n in range(2)]
        absw = sb("b_absw", [128, 4], F32, pB)
        sgn = sb("b_sgn", [128, 4], F32, pB)
        qis = sb("b_qis", [128, 4, 64], BF16, pB)
        qiT = sb("b_qiT", [128, 2, 128], BF16, pB)
        Dg = sb("b_Dg", [128, 4, 128], BF16, pB)
        Rsb = [sb(f"b_R{i}", [128, 4, 512], BF16, pB) for i in range(2)]
        Ibuf = sb("b_Ibuf", [128, L], F32, pB)
        nm2 = [sb(f"b_nm{n}", [128, L], BF16, pB) for n in range(2)]
        NW2 = sb("b_NW2", [128, NBIS], F32, pB)
        GMX = sb("b_GMX", [128, 18], F32, pB)
        GMN = sb("b_GMN", [128, 18], F32, pB)
        t256 = sb("b_t256", [128, 256], F32, pB)
        mx8 = sb("b_mx8", [128, 8], F32, pB)
        bs = sb("b_bs", [128, 8], F32, pB)
        WK = sb("b_WK", [128, NBIS], F32, pB)
        pT = [sb(f"b_pT{i}", [128, 1024], BF16, pB) for i in range(2)]
        rden = sb("b_rden", [128, 8], F32, pB)
        o_n = sb("b_on", [128, 8, 128], BF16, pB)
        onT = sb("b_onT", [128, 8, 128], BF16, pB)
        attT = [sb(f"b_attT{i}", [128, 4, 128], BF16, pB) for i in range(2)]
        col = lambda n: bs[:, n:n + 1].k(("bs", n))
        M1, M2, LO, W0, MID, CNT, TMP = [col(n) for n in range(7)]
        d_att = dbg_out("att", [NOWN, 128, 512], BF16) if "att" in dbg else None

        def prologue_indexer(i):
            nkt = 2 * i + 2
            Lq = nkt * 128
            qlatT = qlat2[i % 2]
            norm_block(x_own[i * 128:(i + 1) * 128, :], A1, SH1, uT1.v, W, i)
            for hp in range(4):
                for kt in range(8):
                    O.mm(PS[1][:, hp * 128:(hp + 1) * 128], wq[:, kt, hp * 128:(hp + 1) * 128], uT1[:, kt, :],
                         kt == 0, kt == 7)
            O.cp("act", qT.v, PS[1].v.rearrange("p (a b) -> p a b", a=4))
            for h in range(8):
                hp, hl = h // 2, h % 2
                O.mm(PS[2 + hl][:, hp * 128:(hp + 1) * 128], wukT[hl * 64:(hl + 1) * 64, hp, :],
                     qT[hl * 64:(hl + 1) * 64, hp, :], True, True)
            for hl in range(2):
                O.act(qlatT.v.rearrange("p (a b c) -> p a b c", a=4, b=2)[:, :, hl, :].k((qlatT.key, hl)),
                      PS[2 + hl].v.rearrange("p (a c) -> p a c", a=4), AF.Copy, scale=0.125)
            for kt in range(8):
                O.mm(PS[4][:, 0:260], uT1[:, kt, :], wqi[:, kt, :], kt == 0, kt == 7)
            O.act(absw.v, PS[4][:, 256:260], AF.Abs, scale=1.0 / 16)
            O.act(sgn.v, PS[4][:, 256:260], AF.Sign)
            O.tt("dve", qis.v, PS[4][:, 0:256].rearrange("p (h d) -> p h d", h=4),
                 absw.v.unsqueeze(2).broadcast_to([128, 4, 64]), ALU.mult)
            O.tt("pool", Dg.v, identf.v.unsqueeze(1).broadcast_to([128, 4, 128]),
                 sgn.v.unsqueeze(2).broadcast_to([128, 4, 128]), ALU.mult)
            pst = PS[0].v.bitcast(BF16)
            for hp2 in range(2):
                O.tr(pst[:, hp2 * 128:(hp2 + 1) * 128], qis[:, 2 * hp2:2 * hp2 + 2, :].rearrange("p a b -> p (a b)"),
                     identb.v)
            O.cp("act", qiT.v, pst[:, 0:256].rearrange("p (a b) -> p a b", a=2))
            ngr = (Lq + 511) // 512
            Ik = []
            for kg in range(ngr):
                nk = min(512, Lq - kg * 512)
                k0 = kg * 512
                R = Rsb[kg % 2]
                for h in range(4):
                    hl = h % 2
                    O.mm(PS[1 + h][:, 0:nk], qiT[hl * 64:(hl + 1) * 64, h // 2, :], kiT2[hl * 64:(hl + 1) * 64, k0:k0 + nk],
                         True, True)
                    O.act(R[:, h, 0:nk].k((R.key, h)), PS[1 + h][:, 0:nk], AF.Relu)
                ib = PS[5 + kg % 2]
                for h in range(4):
                    O.mm(ib[:, 0:nk], Dg[:, h, :], R[:, h, 0:nk].k((R.key, h)), h == 0, h == 3)
                Ik.append(("I", kg))
                gx = lambda n: GMX[:, n:n + 1].k(("gmx", n))
                gn = lambda n: GMN[:, n:n + 1].k(("gmn", n))
                if kg == ngr - 1:
                    if nk > 256:
                        O.cp("act", Ibuf[:, k0:k0 + nk - 256].k(("I", kg)), ib[:, 0:nk - 256])
                        O.reduce(gx(kg), Ibuf[:, k0:k0 + nk - 256].k(("I", kg)), ALU.max)
                        O.reduce(gn(kg), Ibuf[:, k0:k0 + nk - 256].k(("I", kg)), ALU.min)
                    else:
                        O.memset("dve", gx(kg), NEG)
                        O.memset("dve", gn(kg), -NEG)
                    O.tt("dve", Ibuf[:, Lq - 256:Lq].k(("I", kg)), ib[:, nk - 256:nk], cm2.v, ALU.add)
                    O.reduce(gx(ngr), Ibuf[:, Lq - 256:Lq].k(("I", kg)), ALU.max)
                    O.tt("dve", t256.v, ib[:, nk - 256:nk], cm2p.v, ALU.add)
                    O.reduce(gn(ngr), t256.v, ALU.min)
                else:
                    O.cp("act", Ibuf[:, k0:k0 + nk].k(("I", kg)), ib[:, 0:nk])
                    O.reduce(gx(kg), Ibuf[:, k0:k0 + nk].k(("I", kg)), ALU.max)
                    O.reduce(gn(kg), Ibuf[:, k0:k0 + nk].k(("I", kg)), ALU.min)
            return Ik

        def bisect_chunks(i, Ik):
            Lq = (2 * i + 2) * 128
            Iall = Ibuf[:, 0:Lq].k(*Ik)
            nmv = nm2[i % 2]
            ch = []

            ngr = (Lq + 511) // 512

            def init():
                O.reduce(M1, GMX[:, 0:ngr + 1].k(*[("gmx", n) for n in range(ngr + 1)]), ALU.max)
                O.reduce(LO, GMN[:, 0:ngr + 1].k(*[("gmn", n) for n in range(ngr + 1)]), ALU.min)
                O.tt("dve", W0, M1, LO, ALU.subtract)
                O.ts("dve", WK.v, pow2.v, W0, ALU.mult)
                O.ts("dve", NW2[:, 0:NBIS - 1], WK[:, 1:NBIS], -1.0, ALU.mult)
                O.ts("dve", NW2[:, NBIS - 1:NBIS], WK[:, NBIS - 1:NBIS], -1.0, ALU.mult)
                O.stt(NW2[:, NBIS - 1:NBIS], W0, -(2.0 ** -20), NW2[:, NBIS - 1:NBIS], ALU.mult, ALU.add)
                O.tt("dve", MID, LO, WK[:, 0:1], ALU.add)
            ch.append(init)

            def it(k):
                O.ts("dve", nmv[:, 0:Lq], Iall, MID, ALU.is_ge, 0.0, ALU.add, accum=CNT)
                O.stt(TMP, CNT, TOPK - 0.5, WK[:, k:k + 1], ALU.is_ge, ALU.mult)
                O.stt(MID, TMP, NW2[:, k:k + 1], MID, ALU.add, ALU.add)
            for k in range(NBIS):
                ch.append(lambda k=k: it(k))
            ch.append(lambda: O.ts("dve", nmv[:, 0:Lq], Iall, MID, ALU.is_lt))
            return ch

        def attention_chunks(i):
            nkt = 2 * i + 2
            qlatT = qlat2[i % 2]
            qlk = [(qlatT.key, 0), (qlatT.key, 1)]
            nmv = nm2[i % 2]
            ch = []

            def tile(kt):
                lb = (PS[1], PS[2]) if kt % 2 == 0 else (PS[3], PS[4])
                p = pT[kt % 2]
                for half in range(2):
                    O.mm(lb[half].v, ckvT[:, kt * 128:(kt + 1) * 128], qlatT[:, half * 512:(half + 1) * 512].k(*qlk),
                         True, False)
                    O.mm(lb[half].v, nmv[:, kt * 128:(kt + 1) * 128], negi8[:, half * 512:(half + 1) * 512], False, True)
                    O.act(p[:, half * 512:(half + 1) * 512].k((p.key, half)), lb[half].v, AF.Exp)
                for h in range(8):
                    bank, off = PS[5 + h // 3], (h % 3) * 129
                    O.mm(bank[:, off:off + 129], p[:, h * 128:(h + 1) * 128].k((p.key, h // 4)), ckv_sb[:, kt, :],
                         kt == 0 and h % 3 == 0, kt == nkt - 1)
            for kt in range(nkt):
                ch.append(lambda kt=kt: tile(kt))
            return ch

        def epilogue(i):
            pst = PS[0].v.bitcast(BF16)
            for b3 in range(3):
                nh = 3 if b3 < 2 else 2
                v3 = PS[5 + b3][:, 0:nh * 129].rearrange("p (a b) -> p a b", a=nh)
                O.recip(rden[:, 3 * b3:3 * b3 + nh].k(("rden", b3)), v3[:, :, 128])
                O.tt("dve", o_n[:, 3 * b3:3 * b3 + nh, :].k(("on", b3)), v3[:, :, 0:128],
                     rden[:, 3 * b3:3 * b3 + nh].k(("rden", b3)).unsqueeze(2).broadcast_to([128, nh, 128]), ALU.mult)
            for h in range(8):
                O.tr(pst[:, h * 128:(h + 1) * 128], o_n[:, h, :].k(("on", h // 3)), identb.v)
            O.cp("act", onT.v, pst.rearrange("p (a b) -> p a b", a=8))
            for h in range(8):
                hp, hl = h // 2, h % 2
                O.mm(PS[1][hl * 64:(hl + 1) * 64, hp * 128:(hp + 1) * 128], wuv[:, h * 64:(h + 1) * 64], onT[:, h, :],
                     True, True)
            at = attT[i % 2]
            O.cp("act", at.v, PS[1].v.rearrange("p (a b) -> p a b", a=4))
            O.dma("sp", att_d[i].rearrange("p (a b) -> p a b", a=4), at.v)
            if d_att is not None:
                O.dma("sp", d_att[i].rearrange("p (a b) -> p a b", a=4), at.v, final=True)

        def merge(ca, cb):
            na, nb = len(ca), len(cb)
            ia = ib = 0
            while ia < na or ib < nb:
                if ib >= nb or (ia < na and ia * nb <= ib * na):
                    ca[ia]()
                    ia += 1
                else:
                    cb[ib]()
                    ib += 1

        Ik0 = prologue_indexer(0)
        for c in bisect_chunks(0, Ik0):
            c()
        for i in range(nblk):
            bc = []
            if i + 1 < nblk:
                Ik1 = prologue_indexer(i + 1)
                bc = bisect_chunks(i + 1, Ik1)
            merge(bc, attention_chunks(i))
            epilogue(i)
        S.barrier()
        S.emit()
    if "stopB1" in dbg:
        return

    ngrp = (nblk + 3) // 4
    mod_d, final_g = g["mod_d"], g["final_g"]
    with contextlib.ExitStack() as pC:
        W = norm_work(pC, "c_")
        SH1, A1, G1 = load_mod(pC, "c_", [0, 1, 2])
        wg = sb("c_wg", [128, 8, 2048], BF16, pC)
        O.dma("pool", wg[:, :, 0:1024], wview(w_in, OFF["ga"], OFF["ga"] + 1024))
        O.dma("pool", wg[:, :, 1024:2048], wview(w_in, OFF["gb"], OFF["gb"] + 1024))
        wglu = sb("c_wglu", [128, 4, 2048], BF16, pC)
        O.dma("pool", wglu.v, g["w_glu"].rearrange("(kt p) c -> p kt c", p=128))
        bglu = sb("c_bglu", [128, 16], F32, pC)
        O.dma("sp", bglu.v, g["b_glu"].rearrange("(ft p) -> p ft", p=128))
        wap = sb("c_wap", [128, 4, 1024], BF16, pC)
        O.dma("pool", wap.v, g["w_ap"].rearrange("(kt p) c -> p kt c", p=128))
        wout = sb("c_wout", [128, 8, 1024], BF16, pC)
        O.dma("pool", wout.v, g["w_out"].rearrange("(kt p) c -> p kt c", p=128))
        uT4 = sb("c_uT4", [128, 8, 512], BF16, pC)
        sg = [sb(f"c_sg{w}", [128, 8, 512], BF16, pC) for w in range(2)]
        ygl = sb("c_ygl", [128, 4, 512], BF16, pC)
        attl = sb("c_attl", [128, 4, 512], BF16, pC)
        sgt2 = [sb(f"c_sgt{n}", [128, 512], BF16, pC) for n in range(2)]
        ys2 = [sb(f"c_ys{n}", [128, 512], BF16, pC) for n in range(2)]
        m1 = sb("c_m1", [128, 8, 512], BF16, pC)
        mT = sb("c_mT", [128, 8, 512], BF16, pC)
        ta2 = [sb(f"c_ta{n}", [128, 512], F32, pC) for n in range(2)]
        xr = [sb(f"c_xr{n}", [128, D], F32, pC) for n in range(2)]
        hout = [sb(f"c_hout{n}", [128, D], F32, pC) for n in range(2)]
        d_h = dbg_out("h", [NOWN * 128, D]) if "h" in dbg else None
        for gi in range(ngrp):
            for bl in range(4):
                i = 4 * gi + bl
                norm_block(x_own[i * 128:(i + 1) * 128, :], A1, SH1, uT4[:, :, bl * 128:(bl + 1) * 128].k(("uT4", bl)),
                           W, i)
                O.dma("sp", ygl[:, :, bl * 128:(bl + 1) * 128].k(("ygl", bl)), yg_d[i].rearrange("p (c t) -> p c t", c=4))
                O.dma("sp", attl[:, :, bl * 128:(bl + 1) * 128].k(("attl", bl)),
                      att_d[i].rearrange("p (c t) -> p c t", c=4))
            uk = [("uT4", bl) for bl in range(4)]
            yk = [("ygl", bl) for bl in range(4)]
            ak = [("attl", bl) for bl in range(4)]
            for w in range(2):
                for ft in range(8):
                    bank = PS[1 + ft % 2]
                    for kt in range(8):
                        O.mm(bank.v, wg[:, kt, w * 1024 + ft * 128:w * 1024 + (ft + 1) * 128], uT4[:, kt, :].k(*uk),
                             kt == 0, kt == 7)
                    O.act(sg[w][:, ft, :].k((sg[w].key, ft)), bank.v, AF.Sigmoid)
            for ft in range(8):
                sgt, ys = sgt2[ft % 2], ys2[ft % 2]
                bv, bg_ = (PS[3], PS[4]) if ft % 2 == 0 else (PS[7], PS[0])
                for c4 in range(4):
                    O.mm(bv.v, wglu[:, c4, ft * 128:(ft + 1) * 128], ygl[:, c4, :].k(*yk), c4 == 0, c4 == 3)
                for c4 in range(4):
                    O.mm(bg_.v, wglu[:, c4, 1024 + ft * 128:1024 + (ft + 1) * 128], ygl[:, c4, :].k(*yk),
                         c4 == 0, c4 == 3)
                O.act(sgt.v, bg_.v, AF.Sigmoid, bias=bglu[:, 8 + ft:9 + ft])
                O.stt(ys.v, bv.v, bglu[:, ft:ft + 1], sgt.v, ALU.add, ALU.mult)
                O.tt("pool", m1[:, ft, :].k(("m1", ft)), ys.v, sg[0][:, ft, :].k((sg[0].key, ft)), ALU.mult)
            for ft in range(8):
                bank = PS[5 + ft % 2]
                ta = ta2[ft % 2]
                for hp in range(4):
                    O.mm(bank.v, wap[:, hp, ft * 128:(ft + 1) * 128], attl[:, hp, :].k(*ak), hp == 0, hp == 3)
                O.tt("dve", ta.v, bank.v, sg[1][:, ft, :].k((sg[1].key, ft)), ALU.mult)
                O.tt("pool", mT[:, ft, :].k(("mT", ft)), ta.v, m1[:, ft, :].k(("m1", ft)), ALU.add)
            mk = [("mT", ft) for ft in range(8)]
            for bl in range(4):
                i = 4 * gi + bl
                xt = xr[i % 2]
                O.dma("sp", xt.v, x_own[i * 128:(i + 1) * 128, :])
                ho = hout[i % 2]
                for dn in range(2):
                    bank = PS[1 + dn] if bl % 2 == 0 else PS[3 + dn]
                    ta = ta2[dn]
                    for ft in range(8):
                        O.mm(bank.v, mT[:, ft, bl * 128:(bl + 1) * 128].k(*mk), wout[:, ft, dn * 512:(dn + 1) * 512],
                             ft == 0, ft == 7)
                    O.tt("dve", ta.v, bank.v, G1[:, dn * 512:(dn + 1) * 512], ALU.mult)
                    O.tt("pool", ho[:, dn * 512:(dn + 1) * 512].k((ho.key, dn)), ta.v, xt[:, dn * 512:(dn + 1) * 512],
                         ALU.add)
                hk = ho.v.k((ho.key, 0), (ho.key, 1))
                O.dma("sp", h_d[i * 128:(i + 1) * 128, :], hk)
                if d_h is not None:
                    O.dma("sp", d_h[i * 128:(i + 1) * 128, :], hk, final=True)
        S.barrier()
        S.emit()
    if "stopB2" in dbg:
        return

    with contextlib.ExitStack() as pD:
        W = norm_work(pD, "d_", nbuf=1)
        SH2, A2, G2 = load_mod(pD, "d_", [3, 4, 5])
        fgb = sb("d_fgb", [128, D], F32, pD)
        O.dma("sp", fgb.v, final_g.partition_broadcast(128))
        wfg = sb("d_wfg", [128, 8, FF], BF16, pD)
        wfu = sb("d_wfu", [128, 8, FF], BF16, pD)
        wfd = sb("d_wfd", [128, FF // 128, D], BF16, pD)
        for kt0 in range(0, 8, 4):
            O.dma("pool", wfg[:, kt0:kt0 + 4, :], g["w_fg"][kt0 * 128:(kt0 + 4) * 128, :].rearrange("(kt p) c -> p kt c", p=128))
            O.dma("pool", wfu[:, kt0:kt0 + 4, :], g["w_fu"][kt0 * 128:(kt0 + 4) * 128, :].rearrange("(kt p) c -> p kt c", p=128))
        for f0 in range(0, 22, 11):
            O.dma("pool", wfd[:, f0:f0 + 11, :], g["w_fd"][f0 * 128:(f0 + 11) * 128, :].rearrange("(kt p) c -> p kt c", p=128))
        u2T = sb("d_u2T", [128, 8, 512], BF16, pD)
        hid = sb("d_hid", [128, FF // 128, 512], BF16, pD)
        sil2 = [sb(f"d_sil{n}", [128, 512], BF16, pD) for n in range(2)]
        ta2 = [sb(f"d_ta{n}", [128, 512], F32, pD) for n in range(2)]
        hr = W["x"][0]
        h2 = sb("d_h2", [128, D], F32, pD)
        ot = [sb(f"d_ot{n}", [128, D], F32, pD) for n in range(1)]
        st = sb("d_st", [128, 4], F32, pD)
        for gi in range(ngrp):
            for bl in range(4):
                i = 4 * gi + bl
                norm_block(h_d[i * 128:(i + 1) * 128, :], A2, SH2, u2T[:, :, bl * 128:(bl + 1) * 128].k(("u2T", bl)), W, i)
            uk = [("u2T", bl) for bl in range(4)]
            for ft in range(FF // 128):
                bg, bu = PS[1 + ft % 2], PS[3 + ft % 2]
                for kt in range(8):
                    O.mm(bg.v, wfg[:, kt, ft * 128:(ft + 1) * 128], u2T[:, kt, :].k(*uk), kt == 0, kt == 7)
                for kt in range(8):
                    O.mm(bu.v, wfu[:, kt, ft * 128:(ft + 1) * 128], u2T[:, kt, :].k(*uk), kt == 0, kt == 7)
                sil = sil2[ft % 2]
                O.act(sil.v, bg.v, AF.Silu)
                O.tt("dve", hid[:, ft, :].k(("hid", ft)), bu.v, sil.v, ALU.mult)
            hk = [("hid", ft) for ft in range(FF // 128)]
            for bl in range(4):
                i = 4 * gi + bl
                O.dma("sp", hr.v, h_d[i * 128:(i + 1) * 128, :])
                for dn in range(2):
                    bank = PS[5 + dn]
                    for ft in range(FF // 128):
                        O.mm(bank.v, hid[:, ft, bl * 128:(bl + 1) * 128].k(*hk), wfd[:, ft, dn * 512:(dn + 1) * 512],
                             ft == 0, ft == FF // 128 - 1)
                    ta = ta2[dn]
                    O.tt("dve", ta.v, bank.v, G2[:, dn * 512:(dn + 1) * 512], ALU.mult)
                    O.tt("pool", h2[:, dn * 512:(dn + 1) * 512].k(("h2", dn)), ta.v, hr[:, dn * 512:(dn + 1) * 512],
                         ALU.add)
                h2k = h2.v.k(("h2", 0), ("h2", 1))
                o_t = ot[0]
                O.act(o_t.v, h2k, AF.Square, accum=st[:, 0:1].k("st0"))
                O.ts("dve", st[:, 1:2].k("st1"), st[:, 0:1].k("st0"), 1.0 / D, ALU.mult, EPS, ALU.add)
                O.act(st[:, 1:2].k("st1"), st[:, 1:2].k("st1"), AF.Sqrt)
                O.recip(st[:, 2:3].k("st2"), st[:, 1:2].k("st1"))
                O.stt(o_t.v, h2k, st[:, 2:3].k("st2"), fgb.v, ALU.mult, ALU.mult)
                O.dma("sp", out[i * 128:(i + 1) * 128, :], o_t.v, final=True)
        S.barrier()
        S.emit()
def _consts(p):
    bf = ml_dtypes.bfloat16
    ident = np.eye(128, dtype=np.float32)
    negi8 = np.tile(-BIGM * ident, (1, 8)).astype(bf)
    t = np.arange(128)[:, None]
    s = np.arange(128)[None, :]
    tri = np.where(s <= t, 0.0, NEG).astype(np.float32)
    full = np.full((128, 128), NEG, np.float32)
    zero = np.zeros((128, 128), np.float32)
    cm2 = np.concatenate([tri, full], 1) if p == 0 else np.concatenate([zero, tri], 1)
    cm2p = np.where(cm2 < 0, 1.0e30, 0.0).astype(np.float32)
    selv = np.zeros((128, 2), np.float32)
    selv[:, p] = 1.0
    pp = np.arange(128)
    maskf = np.stack([((pp // 32) % 2 == w) & ((pp // 16) % 2 == g2) for w in range(2) for g2 in range(2)],
                     1).astype(np.float32)
    masks = np.stack([(pp // 64 == 0), (pp // 64 == 1)], 1).astype(np.float32)
    pow2 = np.tile((0.5 ** np.arange(1, NBIS + 1))[None, :], (128, 1)).astype(np.float32)
    jv = np.tile(np.arange(9, dtype=np.float32)[None, :], (128, 1))
    iv = np.tile(np.arange(64, dtype=np.float32)[None, :], (128, 1))
    return dict(k_jv=jv, k_iv=iv, k_identb=ident.astype(bf), k_identf=ident, k_negi8=negi8, k_cm2=cm2, k_cm2p=cm2p,
                k_sel=selv, k_maskf=maskf, k_masks=masks, k_pow2=pow2)


def make_in_maps(inputs, cores=range(8)):
    f = lambda a: np.ascontiguousarray(np.asarray(a, dtype=np.float32))
    c_ = np.ascontiguousarray
    x = f(inputs["x"])
    shared = dict(
        w_mod=f(inputs["w_mod"])[0], b_mod=f(inputs["b_mod"])[0], norm1_g=f(inputs["norm1_g"])[0],
        w_in=f(inputs["w_in"])[0],
        w_ssm_glu=f(inputs["w_ssm_glu"])[0], b_ssm_glu=f(inputs["b_ssm_glu"])[0],
        kv_norm_g=f(inputs["kv_norm_g"])[0], idx_k_norm_g=f(inputs["idx_k_norm_g"])[0],
        w_ukT=c_(f(inputs["w_uk"])[0].reshape(128, 4, 2, 64).transpose(2, 3, 1, 0).reshape(128, 4, 128)),
        w_uv=f(inputs["w_uv"])[0].reshape(128, 512),
        w_attn_proj=f(inputs["w_attn_proj"])[0], w_out=f(inputs["w_out"])[0], norm2_g=f(inputs["norm2_g"])[0],
        w_ffn_gate=f(inputs["w_ffn_gate"])[0], w_ffn_up=f(inputs["w_ffn_up"])[0],
        w_ffn_down=f(inputs["w_ffn_down"])[0], final_g=f(inputs["final_g"]),
    )
    are, aim, ldt = f(inputs["ssm_a_re"])[0], f(inputs["ssm_a_im"])[0], f(inputs["ssm_log_dt"])[0]
    bre, bim = f(inputs["ssm_b_re"])[0], f(inputs["ssm_b_im"])[0]
    cre, cim = f(inputs["ssm_c_re"])[0], f(inputs["ssm_c_im"])[0]
    sl_a = lambda a: c_(a.reshape(16, 2, 64).transpose(1, 2, 0).reshape(128, 16))
    fl_a = lambda a: c_(np.broadcast_to(a.reshape(4, 8, 64).transpose(1, 0, 2)[:, None], (8, 16, 4, 64)).reshape(128, 256))
    shared.update(
        sl_are=sl_a(are), sl_aim=sl_a(aim),
        sl_ldt=c_(np.broadcast_to(ldt.reshape(16, 2).T[:, None, :], (2, 64, 16)).reshape(128, 16)),
        fl_are=fl_a(are), fl_aim=fl_a(aim),
        fl_ldt=c_(np.broadcast_to(ldt.reshape(4, 8).T[:, None, :, None], (8, 16, 4, 64)).reshape(128, 256)),
        sl_bre=c_(bre.reshape(16, 2, 64, 16).transpose(1, 2, 0, 3).reshape(128, 16, 16)),
        sl_bim=c_(bim.reshape(16, 2, 64, 16).transpose(1, 2, 0, 3).reshape(128, 16, 16)),
        sl_cre=c_(cre.reshape(16, 2, 16, 64).transpose(1, 3, 0, 2).reshape(128, 16, 16)),
        sl_cim=c_(cim.reshape(16, 2, 16, 64).transpose(1, 3, 0, 2).reshape(128, 16, 16)),
        fl_bre=c_(bre.reshape(4, 8, 64, 16).transpose(1, 3, 0, 2).reshape(128, 256)),
        fl_bim=c_(bim.reshape(4, 8, 64, 16).transpose(1, 3, 0, 2).reshape(128, 256)),
        dsk=c_(f(inputs["ssm_d"])[0].reshape(4, 128).T),
    )
    maps = []
    for core in cores:
        b, p = core // 2, core % 2
        xb = x[b]
        x_own = np.ascontiguousarray(xb.reshape(NB, 128, D)[p::2].reshape(NOWN * 128, D))
        m = dict(shared)
        m.update(x_all=xb, x_own=x_own, c=f(inputs["c"])[b])
        m.update(_consts(p))
        maps.append(m)
    return maps


def kernel(**inputs):
    nc = build()
    maps = make_in_maps(inputs)
    res = run_bass_kernel_spmd(nc, maps, core_ids=list(range(8)))
    x = np.asarray(inputs["x"])
    outp = np.empty(x.shape, np.float32)
    for core in range(8):
        b, p = core // 2, core % 2
        o = np.asarray(res.results[core]["out"]).reshape(NOWN, 128, D)
        outp[b].reshape(NB, 128, D)[p::2] = o
    return outp
```
